# Optimizing a Trainium2 kernel written in Bass

```python
import jax, jax.numpy as jnp
from jax import lax
import numpy as np

D_MODEL = 1024
BATCH = 2
SEQ = 16384
DEPTH = 2

RWKV_HEADS = 8
RWKV_HEAD_DIM = 64
RWKV_WIDTH = RWKV_HEADS * RWKV_HEAD_DIM
DECAY_LORA = 64
AAA_LORA = 64
GATE_LORA = 128
RWKV_GN_EPS = 64e-5
RWKV_COLS = 3 * RWKV_WIDTH + DECAY_LORA + AAA_LORA + GATE_LORA
CONF_WIDTH = 256
CONF_CONV_WIDTH = 31
CONF_COLS = 2 * CONF_WIDTH
SHORT_WIDTH = 256
SHORT_CONV_WIDTH = 3
SHORT_COLS = 3 * SHORT_WIDTH
N_BRANCHES = 3
GATE_COLS = N_BRANCHES * D_MODEL
IN_COLS = RWKV_COLS + CONF_COLS + SHORT_COLS + GATE_COLS
D_FF = 2816
N_EXPERTS = 8
TOP_K = 2
EXPERT_FF = 2816
MOE_BLOCK = 256
LN_EPS = 1e-5

kernel_name = "hybrid_rwkv7_conformer_shortconv_moe_deepnorm"


def _layernorm(x, g, b, eps=LN_EPS):
    xf = x.astype(jnp.float32)
    mu = jnp.mean(xf, axis=-1, keepdims=True)
    var = jnp.mean(jnp.square(xf - mu), axis=-1, keepdims=True)
    return ((xf - mu) * lax.rsqrt(var + eps) * g + b).astype(x.dtype)


def _causal_depthwise_conv(x, w):
    width, ch = w.shape
    return lax.conv_general_dilated(
        x, w[:, None, :].astype(x.dtype), window_strides=(1,), padding=[(width - 1, 0)],
        dimension_numbers=("NWC", "WIO", "NWC"), feature_group_count=ch)


def _rwkv7_scan(r, decay, k, v, kk, a):
    bn, _, h, n = r.shape

    def step(state, inp):
        r_t, w_t, k_t, v_t, kk_t, a_t = inp
        sa = jnp.einsum("bhvk,bhk->bhv", state, -kk_t)
        state = (state * w_t[:, :, None, :]
                 + sa[..., None] * (kk_t * a_t)[:, :, None, :]
                 + v_t[..., None] * k_t[:, :, None, :])
        out = jnp.einsum("bhvk,bhk->bhv", state, r_t)
        return state, out

    xs = tuple(jnp.moveaxis(t, 1, 0) for t in (r, decay, k, v, kk, a))
    s0 = jnp.zeros((bn, h, n, n), jnp.float32)
    _, out = lax.scan(step, s0, xs)
    return jnp.moveaxis(out, 0, 1)


def _rwkv7_branch(p, mu, w0, w2, a0, a2, g2, k_k, k_a, r_k, ln_g, ln_b, w_o):
    bn, s, _ = p.shape
    H, N, W = RWKV_HEADS, RWKV_HEAD_DIM, RWKV_WIDTH
    f32 = jnp.float32
    p_prev = jnp.pad(p, ((0, 0), (1, 0), (0, 0)))[:, :-1]
    p = p + (p_prev - p) * mu
    cuts = [W, 2 * W, 3 * W, 3 * W + DECAY_LORA, 3 * W + DECAY_LORA + AAA_LORA]
    r, k, v, pw, pa, pg = jnp.split(p, cuts, axis=-1)
    w = -jax.nn.softplus(-(w0 + jnp.tanh(pw) @ w2).astype(f32)) - 0.5
    decay = jnp.exp(-jnp.exp(w))
    a = jax.nn.sigmoid((a0 + pa @ a2).astype(f32))
    g = jax.nn.sigmoid(pg) @ g2
    heads = lambda t: t.reshape(bn, s, H, N)
    k = k.astype(f32)
    kk = heads(k * k_k)
    kk = kk / jnp.maximum(jnp.sqrt(jnp.sum(kk * kk, axis=-1, keepdims=True)), 1e-12)
    k = k * (1.0 + (a - 1.0) * k_a)
    r4, k4, v4 = heads(r.astype(f32)), heads(k), heads(v.astype(f32))
    o = _rwkv7_scan(r4, heads(decay), k4, v4, kk, heads(a))
    mean = jnp.mean(o, axis=-1, keepdims=True)
    var = jnp.mean(jnp.square(o - mean), axis=-1, keepdims=True)
    o = (o - mean) * lax.rsqrt(var + RWKV_GN_EPS) * ln_g.reshape(H, N) + ln_b.reshape(H, N)
    o = o + jnp.sum(r4 * k4 * r_k, axis=-1, keepdims=True) * v4
    o = (o.reshape(bn, s, W) * g).astype(p.dtype)
    return o @ w_o


def _conformer_conv_branch(p, dw, dw_b, ln_g, ln_b, w_o):
    u = p[..., :CONF_WIDTH] * jax.nn.sigmoid(p[..., CONF_WIDTH:])
    u = _causal_depthwise_conv(u, dw) + dw_b
    u = jax.nn.silu(_layernorm(u, ln_g, ln_b))
    return u @ w_o


def _short_conv_branch(p, dw, w_o):
    gb, gc, h = jnp.split(p, 3, axis=-1)
    return (gb * _causal_depthwise_conv(gc * h, dw)) @ w_o


def _hybrid_mixer(x, w_in, rwkv_mu, rwkv_w0, rwkv_w2, rwkv_a0, rwkv_a2, rwkv_g2, rwkv_k_k,
                  rwkv_k_a, rwkv_r_k, rwkv_ln_g, rwkv_ln_b, rwkv_w_o, conf_dw, conf_dw_b,
                  conf_ln_g, conf_ln_b, conf_w_o, short_dw, short_w_o, w_out):
    p = x @ w_in
    c1 = RWKV_COLS
    c2 = c1 + CONF_COLS
    c3 = c2 + SHORT_COLS
    p_rwkv, p_conf, p_short, p_gate = jnp.split(p, [c1, c2, c3], axis=-1)
    y_a = _rwkv7_branch(p_rwkv, rwkv_mu, rwkv_w0, rwkv_w2, rwkv_a0, rwkv_a2, rwkv_g2,
                        rwkv_k_k, rwkv_k_a, rwkv_r_k, rwkv_ln_g, rwkv_ln_b, rwkv_w_o)
    y_b = _conformer_conv_branch(p_conf, conf_dw, conf_dw_b, conf_ln_g, conf_ln_b, conf_w_o)
    y_c = _short_conv_branch(p_short, short_dw, short_w_o)
    g_a, g_b, g_c = jnp.split(jax.nn.sigmoid(p_gate), N_BRANCHES, axis=-1)
    return (g_a * y_a.astype(x.dtype) + g_b * y_b + g_c * y_c) @ w_out


def _swiglu(x, w_gate, w_up, w_down):
    return (jax.nn.silu(x @ w_gate) * (x @ w_up)) @ w_down


def _moe_swiglu(x, w_router, w_gate, w_up, w_down):
    bn, s, d = x.shape
    xf = x.reshape(-1, d)
    n_tok = xf.shape[0]
    logits = (xf @ w_router).astype(jnp.float32)
    top_logit, top_idx = lax.top_k(logits, TOP_K)
    top_w = jax.nn.softmax(top_logit, axis=-1)
    n_assign = n_tok * TOP_K
    expert = top_idx.reshape(-1).astype(jnp.int32)
    token = jnp.repeat(jnp.arange(n_tok, dtype=jnp.int32), TOP_K)
    weight = top_w.reshape(-1)
    order = jnp.argsort(expert)
    s_expert, s_token, s_weight = expert[order], token[order], weight[order]
    counts = jnp.bincount(expert, length=N_EXPERTS).astype(jnp.int32)
    starts = jnp.cumsum(counts) - counts
    padded = ((counts + MOE_BLOCK - 1) // MOE_BLOCK) * MOE_BLOCK
    pad_ends = jnp.cumsum(padded)
    pad_starts = pad_ends - padded
    rank = jnp.arange(n_assign, dtype=jnp.int32) - starts[s_expert]
    dest = pad_starts[s_expert] + rank
    n_rows = (-(-n_assign // MOE_BLOCK) + N_EXPERTS) * MOE_BLOCK
    n_blocks = n_rows // MOE_BLOCK
    row_token = jnp.zeros((n_rows,), jnp.int32).at[dest].set(s_token)
    row_weight = jnp.zeros((n_rows,), jnp.float32).at[dest].set(s_weight)
    block_start = jnp.arange(n_blocks, dtype=jnp.int32) * MOE_BLOCK
    block_expert = jnp.minimum(jnp.searchsorted(pad_ends, block_start, side="right"),
                               N_EXPERTS - 1)
    xb = xf[row_token].reshape(n_blocks, MOE_BLOCK, d)

    def expert_block(args):
        xs, e = args
        return (jax.nn.silu(xs @ w_gate[e]) * (xs @ w_up[e])) @ w_down[e]

    yb = lax.map(expert_block, (xb, block_expert))
    y_rows = yb.reshape(n_rows, d) * row_weight[:, None].astype(x.dtype)
    out = jnp.zeros_like(xf).at[row_token].add(y_rows)
    return out.reshape(bn, s, d)


def setup_inputs(seed: int = 0) -> dict:
    key = jax.random.key(seed)
    ks = jax.random.split(key, 40)
    L = DEPTH
    n_dense = (DEPTH + 1) // 2
    n_moe = DEPTH // 2
    beta = (8.0 * DEPTH) ** -0.25
    D, W = D_MODEL, RWKV_WIDTH

    def nrm(k, shape, scale):
        return jax.random.normal(k, shape, jnp.float32) * scale

    w0_base = -6.0 + 5.0 * (jnp.arange(W, dtype=jnp.float32) / (W - 1)) ** 0.9
    return {
        "x": nrm(ks[0], (BATCH, SEQ, D), 1.0),
        "w_in": nrm(ks[1], (L, D, IN_COLS), D ** -0.5),
        "rwkv_mu": jax.random.uniform(ks[2], (L, RWKV_COLS), jnp.float32),
        "rwkv_w0": w0_base + nrm(ks[3], (L, W), 0.1),
        "rwkv_w2": nrm(ks[4], (L, DECAY_LORA, W), 0.1 * DECAY_LORA ** -0.5),
        "rwkv_a0": nrm(ks[5], (L, W), 0.1),
        "rwkv_a2": nrm(ks[6], (L, AAA_LORA, W), AAA_LORA ** -0.5),
        "rwkv_g2": nrm(ks[7], (L, GATE_LORA, W), GATE_LORA ** -0.5),
        "rwkv_k_k": 0.85 + nrm(ks[8], (L, W), 0.02),
        "rwkv_k_a": 1.0 + nrm(ks[9], (L, W), 0.02),
        "rwkv_r_k": nrm(ks[10], (L, RWKV_HEADS, RWKV_HEAD_DIM), 0.1),
        "rwkv_ln_g": 1.0 + nrm(ks[11], (L, W), 0.02),
        "rwkv_ln_b": nrm(ks[12], (L, W), 0.02),
        "rwkv_w_o": nrm(ks[13], (L, W, D), W ** -0.5),
        "conf_dw": nrm(ks[14], (L, CONF_CONV_WIDTH, CONF_WIDTH), CONF_CONV_WIDTH ** -0.5),
        "conf_dw_b": nrm(ks[15], (L, CONF_WIDTH), 0.02),
        "conf_ln_g": 1.0 + nrm(ks[16], (L, CONF_WIDTH), 0.02),
        "conf_ln_b": nrm(ks[17], (L, CONF_WIDTH), 0.02),
        "conf_w_o": nrm(ks[18], (L, CONF_WIDTH, D), CONF_WIDTH ** -0.5),
        "short_dw": nrm(ks[19], (L, SHORT_CONV_WIDTH, SHORT_WIDTH), SHORT_CONV_WIDTH ** -0.5),
        "short_w_o": nrm(ks[20], (L, SHORT_WIDTH, D), SHORT_WIDTH ** -0.5),
        "w_out": nrm(ks[21], (L, D, D), beta * D ** -0.5),
        "ln1_g": 1.0 + nrm(ks[22], (L, D), 0.02),
        "ln1_b": nrm(ks[23], (L, D), 0.02),
        "ffn_w_gate": nrm(ks[24], (n_dense, D, D_FF), D ** -0.5),
        "ffn_w_up": nrm(ks[25], (n_dense, D, D_FF), D ** -0.5),
        "ffn_w_down": nrm(ks[26], (n_dense, D_FF, D), beta * D_FF ** -0.5),
        "moe_router": nrm(ks[27], (n_moe, D, N_EXPERTS), D ** -0.5),
        "moe_w_gate": nrm(ks[28], (n_moe, N_EXPERTS, D, EXPERT_FF), D ** -0.5),
        "moe_w_up": nrm(ks[29], (n_moe, N_EXPERTS, D, EXPERT_FF), D ** -0.5),
        "moe_w_down": nrm(ks[30], (n_moe, N_EXPERTS, EXPERT_FF, D), beta * EXPERT_FF ** -0.5),
        "ln2_g": 1.0 + nrm(ks[31], (L, D), 0.02),
        "ln2_b": nrm(ks[32], (L, D), 0.02),
    }


def reference(x, w_in, rwkv_mu, rwkv_w0, rwkv_w2, rwkv_a0, rwkv_a2, rwkv_g2, rwkv_k_k,
              rwkv_k_a, rwkv_r_k, rwkv_ln_g, rwkv_ln_b, rwkv_w_o, conf_dw, conf_dw_b,
              conf_ln_g, conf_ln_b, conf_w_o, short_dw, short_w_o, w_out, ln1_g, ln1_b,
              ffn_w_gate, ffn_w_up, ffn_w_down, moe_router, moe_w_gate, moe_w_up,
              moe_w_down, ln2_g, ln2_b):
    alpha = (2.0 * DEPTH) ** 0.25
    for i in range(DEPTH):
        mix = _hybrid_mixer(x, w_in[i], rwkv_mu[i], rwkv_w0[i], rwkv_w2[i], rwkv_a0[i],
                            rwkv_a2[i], rwkv_g2[i], rwkv_k_k[i], rwkv_k_a[i], rwkv_r_k[i],
                            rwkv_ln_g[i], rwkv_ln_b[i], rwkv_w_o[i], conf_dw[i], conf_dw_b[i],
                            conf_ln_g[i], conf_ln_b[i], conf_w_o[i], short_dw[i],
                            short_w_o[i], w_out[i])
        x = _layernorm(alpha * x + mix, ln1_g[i], ln1_b[i])
        j = i // 2
        if i % 2 == 0:
            f = _swiglu(x, ffn_w_gate[j], ffn_w_up[j], ffn_w_down[j])
        else:
            f = _moe_swiglu(x, moe_router[j], moe_w_gate[j], moe_w_up[j], moe_w_down[j])
        x = _layernorm(alpha * x + f, ln2_g[i], ln2_b[i])
    return x
```

```python
import numpy as np
from contextlib import ExitStack
import concourse.bass as bass
import concourse.mybir as mybir
from concourse.bass_utils import run_bass_kernel_spmd

F32 = mybir.dt.float32
BF16 = mybir.dt.bfloat16
AF = mybir.ActivationFunctionType
ALU = mybir.AluOpType
AX = mybir.AxisListType

D = 1024
TG = 128
CH = 64
C0 = float(np.exp(-0.5))
ALPHA = float(4.0 ** 0.25)
LN_EPS = 1e-5
GN_EPS = 64e-5
NF = 2816
NFG = 11
K_ID, K_I2, K_MKA, K_ML, K_OB, K_OF, K_SM, K_END = 0, 128, 192, 320, 384, 512, 640, 1152
NVEC = 108


class Tok:
    __slots__ = ("w", "r", "sem", "cnt", "name")

    def __init__(self, name="t"):
        self.name = name
        self.w = {}
        self.r = {}
        self.sem = None
        self.cnt = 0


class DSem:
    __slots__ = ("h", "cnt", "q")

    def __init__(self, h, q):
        self.h = h
        self.cnt = 0
        self.q = q


class Buf:
    def __init__(self, t, name="b"):
        self.t = t
        self.k = Tok(name)


class Prog:
    def __init__(self, nc, es):
        self.nc = nc
        self.es = es
        self.eng = {"pe": nc.tensor, "act": nc.scalar, "dve": nc.vector, "pool": nc.gpsimd, "sp": nc.sync}
        self.sem = {}
        self.cnt = {}
        self.known = {e: {} for e in self.eng}
        for e in ("pe", "act", "dve", "pool"):
            self.sem[e] = es.enter_context(nc.semaphore("sem_" + e))
            self.cnt[e] = 0
        self.nsem = 0
        self.dsems = []
        self.free_dsems = {"sp": [], "pool": []}
        self.banks = []
        self.bi = 0
        self.bbanks = []
        self.bbi = 0
        self.dummy = None
        self.ninst = 0

    def _wait(self, e, deps):
        kn = self.known[e]
        need = {}
        for (sem, val, owner) in deps:
            if owner is not None:
                if owner == e and e == "pe":
                    continue
                assert val <= self.cnt[owner], "uncovered dependency"
            key = id(sem)
            if kn.get(key, 0) >= val:
                continue
            if key not in need or need[key][1] < val:
                need[key] = (sem, val)
        for key, (sem, val) in need.items():
            self.eng[e].wait_ge(sem, val)
            kn[key] = val
            self.ninst += 1

    @staticmethod
    def _put(d, rec):
        key = id(rec[0])
        if key not in d or d[key][1] < rec[1]:
            d[key] = rec

    def op(self, e, fn, R=(), W=(), inc=True):
        deps = []
        for t in R:
            deps += list(t.w.values())
        for t in W:
            deps += list(t.w.values())
            deps += list(t.r.values())
        self._wait(e, deps)
        ins = fn()
        self.ninst += 1
        if inc:
            self.cnt[e] += 1
            ins.then_inc(self.sem[e], 1)
            rec = (self.sem[e], self.cnt[e], e)
        else:
            rec = (self.sem[e], self.cnt[e] + 1, e)
        for t in R:
            self._put(t.r, rec)
        for t in W:
            t.w = {id(rec[0]): rec}
            t.r = {}
        return ins

    def dma(self, q, out, in_, R=(), W=(), part=False, own=None):
        deps = []
        for t in R:
            deps += list(t.w.values())
        for t in W:
            if not part:
                deps += list(t.w.values())
            deps += list(t.r.values())
        self._wait(q, deps)
        ins = self.eng[q].dma_start(out=out, in_=in_)
        self.ninst += 1
        t0 = own if own is not None else W[0]
        if t0.sem is None:
            if self.free_dsems[q]:
                t0.sem = self.free_dsems[q].pop()
            else:
                t0.sem = DSem(self.es.enter_context(self.nc.semaphore("d%d" % self.nsem)), q)
                self.nsem += 1
                self.dsems.append(t0.sem)
        assert t0.sem.q == q, "token DMA'd from two queue kinds"
        t0.sem.cnt += 16
        ins.then_inc(t0.sem.h, 16)
        rec = (t0.sem.h, t0.sem.cnt, None)
        for t in R:
            self._put(t.r, rec)
        for t in W:
            if part:
                self._put(t.w, rec)
            else:
                t.w = {id(rec[0]): rec}
                t.r = {}
        return ins

    def barrier(self):
        f = "pool"
        deps = [(self.sem[e], self.cnt[e], e) for e in ("pe", "act", "dve")]
        deps += [(d.h, d.cnt, None) for d in self.dsems]
        self._wait(f, deps)
        if self.cnt[f] > 0:
            self.eng[f].wait_ge(self.sem[f], self.cnt[f])
        ins = self.nc.gpsimd.memset(self.dummy.t[0:1, 0:1], 0.0)
        self.cnt[f] += 1
        ins.then_inc(self.sem[f], 1)
        for e in ("pe", "act", "dve", "sp"):
            self.eng[e].wait_ge(self.sem[f], self.cnt[f])
        for e in self.eng:
            kn = self.known[e]
            for c in ("pe", "act", "dve", "pool"):
                kn[id(self.sem[c])] = self.cnt[c]
            for d in self.dsems:
                kn[id(d.h)] = d.cnt

    def release(self, st):
        for b in getattr(st, "_bufs", []):
            if b.k.sem is not None:
                self.free_dsems[b.k.sem.q].append(b.k.sem)
                b.k.sem = None

    def bank(self):
        b = self.banks[self.bi % len(self.banks)]
        self.bi += 1
        return b

    def bbank(self):
        b = self.bbanks[self.bbi % len(self.bbanks)]
        self.bbi += 1
        return b


def build(S, layers=(0, 1), moe=(False, True), NT=1024, debug_xm=False, phases="XRBGF", rstop=0, mdbg=9):
    nc = bass.Bass("TRN2", target_bir_lowering=False)
    es = ExitStack()
    P = Prog(nc, es)
    V, A, T = nc.vector, nc.scalar, nc.tensor
    npass = S // NT
    assert S % NT == 0 and NT % 512 == 0
    nlast = len(layers) - 1

    def din(name, shape):
        return nc.dram_tensor(name, list(shape), F32, kind="ExternalInput")

    x_in = din("x", [S, D])
    y_out = nc.dram_tensor("y", [S, D], F32, kind="ExternalOutput")
    xl1 = nc.dram_tensor("xl1", [S, D], F32)
    if debug_xm:
        xm = nc.dram_tensor("xm", [S, D], F32, kind="ExternalOutput")
    else:
        xm = nc.dram_tensor("xm", [S, D], F32)
    consts_d = din("consts", [128, K_END])
    LW = {}
    for l in layers:
        E = 8 if moe[l] else 1
        LW[l] = dict(
            winA=din("winA%d" % l, [14, 128, 8, 128]), winB=din("winB%d" % l, [34, 128, 8, 128]),
            vec=din("vec%d" % l, [128, NVEC]), vec64=din("vec64_%d" % l, [64, 16]),
            w2a2=din("w2a2_%d" % l, [128, 512]), g2=din("g2_%d" % l, [128, 512]),
            woa=din("woa%d" % l, [64, 8, 1024]), wob=din("wob%d" % l, [128, 2, 1024]),
            woc=din("woc%d" % l, [128, 2, 1024]), wout=din("wout%d" % l, [128, 8, 1024]),
            lnp=din("lnp%d" % l, [128, 4, 1024]),
            wg=din("wg%d" % l, [E, NFG, 128, 8, 256]), wu=din("wu%d" % l, [E, NFG, 128, 8, 256]),
            wd=din("wd%d" % l, [E, NFG, 128, 2, 1024]))
        if moe[l]:
            LW[l]["router"] = din("router%d" % l, [128, 8, 8])
    tok_y = Tok("y")
    tok_xl1 = Tok("xl1")
    tok_xm = Tok("xm")

    uid = [0]

    def sb(st, name, shape, dt):
        uid[0] += 1
        b = Buf(st.enter_context(nc.sbuf_tensor("%s_u%d" % (name, uid[0]), list(shape), dt)), name)
        if not hasattr(st, "_bufs"):
            st._bufs = []
        st._bufs.append(b)
        return b

    def dve(fn, R, W):
        return P.op("dve", fn, R, W)

    def act(fn, R, W):
        return P.op("act", fn, R, W)

    def mm(out, lhsT, rhs, start, stop, R, W, inc):
        return P.op("pe", lambda: T.matmul(out, lhsT, rhs, start=start, stop=stop), R, W, inc)

    for i in range(6):
        P.banks.append(Buf(es.enter_context(nc.psum_tensor("pb%d" % i, [128, 512], F32))))
    for i in range(2):
        P.bbanks.append(Buf(es.enter_context(nc.psum_tensor("pbb%d" % i, [128, 1024], BF16))))
    P.dummy = sb(es, "dummy", [128, 4], F32)
    CON = sb(es, "CON", [128, K_END], F32)
    IDB = sb(es, "IDB", [128, 128], BF16)
    XT = sb(es, "XT", [128, 8, NT], BF16)
    OG = sb(es, "OG", [64, 8, NT], BF16)
    UB = sb(es, "UB", [128, 2, NT], BF16)
    UC = sb(es, "UC", [128, 2, NT], BF16)
    UH = sb(es, "UH", [128, 2, 32 + NT], F32)
    GH = sb(es, "GH", [128, 2, 32 + NT], F32)
    VEC = sb(es, "VEC", [128, NVEC], F32)
    OMM = sb(es, "OMM", [128, 14], F32)
    OMKA = sb(es, "OMKA", [128, 4], F32)
    V64 = sb(es, "V64", [64, 16], F32)
    W2A2 = sb(es, "W2A2", [128, 512], BF16)
    G2 = sb(es, "G2", [128, 512], BF16)
    TAILS = sb(es, "TAILS", [128, 14], F32)
    SRING = [sb(es, "SR%d" % i, [64, 8, 64], F32) for i in range(3)]
    GATE = sb(es, "GATE", [128, NT // 128, 8], F32)
    ROUT = sb(es, "ROUT", [128, 8, 8], F32)
    sci = [0]

    P.dma("sp", CON.t[:], consts_d.ap(), W=[CON.k])
    dve(lambda: V.tensor_copy(IDB.t[:], CON.t[:, K_ID:K_ID + 128]), [CON.k], [IDB.k])
    ID = CON.t[:, K_ID:K_ID + 128]

    def cview(lo, hi, rows=128):
        return CON.t[0:rows, lo:hi]

    def phase_X(src, p):
        with ExitStack() as ph:
            xs = [sb(ph, "xs%d" % i, [128, D], F32) for i in range(2)]
            for i in range(NT // 128):
                xb = xs[i % 2]
                r0 = p * NT + i * 128
                P.dma("sp", xb.t[:], src[r0:r0 + 128, :], W=[xb.k])
                for hf in range(2):
                    bk = P.bank()
                    for q in range(4):
                        kc = hf * 4 + q
                        P.op("pe", lambda: T.transpose(bk.t[:, q * 128:(q + 1) * 128], xb.t[:, kc * 128:(kc + 1) * 128], ID),
                             [xb.k, CON.k], [bk.k], inc=(q == 3))
                    o = XT.t[:, hf * 4:hf * 4 + 4, i * 128:(i + 1) * 128]
                    iv = bk.t[:].rearrange("p (q t) -> p q t", q=4)
                    if hf == 0:
                        act(lambda: A.copy(o, iv), [bk.k], [XT.k])
                    else:
                        dve(lambda: V.tensor_copy(o, iv), [bk.k], [XT.k])
            P.barrier()
            P.release(ph)

    def phase_R(l, p):
        w = LW[l]
        with ExitStack() as ph:
            WA = sb(ph, "WA", [128, 14, 8, 128], BF16)
            for g0 in range(0, 14, 7):
                P.dma("pool", WA.t[:, g0:g0 + 7], w["winA"].ap()[g0:g0 + 7].rearrange("g p k c -> p g k c"),
                      W=[WA.k], part=True)
            sl = {n: sb(ph, "R_" + n, [128, 4, TG], F32) for n in ("r", "k", "v", "A", "S", "T1", "C", "T2", "BV")}
            r_, k_, v_, A_, S_, T1, C_, T2, BV = (sl[n] for n in ("r", "k", "v", "A", "S", "T1", "C", "T2", "BV"))
            DG = sb(ph, "DG", [128, 4, 2, 64], F32)
            GC = sb(ph, "GC", [128, 4, 2], F32)
            PWPA = sb(ph, "PWPA", [128, TG], F32)
            PG = sb(ph, "PG", [128, TG], F32)
            MX = [sb(ph, "MX%d" % i, [128, TG], F32) for i in range(2)]
            TP = sb(ph, "TP", [128, TG], BF16)
            SGB = sb(ph, "SGB", [128, TG], BF16)
            AR = sb(ph, "AR", [128, 4, 2, 2, 64], BF16)
            BK = sb(ph, "BK", [128, 4, 2, 2, 64], BF16)
            VB = sb(ph, "VB", [128, 4, TG], BF16)
            BH = sb(ph, "BH", [128, 4, TG], BF16)
            KH = sb(ph, "KH", [128, 4, TG], BF16)
            VTM = sb(ph, "VTM", [64, 2, 512], BF16)
            BHTM = sb(ph, "BHTM", [64, 2, 512], BF16)
            KHTM = sb(ph, "KHTM", [64, 2, 512], BF16)
            AVA = sb(ph, "AVA", [64, 2, 8, 128], BF16)
            ATA = [sb(ph, "ATA%d" % i, [64, 8, 128], BF16) for i in range(2)]
            ATB = [sb(ph, "ATB%d" % i, [64, 8, 128], BF16) for i in range(2)]
            XX = [sb(ph, "XX%d" % i, [64, 8, 64], F32) for i in range(2)]
            YY = [sb(ph, "YY%d" % i, [64, 8, 64], F32) for i in range(2)]
            ZZ = sb(ph, "ZZ", [64, 8, 64], F32)
            TT = [sb(ph, "TT%d" % i, [64, 8, 64], BF16) for i in range(2)]
            UW = [sb(ph, "UW%d" % i, [64, 8, 128], BF16) for i in range(2)]
            GT = [sb(ph, "GT%d" % i, [64, 8, 64], F32) for i in range(2)]
            HH = [sb(ph, "HH%d" % i, [64, 8, 64], F32) for i in range(2)]
            OLOC = sb(ph, "OLOC", [64, 8, TG], F32)
            QT = sb(ph, "QT", [64, 8, TG], F32)
            BV64 = sb(ph, "BV64", [64, 8, TG], F32)
            G64 = sb(ph, "G64", [64, 8, TG], F32)
            DN = sb(ph, "DN", [64, 8, TG], F32)
            SQ = sb(ph, "SQ", [64, 8, TG], F32)

            def vb(c0, n=TG):
                return VEC.t[:, c0:c0 + 4].unsqueeze(2).broadcast_to([128, 4, n])

            def cv(b):
                return b.t[:].rearrange("p j (c t) -> p j c t", t=64)

            def fl(b):
                return b.t[:].rearrange("p j t -> p (j t)")

            MKA = cview(K_MKA, K_MKA + 128, 64)
            ML = cview(K_ML, K_ML + 64, 64)
            ONB = cview(K_OB, K_OB + 128)
            ON64 = CON.t[0:64, K_OB:K_OB + 64]
            I2 = cview(K_I2, K_I2 + 64)
            SMK = cview(K_SM, K_SM + 512)

            for tg in range(NT // TG):
                c0 = tg * TG
                for g in range(14):
                    bk = P.bank()
                    for kc in range(8):
                        mm(bk.t[:, 0:TG], WA.t[:, g, kc, :], XT.t[:, kc, c0:c0 + TG], kc == 0, kc == 7,
                           [WA.k, XT.k], [bk.k], kc == 7)
                    if g < 4:
                        dst, dk = r_.t[:, g, :], r_.k
                    elif g < 8:
                        dst, dk = k_.t[:, g - 4, :], k_.k
                    elif g < 12:
                        dst, dk = v_.t[:, g - 8, :], v_.k
                    elif g == 12:
                        dst, dk = PWPA.t[:], PWPA.k
                    else:
                        dst, dk = PG.t[:], PG.k
                    m_ = MX[g % 2]
                    act(lambda: A.activation(out=m_.t[:], in_=bk.t[:, 0:TG], func=AF.Copy, scale=OMM.t[:, g:g + 1]),
                        [bk.k, OMM.k], [m_.k])
                    dve(lambda: V.scalar_tensor_tensor(dst[:, 1:TG], bk.t[:, 0:TG - 1], VEC.t[:, g:g + 1],
                                                       m_.t[:, 1:TG], ALU.mult, ALU.add),
                        [bk.k, VEC.k, m_.k], [dk])
                    dve(lambda: V.scalar_tensor_tensor(dst[:, 0:1], TAILS.t[:, g:g + 1], VEC.t[:, g:g + 1],
                                                       m_.t[:, 0:1], ALU.mult, ALU.add),
                        [TAILS.k, VEC.k, m_.k], [dk])
                    dve(lambda: V.tensor_copy(TAILS.t[:, g:g + 1], bk.t[:, TG - 1:TG]), [bk.k], [TAILS.k])
                if rstop == 1:
                    P.barrier()
                    P.release(ph)
                    return
                act(lambda: A.activation(out=TP.t[0:64, :], in_=PWPA.t[0:64, :], func=AF.Tanh), [PWPA.k], [TP.k])
                dve(lambda: V.tensor_copy(TP.t[64:128, :], PWPA.t[64:128, :]), [PWPA.k], [TP.k])
                act(lambda: A.activation(out=SGB.t[:], in_=PG.t[:], func=AF.Sigmoid), [PG.k], [SGB.k])
                for j in range(4):
                    bk = P.bank()
                    mm(bk.t[:, 0:TG], W2A2.t[0:64, j * 128:(j + 1) * 128], TP.t[0:64, :], True, True,
                       [W2A2.k, TP.k], [bk.k], True)
                    act(lambda: A.activation(out=S_.t[:, j, :], in_=bk.t[:, 0:TG], func=AF.Sigmoid,
                                             bias=VEC.t[:, 14 + j:15 + j]), [bk.k, VEC.k], [S_.k])
                    bk2 = P.bank()
                    mm(bk2.t[:, 0:TG], W2A2.t[64:128, j * 128:(j + 1) * 128], TP.t[64:128, :], True, True,
                       [W2A2.k, TP.k], [bk2.k], True)
                    act(lambda: A.activation(out=A_.t[:, j, :], in_=bk2.t[:, 0:TG], func=AF.Sigmoid,
                                             bias=VEC.t[:, 18 + j:19 + j]), [bk2.k, VEC.k], [A_.k])
                if rstop == 2:
                    P.barrier()
                    P.release(ph)
                    return
                dve(lambda: V.tensor_tensor(T1.t[:], k_.t[:], vb(22), ALU.mult), [k_.k, VEC.k], [T1.k])
                act(lambda: A.activation(out=T2.t[:], in_=T1.t[:], func=AF.Square), [T1.k], [T2.k])
                bk = P.bank()
                mm(bk.t[:, 0:512], ONB, fl(T2), True, True, [CON.k, T2.k], [bk.k], True)
                act(lambda: A.activation(out=fl(T2), in_=bk.t[:, 0:512], func=AF.Sqrt, bias=1e-24, scale=1.0),
                    [bk.k], [T2.k])
                dve(lambda: V.reciprocal(T2.t[:], T2.t[:]), [T2.k], [T2.k])
                dve(lambda: V.tensor_tensor(T1.t[:], T1.t[:], T2.t[:], ALU.mult), [T1.k, T2.k], [T1.k])
                if rstop == 3:
                    P.barrier()
                    P.release(ph)
                    return
                dve(lambda: V.tensor_tensor_scan(fl(C_), SMK, fl(S_), 0.0, ALU.mult, ALU.add), [CON.k, S_.k], [C_.k])
                act(lambda: A.activation(out=T2.t[:], in_=C_.t[:], func=AF.Exp, scale=-C0), [C_.k], [T2.k])
                dve(lambda: V.tensor_tensor(AR.t[:, :, :, 1, :], cv(r_), cv(T2), ALU.mult), [r_.k, T2.k], [AR.k])
                dve(lambda: V.tensor_tensor(T2.t[:], C_.t[:], S_.t[:], ALU.subtract), [C_.k, S_.k], [T2.k])
                act(lambda: A.activation(out=T2.t[:], in_=T2.t[:], func=AF.Exp, scale=-C0), [T2.k], [T2.k])
                dve(lambda: V.scalar_tensor_tensor(AR.t[:, :, :, 0, :], cv(T1), -1.0, cv(T2), ALU.mult, ALU.mult),
                    [T1.k, T2.k], [AR.k])
                act(lambda: A.activation(out=T2.t[:], in_=C_.t[:], func=AF.Exp, scale=C0), [C_.k], [T2.k])
                dve(lambda: V.tensor_tensor(S_.t[:], T1.t[:], A_.t[:], ALU.mult), [T1.k, A_.k], [S_.k])
                dve(lambda: V.tensor_tensor(BK.t[:, :, :, 0, :], cv(S_), cv(T2), ALU.mult), [S_.k, T2.k], [BK.k])
                for j in range(4):
                    dve(lambda: V.tensor_scalar(A_.t[:, j, :], A_.t[:, j, :], VEC.t[:, 26 + j:27 + j],
                                                OMKA.t[:, j:j + 1], ALU.mult, ALU.add), [A_.k, VEC.k, OMKA.k], [A_.k])
                dve(lambda: V.tensor_tensor(k_.t[:], k_.t[:], A_.t[:], ALU.mult), [k_.k, A_.k], [k_.k])
                dve(lambda: V.tensor_tensor(BK.t[:, :, :, 1, :], cv(k_), cv(T2), ALU.mult), [k_.k, T2.k], [BK.k])
                clast = cv(C_)[:, :, :, 63:64]
                dve(lambda: V.tensor_tensor(cv(T2), clast.broadcast_to([128, 4, 2, 64]), cv(C_), ALU.subtract),
                    [C_.k], [T2.k])
                act(lambda: A.activation(out=T2.t[:], in_=T2.t[:], func=AF.Exp, scale=-C0), [T2.k], [T2.k])
                dve(lambda: V.tensor_tensor(BH.t[:], S_.t[:], T2.t[:], ALU.mult), [S_.k, T2.k], [BH.k])
                dve(lambda: V.tensor_tensor(KH.t[:], k_.t[:], T2.t[:], ALU.mult), [k_.k, T2.k], [KH.k])
                act(lambda: A.activation(out=GC.t[:], in_=cv(C_)[:, :, :, 63], func=AF.Exp, scale=-C0), [C_.k], [GC.k])
                dve(lambda: V.tensor_tensor(DG.t[:], I2.unsqueeze(1).unsqueeze(1).broadcast_to([128, 4, 2, 64]),
                                            GC.t[:].unsqueeze(3).broadcast_to([128, 4, 2, 64]), ALU.mult),
                    [CON.k, GC.k], [DG.k])
                if rstop == 4:
                    P.barrier()
                    P.release(ph)
                    return
                dve(lambda: V.tensor_tensor(T2.t[:], r_.t[:], k_.t[:], ALU.mult), [r_.k, k_.k], [T2.k])
                dve(lambda: V.tensor_tensor(T2.t[:], T2.t[:], vb(30), ALU.mult), [T2.k, VEC.k], [T2.k])
                bk = P.bank()
                mm(bk.t[:, 0:512], ONB, fl(T2), True, True, [CON.k, T2.k], [bk.k], True)
                dve(lambda: V.tensor_tensor(fl(BV), bk.t[:, 0:512], fl(v_), ALU.mult), [bk.k, v_.k], [BV.k])
                act(lambda: A.copy(VB.t[:], v_.t[:]), [v_.k], [VB.k])
                for (srcb, dstb, kind) in ((BV, BV64, 0), (SGB, G64, 1)):
                    bks = [P.bank(), P.bank()]
                    for h in range(8):
                        j, hp = h // 2, h % 2
                        rows = slice(64 * hp, 64 * hp + 64)
                        bkx = bks[h // 4]
                        o = bkx.t[0:64, (h % 4) * 128:(h % 4 + 1) * 128]
                        if kind == 0:
                            mm(o, CON.t[:, K_ID + 64 * hp:K_ID + 64 * hp + 64], BV.t[:, j, :], True, True,
                               [CON.k, BV.k], [bkx.k], h % 4 == 3)
                        else:
                            mm(o, G2.t[:, h * 64:(h + 1) * 64], SGB.t[:], True, True, [G2.k, SGB.k], [bkx.k], h % 4 == 3)
                    for q in range(2):
                        o = dstb.t[:, q * 4:q * 4 + 4, :]
                        iv = bks[q].t[0:64, :].rearrange("p (h t) -> p h t", h=4)
                        if q == 0:
                            act(lambda: A.copy(o, iv), [bks[q].k], [dstb.k])
                        else:
                            dve(lambda: V.tensor_copy(o, iv), [bks[q].k], [dstb.k])
                if rstop == 5:
                    P.barrier()
                    P.release(ph)
                    return
                for c in range(2):
                    for si, (srcb, dstb) in enumerate(((VB, VTM), (BH, BHTM), (KH, KHTM), (AR, AVA))):
                        bb = P.bbank()
                        for j in range(4):
                            if srcb is AR:
                                iv = AR.t[:, j, c, 0, :]
                            else:
                                iv = srcb.t[:, j, c * 64:(c + 1) * 64]
                            P.op("pe", lambda: T.transpose(bb.t[0:64, j * 128:(j + 1) * 128], iv, IDB.t[:]),
                                 [srcb.k, IDB.k], [bb.k], inc=(j == 3))
                        if srcb is AR:
                            o = AVA.t[:, c, :, 64:128]
                            iv2 = bb.t[0:64, 0:512].rearrange("p (h t) -> p h t", h=8)
                        else:
                            o = dstb.t[:, c, :]
                            iv2 = bb.t[0:64, 0:512]
                        if si % 2 == 0:
                            act(lambda: A.copy(o, iv2), [bb.k], [dstb.k])
                        else:
                            dve(lambda: V.tensor_copy(o, iv2), [bb.k], [dstb.k])
                if rstop == 6:
                    P.barrier()
                    P.release(ph)
                    return
                for c in range(2):
                    ata, atb, tt, uw, gt, hh = ATA[c], ATB[c], TT[c], UW[c], GT[c], HH[c]
                    for which, dstb in ((0, ata), (1, atb)):
                        bks = [P.bank(), P.bank()]
                        for h in range(8):
                            j, hp = h // 2, h % 2
                            rows = slice(64 * hp, 64 * hp + 64)
                            bkx = bks[hp]
                            mm(bkx.t[0:64, j * 128:(j + 1) * 128], BK.t[rows, j, c, which, :],
                               AR.t[rows, j, c, :, :], True, True, [BK.k, AR.k], [bkx.k], h >= 6)
                        for q in range(2):
                            iv = bks[q].t[0:64, :].rearrange("p (h t) -> p h t", h=4)
                            ov = dstb.t[:].rearrange("p (j hp) n -> p hp j n", hp=2)[:, q]
                            dve(lambda: V.tensor_tensor(ov, iv, MKA.unsqueeze(1).broadcast_to([64, 4, 128]), ALU.mult),
                                [bks[q].k, CON.k], [dstb.k])
                            if which == 0:
                                ox = XX[0].t[:].rearrange("p (j hp) n -> p hp j n", hp=2)[:, q]
                                dve(lambda: V.tensor_tensor(ox, iv[:, :, 0:64],
                                                            MKA[:, 0:64].unsqueeze(1).broadcast_to([64, 4, 64]), ALU.mult),
                                    [bks[q].k, CON.k], [XX[0].k])
                    bks = [P.bank(), P.bank()]
                    for h in range(8):
                        j, hp = h // 2, h % 2
                        rows = slice(64 * hp, 64 * hp + 64)
                        mm(bks[hp].t[0:64, j * 64:(j + 1) * 64], AR.t[rows, j, c, 0, :], BK.t[rows, j, c, 0, :], True, True,
                           [AR.k, BK.k], [bks[hp].k], h >= 6)
                    for q in range(2):
                        oy = YY[0].t[:].rearrange("p (j hp) n -> p hp j n", hp=2)[:, q]
                        dve(lambda: V.tensor_tensor(oy, bks[q].t[0:64, 0:256].rearrange("p (h t) -> p h t", h=4),
                                                    ML.unsqueeze(1).broadcast_to([64, 4, 64]), ALU.mult),
                            [bks[q].k, CON.k], [YY[0].k])
                    if rstop == 7:
                        P.barrier()
                        return
                    dve(lambda: V.tensor_tensor(ZZ.t[:], XX[0].t[:],
                                                CON.t[0:64, K_ID:K_ID + 64].unsqueeze(1).broadcast_to([64, 8, 64]), ALU.add),
                        [XX[0].k, CON.k], [ZZ.k])
                    xc, yc = XX[0], YY[0]
                    for lvl in range(5):
                        xn, yn = XX[(lvl + 1) % 2], YY[(lvl + 1) % 2]
                        if lvl < 4:
                            px = P.bank()
                            for h in range(8):
                                mm(px.t[0:64, h * 64:(h + 1) * 64], yc.t[:, h, :], xc.t[:, h, :], True, True,
                                   [yc.k, xc.k], [px.k], h == 7)
                        py = P.bank()
                        for h in range(8):
                            mm(py.t[0:64, h * 64:(h + 1) * 64], xc.t[:, h, :], yc.t[:, h, :], True, True,
                               [yc.k, xc.k], [py.k], h == 7)
                        if lvl < 4:
                            act(lambda: A.copy(xn.t[:], px.t[0:64, :].rearrange("p (h t) -> p h t", h=8)), [px.k], [xn.k])
                        dve(lambda: V.tensor_copy(yn.t[:], py.t[0:64, :].rearrange("p (h t) -> p h t", h=8)), [py.k], [yn.k])
                        pz = P.bank()
                        for h in range(8):
                            mm(pz.t[0:64, h * 64:(h + 1) * 64], yn.t[:, h, :], ZZ.t[:, h, :], True, True,
                               [yn.k, ZZ.k], [pz.k], h == 7)
                        dve(lambda: V.tensor_tensor(ZZ.t[:], pz.t[0:64, :].rearrange("p (h t) -> p h t", h=8), ZZ.t[:], ALU.add),
                            [pz.k, ZZ.k], [ZZ.k])
                        xc, yc = xn, yn
                    act(lambda: A.copy(tt.t[:], ZZ.t[:]), [ZZ.k], [tt.k])
                    if rstop == 8:
                        P.barrier()
                        return
                    pv = P.bank()
                    for h in range(8):
                        mm(pv.t[0:64, h * 64:(h + 1) * 64], atb.t[:, h, 0:64], VTM.t[:, c, h * 64:(h + 1) * 64], True, True,
                           [atb.k, VTM.k], [pv.k], h == 7)
                    act(lambda: A.copy(AVA.t[:, c, :, 0:64], pv.t[0:64, :].rearrange("p (h t) -> p h t", h=8)),
                        [pv.k], [AVA.k])
                    bks = [P.bank(), P.bank()]
                    for h in range(8):
                        bkx = bks[h // 4]
                        mm(bkx.t[0:64, (h % 4) * 128:(h % 4 + 1) * 128], tt.t[:, h, :], AVA.t[:, c, h, :], True, True,
                           [tt.k, AVA.k], [bkx.k], h % 4 == 3)
                    act(lambda: A.copy(uw.t[:, 0:4, :], bks[0].t[0:64, :].rearrange("p (h t) -> p h t", h=4)), [bks[0].k], [uw.k])
                    dve(lambda: V.tensor_copy(uw.t[:, 4:8, :], bks[1].t[0:64, :].rearrange("p (h t) -> p h t", h=4)), [bks[1].k], [uw.k])
                    if rstop == 9:
                        P.barrier()
                        return
                    po = P.bank()
                    for h in range(8):
                        o = po.t[0:64, h * 64:(h + 1) * 64]
                        mm(o, uw.t[:, h, 0:64], ata.t[:, h, 64:128], True, False, [uw.k, ata.k], [po.k], False)
                        mm(o, VTM.t[:, c, h * 64:(h + 1) * 64], atb.t[:, h, 64:128], False, True, [VTM.k, atb.k], [po.k], h == 7)
                    act(lambda: A.copy(OLOC.t[:, :, c * 64:(c + 1) * 64], po.t[0:64, :].rearrange("p (h t) -> p h t", h=8)),
                        [po.k], [OLOC.k])
                    pq = P.bank()
                    for h in range(8):
                        j, hp = h // 2, h % 2
                        rows = slice(64 * hp, 64 * hp + 64)
                        o = pq.t[0:64, h * 64:(h + 1) * 64]
                        mm(o, uw.t[:, h, 64:128], ata.t[:, h, 64:128], True, False, [uw.k, ata.k], [pq.k], False)
                        mm(o, IDB.t[:, 64 * hp:64 * hp + 64], AR.t[:, j, c, 1, :], False, True, [IDB.k, AR.k], [pq.k], h == 7)
                    dve(lambda: V.tensor_copy(QT.t[:, :, c * 64:(c + 1) * 64], pq.t[0:64, :].rearrange("p (h t) -> p h t", h=8)),
                        [pq.k], [QT.k])
                    pg_ = P.bank()
                    for h in range(8):
                        j, hp = h // 2, h % 2
                        rows = slice(64 * hp, 64 * hp + 64)
                        o = pg_.t[0:64, h * 64:(h + 1) * 64]
                        mm(o, uw.t[:, h, 64:128], BHTM.t[:, c, h * 64:(h + 1) * 64], True, False, [uw.k, BHTM.k], [pg_.k], False)
                        mm(o, CON.t[:, K_ID + 64 * hp:K_ID + 64 * hp + 64], DG.t[:, j, c, :], False, True,
                           [CON.k, DG.k], [pg_.k], h == 7)
                    act(lambda: A.copy(gt.t[:], pg_.t[0:64, :].rearrange("p (h t) -> p h t", h=8)), [pg_.k], [gt.k])
                    phh = P.bank()
                    for h in range(8):
                        o = phh.t[0:64, h * 64:(h + 1) * 64]
                        mm(o, BHTM.t[:, c, h * 64:(h + 1) * 64], uw.t[:, h, 0:64], True, False, [BHTM.k, uw.k], [phh.k], False)
                        mm(o, KHTM.t[:, c, h * 64:(h + 1) * 64], VTM.t[:, c, h * 64:(h + 1) * 64], False, True,
                           [KHTM.k, VTM.k], [phh.k], h == 7)
                    dve(lambda: V.tensor_copy(hh.t[:], phh.t[0:64, :].rearrange("p (h t) -> p h t", h=8)), [phh.k], [hh.k])
                    if rstop == 10:
                        P.barrier()
                        return
                    scur = SRING[sci[0] % 3]
                    snext = SRING[(sci[0] + 1) % 3]
                    sci[0] += 1
                    pO = P.bank()
                    for h in range(8):
                        mm(pO.t[0:64, h * 64:(h + 1) * 64], scur.t[:, h, :], QT.t[:, h, c * 64:(c + 1) * 64], True, True,
                           [scur.k, QT.k], [pO.k], h == 7)
                    dve(lambda: V.tensor_tensor(OLOC.t[:, :, c * 64:(c + 1) * 64],
                                                pO.t[0:64, :].rearrange("p (h t) -> p h t", h=8),
                                                OLOC.t[:, :, c * 64:(c + 1) * 64], ALU.add), [pO.k, OLOC.k], [OLOC.k])
                    pS = P.bank()
                    for h in range(8):
                        mm(pS.t[0:64, h * 64:(h + 1) * 64], gt.t[:, h, :], scur.t[:, h, :], True, True,
                           [gt.k, scur.k], [pS.k], h == 7)
                    dve(lambda: V.tensor_tensor(snext.t[:], pS.t[0:64, :].rearrange("p (h t) -> p h t", h=8), hh.t[:], ALU.add),
                        [pS.k, hh.k], [snext.k])
                if rstop == 11:
                    P.barrier()
                    P.release(ph)
                    return
                ofl = OLOC.t[:].rearrange("p h t -> p (h t)")
                dfl = DN.t[:].rearrange("p h t -> p (h t)")
                sfl = SQ.t[:].rearrange("p h t -> p (h t)")
                for q in range(2):
                    bk = P.bank()
                    mm(bk.t[0:64, :], ON64, ofl[:, q * 512:(q + 1) * 512], True, True, [CON.k, OLOC.k], [bk.k], True)
                    dve(lambda: V.scalar_tensor_tensor(dfl[:, q * 512:(q + 1) * 512], bk.t[0:64, :], -1.0 / 64,
                                                       ofl[:, q * 512:(q + 1) * 512], ALU.mult, ALU.add),
                        [bk.k, OLOC.k], [DN.k])
                act(lambda: A.activation(out=SQ.t[:], in_=DN.t[:], func=AF.Square), [DN.k], [SQ.k])
                for q in range(2):
                    bk = P.bank()
                    mm(bk.t[0:64, :], ON64, sfl[:, q * 512:(q + 1) * 512], True, True, [CON.k, SQ.k], [bk.k], True)
                    act(lambda: A.activation(out=sfl[:, q * 512:(q + 1) * 512], in_=bk.t[0:64, :], func=AF.Sqrt,
                                             bias=GN_EPS, scale=1.0 / 64), [bk.k], [SQ.k])
                dve(lambda: V.reciprocal(SQ.t[:], SQ.t[:]), [SQ.k], [SQ.k])
                dve(lambda: V.tensor_tensor(DN.t[:], DN.t[:], SQ.t[:], ALU.mult), [DN.k, SQ.k], [DN.k])
                dve(lambda: V.tensor_tensor(DN.t[:], DN.t[:], V64.t[:, 0:8].unsqueeze(2).broadcast_to([64, 8, TG]), ALU.mult),
                    [DN.k, V64.k], [DN.k])
                dve(lambda: V.tensor_tensor(DN.t[:], DN.t[:], V64.t[:, 8:16].unsqueeze(2).broadcast_to([64, 8, TG]), ALU.add),
                    [DN.k, V64.k], [DN.k])
                dve(lambda: V.tensor_tensor(DN.t[:], DN.t[:], BV64.t[:], ALU.add), [DN.k, BV64.k], [DN.k])
                dve(lambda: V.tensor_tensor(OG.t[:, :, c0:c0 + TG], DN.t[:], G64.t[:], ALU.mult), [DN.k, G64.k], [OG.k])
            P.barrier()
            P.release(ph)

    def phase_BC(l, p):
        w = LW[l]
        with ExitStack() as ph:
            WB = sb(ph, "WB", [128, 10, 8, 128], BF16)
            P.dma("pool", WB.t[:], w["winB"].ap()[0:10].rearrange("g p k c -> p g k c"), W=[WB.k])
            TMP = [sb(ph, "bcT%d" % i, [128, 512], F32) for i in range(2)]
            ACC = sb(ph, "bcACC", [128, 2, NT], F32)
            DD = sb(ph, "bcD", [128, 2, 512], F32)
            GBB = sb(ph, "bcGB", [128, 2, NT], F32)
            ONF = cview(K_OF, K_OF + 128)
            ntg = NT // 512

            def proj(g, tg):
                bk = P.bank()
                for kc in range(8):
                    mm(bk.t[:], WB.t[:, g, kc, :], XT.t[:, kc, tg * 512:(tg + 1) * 512], kc == 0, kc == 7,
                       [WB.k, XT.k], [bk.k], kc == 7)
                return bk

            for tg in range(ntg):
                for c in range(2):
                    bu = proj(c, tg)
                    bg = proj(2 + c, tg)
                    tm = TMP[c]
                    act(lambda: A.activation(out=tm.t[:], in_=bg.t[:], func=AF.Sigmoid), [bg.k], [tm.k])
                    dve(lambda: V.tensor_tensor(UH.t[:, c, 32 + tg * 512:32 + (tg + 1) * 512], bu.t[:], tm.t[:], ALU.mult),
                        [bu.k, tm.k], [UH.k])
            for c in range(2):
                dve(lambda: V.tensor_scalar(ACC.t[:, c, :], UH.t[:, c, 2:2 + NT], VEC.t[:, 34 + c * 31:35 + c * 31],
                                            VEC.t[:, 96 + c:97 + c], ALU.mult, ALU.add), [UH.k, VEC.k], [ACC.k])
                for jj in range(1, 31):
                    dve(lambda: V.scalar_tensor_tensor(ACC.t[:, c, :], UH.t[:, c, 2 + jj:2 + jj + NT],
                                                       VEC.t[:, 34 + c * 31 + jj:35 + c * 31 + jj], ACC.t[:, c, :],
                                                       ALU.mult, ALU.add), [UH.k, VEC.k, ACC.k], [ACC.k])
            dve(lambda: V.tensor_copy(UH.t[:, :, 0:32], UH.t[:, :, NT:NT + 32]), [UH.k], [UH.k])
            for tg in range(ntg):
                ts_ = slice(tg * 512, (tg + 1) * 512)
                bk = P.bank()
                for c in range(2):
                    mm(bk.t[:], ONF, ACC.t[:, c, ts_], c == 0, c == 1, [CON.k, ACC.k], [bk.k], c == 1)
                for c in range(2):
                    dve(lambda: V.scalar_tensor_tensor(DD.t[:, c, :], bk.t[:], -1.0 / 256, ACC.t[:, c, ts_], ALU.mult, ALU.add),
                        [bk.k, ACC.k], [DD.k])
                    act(lambda: A.activation(out=ACC.t[:, c, ts_], in_=DD.t[:, c, :], func=AF.Square), [DD.k], [ACC.k])
                bk2 = P.bank()
                for c in range(2):
                    mm(bk2.t[:], ONF, ACC.t[:, c, ts_], c == 0, c == 1, [CON.k, ACC.k], [bk2.k], c == 1)
                tm = TMP[0]
                act(lambda: A.activation(out=tm.t[:], in_=bk2.t[:], func=AF.Sqrt, bias=LN_EPS, scale=1.0 / 256), [bk2.k], [tm.k])
                dve(lambda: V.reciprocal(tm.t[:], tm.t[:]), [tm.k], [tm.k])
                for c in range(2):
                    dve(lambda: V.tensor_tensor(DD.t[:, c, :], DD.t[:, c, :], tm.t[:], ALU.mult), [DD.k, tm.k], [DD.k])
                    dve(lambda: V.tensor_scalar(DD.t[:, c, :], DD.t[:, c, :], VEC.t[:, 98 + c:99 + c], VEC.t[:, 100 + c:101 + c],
                                                ALU.mult, ALU.add), [DD.k, VEC.k], [DD.k])
                    act(lambda: A.activation(out=UB.t[:, c, ts_], in_=DD.t[:, c, :], func=AF.Silu), [DD.k], [UB.k])
            for tg in range(ntg):
                for c in range(2):
                    bgb = proj(4 + c, tg)
                    act(lambda: A.copy(GBB.t[:, c, tg * 512:(tg + 1) * 512], bgb.t[:]), [bgb.k], [GBB.k])
                    bgc = proj(6 + c, tg)
                    bh = proj(8 + c, tg)
                    tm = TMP[c]
                    act(lambda: A.copy(tm.t[:], bh.t[:]), [bh.k], [tm.k])
                    dve(lambda: V.tensor_tensor(GH.t[:, c, 32 + tg * 512:32 + (tg + 1) * 512], bgc.t[:], tm.t[:], ALU.mult),
                        [bgc.k, tm.k], [GH.k])
            for c in range(2):
                dve(lambda: V.tensor_scalar(ACC.t[:, c, :], GH.t[:, c, 30:30 + NT], VEC.t[:, 102 + c * 3:103 + c * 3], None,
                                            ALU.mult), [GH.k, VEC.k], [ACC.k])
                for jj in range(1, 3):
                    dve(lambda: V.scalar_tensor_tensor(ACC.t[:, c, :], GH.t[:, c, 30 + jj:30 + jj + NT],
                                                       VEC.t[:, 102 + c * 3 + jj:103 + c * 3 + jj], ACC.t[:, c, :],
                                                       ALU.mult, ALU.add), [GH.k, VEC.k, ACC.k], [ACC.k])
                dve(lambda: V.tensor_tensor(UC.t[:, c, :], GBB.t[:, c, :], ACC.t[:, c, :], ALU.mult), [GBB.k, ACC.k], [UC.k])
            dve(lambda: V.tensor_copy(GH.t[:, :, 0:32], GH.t[:, :, NT:NT + 32]), [GH.k], [GH.k])
            P.barrier()
            P.release(ph)

    def phase_GO(l, p, src):
        w = LW[l]
        with ExitStack() as ph:
            MT = sb(ph, "MT", [128, 8, NT], BF16)
            with ExitStack() as ph2:
                WOA = sb(ph2, "WOA", [64, 8, 1024], BF16)
                WOB = sb(ph2, "WOB", [128, 2, 1024], BF16)
                WOC = sb(ph2, "WOC", [128, 2, 1024], BF16)
                P.dma("pool", WOA.t[:], w["woa"].ap(), W=[WOA.k])
                P.dma("pool", WOB.t[:], w["wob"].ap(), W=[WOB.k])
                P.dma("pool", WOC.t[:], w["woc"].ap(), W=[WOC.k])
                WG = [sb(ph2, "WGt%d" % i, [128, 3, 8, 128], BF16) for i in range(2)]
                GS = [sb(ph2, "GS%d" % i, [128, 512], F32) for i in range(3)]
                MA = sb(ph2, "MA", [128, 512], F32)
                MB = sb(ph2, "MB", [128, 512], F32)
                ntg = NT // 512
                for i in range(8):
                    wgt = WG[i % 2]
                    P.dma("pool", wgt.t[:], w["winB"].ap()[10 + 3 * i:13 + 3 * i].rearrange("g p k c -> p g k c"), W=[wgt.k])
                    for tg in range(ntg):
                        ts_ = slice(tg * 512, (tg + 1) * 512)
                        for br in range(3):
                            bk = P.bank()
                            for kc in range(8):
                                mm(bk.t[:], wgt.t[:, br, kc, :], XT.t[:, kc, ts_], kc == 0, kc == 7, [wgt.k, XT.k], [bk.k], kc == 7)
                            act(lambda: A.activation(out=GS[br].t[:], in_=bk.t[:], func=AF.Sigmoid), [bk.k], [GS[br].k])
                        ba = P.bank()
                        for h in range(8):
                            mm(ba.t[:], WOA.t[:, h, i * 128:(i + 1) * 128], OG.t[:, h, ts_], h == 0, h == 7, [WOA.k, OG.k], [ba.k], h == 7)
                        dve(lambda: V.tensor_tensor(MA.t[:], ba.t[:], GS[0].t[:], ALU.mult), [ba.k, GS[0].k], [MA.k])
                        bb_ = P.bank()
                        for c in range(2):
                            mm(bb_.t[:], WOB.t[:, c, i * 128:(i + 1) * 128], UB.t[:, c, ts_], c == 0, c == 1, [WOB.k, UB.k], [bb_.k], c == 1)
                        dve(lambda: V.tensor_tensor(MB.t[:], bb_.t[:], GS[1].t[:], ALU.mult), [bb_.k, GS[1].k], [MB.k])
                        dve(lambda: V.tensor_tensor(MA.t[:], MA.t[:], MB.t[:], ALU.add), [MA.k, MB.k], [MA.k])
                        bc = P.bank()
                        for c in range(2):
                            mm(bc.t[:], WOC.t[:, c, i * 128:(i + 1) * 128], UC.t[:, c, ts_], c == 0, c == 1, [WOC.k, UC.k], [bc.k], c == 1)
                        dve(lambda: V.tensor_tensor(MB.t[:], bc.t[:], GS[2].t[:], ALU.mult), [bc.k, GS[2].k], [MB.k])
                        dve(lambda: V.tensor_tensor(MT.t[:, i, ts_], MA.t[:], MB.t[:], ALU.add), [MA.k, MB.k], [MT.k])
                P.barrier()
                P.release(ph2)
            WOUT = sb(ph, "WOUT", [128, 8, 1024], BF16)
            P.dma("pool", WOUT.t[:], w["wout"].ap(), W=[WOUT.k])
            LNP = sb(ph, "LNP", [128, 2, 1024], F32)
            P.dma("sp", LNP.t[:], w["lnp"].ap()[:, 0:2, :], W=[LNP.k])
            XR = [sb(ph, "XR%d" % i, [128, D], F32) for i in range(2)]
            ZB = [sb(ph, "ZB%d" % i, [128, D], F32) for i in range(2)]
            ST = sb(ph, "ST", [128, 2, 6], F32)
            MV = sb(ph, "MV", [128, 4], F32)
            XTF = sb(ph, "XTF", [128, 8, 128], F32)
            LG = sb(ph, "LG", [128, 8], F32)
            LG2 = sb(ph, "LG2", [128, 8], F32)
            EQ1 = sb(ph, "EQ1", [128, 8], F32)
            EQ2 = sb(ph, "EQ2", [128, 8], F32)
            SM = sb(ph, "SMx", [128, 8], F32)
            for i in range(NT // 128):
                r0 = p * NT + i * 128
                xr = XR[i % 2]
                zb = ZB[i % 2]
                P.dma("sp", xr.t[:], src[r0:r0 + 128, :], W=[xr.k])
                for hf in range(2):
                    bk = P.bank()
                    for kc in range(8):
                        mm(bk.t[:], MT.t[:, kc, i * 128:(i + 1) * 128], WOUT.t[:, kc, hf * 512:(hf + 1) * 512], kc == 0, kc == 7,
                           [MT.k, WOUT.k], [bk.k], kc == 7)
                    dve(lambda: V.scalar_tensor_tensor(zb.t[:, hf * 512:(hf + 1) * 512], xr.t[:, hf * 512:(hf + 1) * 512], ALPHA,
                                                       bk.t[:], ALU.mult, ALU.add), [xr.k, bk.k], [zb.k])
                layer_norm(zb, LNP, 0, ST, MV)
                P.dma("sp", xm.ap()[r0:r0 + 128, :], zb.t[:], R=[zb.k], W=[tok_xm], part=True, own=zb.k)
                for hf in range(2):
                    bk = P.bank()
                    for q in range(4):
                        kc = hf * 4 + q
                        P.op("pe", lambda: T.transpose(bk.t[:, q * 128:(q + 1) * 128], zb.t[:, kc * 128:(kc + 1) * 128], ID),
                             [zb.k, CON.k], [bk.k], inc=(q == 3))
                    iv = bk.t[:].rearrange("p (q t) -> p q t", q=4)
                    if moe[l]:
                        act(lambda: A.copy(XTF.t[:, hf * 4:hf * 4 + 4, :], iv), [bk.k], [XTF.k])
                        dve(lambda: V.tensor_copy(XT.t[:, hf * 4:hf * 4 + 4, i * 128:(i + 1) * 128], XTF.t[:, hf * 4:hf * 4 + 4, :]),
                            [XTF.k], [XT.k])
                    else:
                        act(lambda: A.copy(XT.t[:, hf * 4:hf * 4 + 4, i * 128:(i + 1) * 128], iv), [bk.k], [XT.k])
                if moe[l] and mdbg >= 2:
                    bk = P.bank()
                    for kc in range(8):
                        mm(bk.t[:, 0:8], XTF.t[:, kc, :], ROUT.t[:, kc, :], kc == 0, kc == 7, [XTF.k, ROUT.k], [bk.k], kc == 7)
                    dve(lambda: V.tensor_copy(LG.t[:], bk.t[:, 0:8]), [bk.k], [LG.k])
                if moe[l] and mdbg >= 3:
                    dve(lambda: V.tensor_reduce(SM.t[:, 0:1], LG.t[:], AX.X, ALU.max), [LG.k], [SM.k])
                    dve(lambda: V.tensor_scalar(EQ1.t[:], LG.t[:], SM.t[:, 0:1], None, ALU.is_equal), [LG.k, SM.k], [EQ1.k])
                    dve(lambda: V.scalar_tensor_tensor(LG2.t[:], EQ1.t[:], -1e30, LG.t[:], ALU.mult, ALU.add), [EQ1.k, LG.k], [LG2.k])
                    dve(lambda: V.tensor_reduce(SM.t[:, 1:2], LG2.t[:], AX.X, ALU.max), [LG2.k], [SM.k])
                    dve(lambda: V.tensor_scalar(EQ2.t[:], LG2.t[:], SM.t[:, 1:2], None, ALU.is_equal), [LG2.k, SM.k], [EQ2.k])
                    dve(lambda: V.tensor_tensor(SM.t[:, 2:3], SM.t[:, 1:2], SM.t[:, 0:1], ALU.subtract), [SM.k], [SM.k])
                    act(lambda: A.activation(out=SM.t[:, 3:4], in_=SM.t[:, 2:3], func=AF.Exp), [SM.k], [SM.k])
                    dve(lambda: V.tensor_scalar(SM.t[:, 4:5], SM.t[:, 3:4], 1.0, None, ALU.add), [SM.k], [SM.k])
                    dve(lambda: V.reciprocal(SM.t[:, 5:6], SM.t[:, 4:5]), [SM.k], [SM.k])
                    dve(lambda: V.tensor_tensor(SM.t[:, 6:7], SM.t[:, 3:4], SM.t[:, 5:6], ALU.mult), [SM.k], [SM.k])
                    dve(lambda: V.tensor_scalar(EQ1.t[:], EQ1.t[:], SM.t[:, 5:6], None, ALU.mult), [EQ1.k, SM.k], [EQ1.k])
                    dve(lambda: V.scalar_tensor_tensor(GATE.t[:, i, :], EQ2.t[:], SM.t[:, 6:7], EQ1.t[:], ALU.mult, ALU.add),
                        [EQ2.k, SM.k, EQ1.k], [GATE.k])
            P.barrier()
            P.release(ph)

    def layer_norm(zb, LNP, gi, ST, MV):
        for hf in range(2):
            dve(lambda: V.bn_stats(ST.t[:, hf, :], zb.t[:, hf * 512:(hf + 1) * 512]), [zb.k], [ST.k])
        dve(lambda: V.bn_aggr(MV.t[:, 0:2], ST.t[:].rearrange("p a b -> p (a b)")), [ST.k], [MV.k])
        act(lambda: A.activation(out=MV.t[:, 2:3], in_=MV.t[:, 1:2], func=AF.Sqrt, bias=LN_EPS, scale=1.0), [MV.k], [MV.k])
        dve(lambda: V.reciprocal(MV.t[:, 3:4], MV.t[:, 2:3]), [MV.k], [MV.k])
        dve(lambda: V.tensor_scalar(zb.t[:], zb.t[:], MV.t[:, 0:1], MV.t[:, 3:4], ALU.subtract, ALU.mult), [zb.k, MV.k], [zb.k])
        dve(lambda: V.tensor_tensor(zb.t[:], zb.t[:], LNP.t[:, gi, :], ALU.mult), [zb.k, LNP.k], [zb.k])
        dve(lambda: V.tensor_tensor(zb.t[:], zb.t[:], LNP.t[:, gi + 1, :], ALU.add), [zb.k, LNP.k], [zb.k])

    def phase_F(l, p, dst, tok_dst):
        w = LW[l]
        E = 8 if moe[l] else 1
        with ExitStack() as ph:
            ACC = sb(ph, "fACC", [128, NT // 128, D], F32)
            WGs = [sb(ph, "fWG%d" % i, [128, 8, 256], BF16) for i in range(2)]
            WUs = [sb(ph, "fWU%d" % i, [128, 8, 256], BF16) for i in range(2)]
            WDs = [sb(ph, "fWD%d" % i, [128, 2, 1024], BF16) for i in range(2)]
            HT = [sb(ph, "fHT%d" % i, [128, 2, 512], BF16) for i in range(2)]
            SGT = [sb(ph, "fSG%d" % i, [128, 512], F32) for i in range(2)]
            LNP = sb(ph, "fLNP", [128, 2, 1024], F32)
            P.dma("sp", LNP.t[:], w["lnp"].ap()[:, 2:4, :], W=[LNP.k])
            ST = sb(ph, "fST", [128, 2, 6], F32)
            MV = sb(ph, "fMV", [128, 4], F32)
            XR = [sb(ph, "fXR%d" % i, [128, D], F32) for i in range(2)]
            ntg = NT // 512
            it = 0
            for e in range(E):
                for g in range(NFG):
                    wg, wu, wd = WGs[it % 2], WUs[it % 2], WDs[it % 2]
                    P.dma("pool", wg.t[:], w["wg"].ap()[e, g], W=[wg.k])
                    P.dma("pool", wu.t[:], w["wu"].ap()[e, g], W=[wu.k])
                    P.dma("pool", wd.t[:], w["wd"].ap()[e, g], W=[wd.k])
                    for tg in range(ntg):
                        ts_ = slice(tg * 512, (tg + 1) * 512)
                        ht = HT[tg % 2]
                        for fc in range(2):
                            bg = P.bank()
                            for kc in range(8):
                                mm(bg.t[:], wg.t[:, kc, fc * 128:(fc + 1) * 128], XT.t[:, kc, ts_], kc == 0, kc == 7,
                                   [wg.k, XT.k], [bg.k], kc == 7)
                            bu = P.bank()
                            for kc in range(8):
                                mm(bu.t[:], wu.t[:, kc, fc * 128:(fc + 1) * 128], XT.t[:, kc, ts_], kc == 0, kc == 7,
                                   [wu.k, XT.k], [bu.k], kc == 7)
                            sg = SGT[fc]
                            act(lambda: A.activation(out=sg.t[:], in_=bg.t[:], func=AF.Silu), [bg.k], [sg.k])
                            dve(lambda: V.tensor_tensor(ht.t[:, fc, :], bu.t[:], sg.t[:], ALU.mult), [bu.k, sg.k], [ht.k])
                        for tt_ in range(4):
                            ti = tg * 4 + tt_
                            for hf in range(2):
                                bk = P.bank()
                                for fc in range(2):
                                    mm(bk.t[:], ht.t[:, fc, tt_ * 128:(tt_ + 1) * 128], wd.t[:, fc, hf * 512:(hf + 1) * 512],
                                       fc == 0, fc == 1, [ht.k, wd.k], [bk.k], fc == 1)
                                o = ACC.t[:, ti, hf * 512:(hf + 1) * 512]
                                if moe[l]:
                                    gsc = GATE.t[:, ti, e:e + 1]
                                    if it == 0:
                                        dve(lambda: V.tensor_scalar(o, bk.t[:], gsc, None, ALU.mult), [bk.k, GATE.k], [ACC.k])
                                    else:
                                        dve(lambda: V.scalar_tensor_tensor(o, bk.t[:], gsc, o, ALU.mult, ALU.add),
                                            [bk.k, GATE.k, ACC.k], [ACC.k])
                                else:
                                    if it == 0:
                                        act(lambda: A.copy(o, bk.t[:]), [bk.k], [ACC.k])
                                    else:
                                        dve(lambda: V.tensor_tensor(o, bk.t[:], o, ALU.add), [bk.k, ACC.k], [ACC.k])
                    it += 1
            for i in range(NT // 128):
                r0 = p * NT + i * 128
                xr = XR[i % 2]
                P.dma("sp", xr.t[:], xm.ap()[r0:r0 + 128, :], W=[xr.k])
                dve(lambda: V.scalar_tensor_tensor(xr.t[:], xr.t[:], ALPHA, ACC.t[:, i, :], ALU.mult, ALU.add), [xr.k, ACC.k], [xr.k])
                layer_norm(xr, LNP, 0, ST, MV)
                P.dma("sp", dst[r0:r0 + 128, :], xr.t[:], R=[xr.k], W=[tok_dst], part=True, own=xr.k)
            P.barrier()
            P.release(ph)

    for li, l in enumerate(layers):
        w = LW[l]
        src = x_in.ap() if li == 0 else xl1.ap()
        if li == nlast:
            dst, tok_dst = y_out.ap(), tok_y
        else:
            dst, tok_dst = xl1.ap(), tok_xl1
        P.dma("sp", VEC.t[:], w["vec"].ap(), W=[VEC.k])
        P.dma("sp", V64.t[:], w["vec64"].ap(), W=[V64.k])
        P.dma("pool", W2A2.t[:], w["w2a2"].ap(), W=[W2A2.k])
        P.dma("pool", G2.t[:], w["g2"].ap(), W=[G2.k])
        if moe[l]:
            P.dma("sp", ROUT.t[:], w["router"].ap(), W=[ROUT.k])
        dve(lambda: V.tensor_scalar(OMM.t[:], VEC.t[:, 0:14], -1.0, 1.0, ALU.mult, ALU.add), [VEC.k], [OMM.k])
        dve(lambda: V.tensor_scalar(OMKA.t[:], VEC.t[:, 26:30], -1.0, 1.0, ALU.mult, ALU.add), [VEC.k], [OMKA.k])
        dve(lambda: V.memset(TAILS.t[:], 0.0), [], [TAILS.k])
        dve(lambda: V.memset(UH.t[:, :, 0:32], 0.0), [], [UH.k])
        dve(lambda: V.memset(GH.t[:, :, 0:32], 0.0), [], [GH.k])
        dve(lambda: V.memset(SRING[sci[0] % 3].t[:], 0.0), [], [SRING[sci[0] % 3].k])
        P.barrier()
        for p in range(npass):
            if "X" in phases:
                phase_X(src, p)
            if "R" in phases:
                phase_R(l, p)
            if "B" in phases:
                phase_BC(l, p)
            if "G" in phases:
                phase_GO(l, p, src)
            if "F" in phases:
                phase_F(l, p, dst, tok_dst)
    P.barrier()
    es.close()
    return nc


def make_consts():
    c = np.zeros((128, K_END), np.float32)
    c[:, K_ID:K_ID + 128] = np.eye(128, dtype=np.float32)
    c[0:64, K_I2:K_I2 + 64] = np.eye(64, dtype=np.float32)
    c[64:128, K_I2:K_I2 + 64] = np.eye(64, dtype=np.float32)
    s = np.arange(64)[:, None]
    n = np.arange(64)[None, :]
    c[0:64, K_MKA:K_MKA + 64] = (s < n)
    c[0:64, K_MKA + 64:K_MKA + 128] = (s <= n)
    c[0:64, K_ML:K_ML + 64] = (n < s)
    c[0:64, K_OB:K_OB + 64] = 1.0
    c[64:128, K_OB + 64:K_OB + 128] = 1.0
    c[:, K_OF:K_OF + 128] = 1.0
    sm = np.ones(512, np.float32)
    sm[::64] = 0.0
    c[:, K_SM:K_SM + 512] = sm[None, :]
    return c


def prep_layer(inp, l, is_moe, j):
    f = np.float32
    out = {}
    w_in = np.asarray(inp["w_in"][l], f)
    W = w_in.reshape(8, 128, 48, 128).transpose(2, 1, 0, 3)
    out["winA%d" % l] = np.ascontiguousarray(W[0:14])
    order = list(range(14, 24)) + [24 + br * 8 + i for i in range(8) for br in range(3)]
    out["winB%d" % l] = np.ascontiguousarray(W[order])
    vec = np.zeros((128, NVEC), f)
    vec[:, 0:14] = np.asarray(inp["rwkv_mu"][l], f).reshape(14, 128).T
    for c0, name in ((14, "rwkv_w0"), (18, "rwkv_a0"), (22, "rwkv_k_k"), (26, "rwkv_k_a"), (30, "rwkv_r_k")):
        vec[:, c0:c0 + 4] = np.asarray(inp[name][l], f).reshape(4, 128).T
    cdw = np.asarray(inp["conf_dw"][l], f)
    for c in range(2):
        vec[:, 34 + c * 31:34 + (c + 1) * 31] = cdw[:, c * 128:(c + 1) * 128].T
    vec[:, 96:98] = np.asarray(inp["conf_dw_b"][l], f).reshape(2, 128).T
    vec[:, 98:100] = np.asarray(inp["conf_ln_g"][l], f).reshape(2, 128).T
    vec[:, 100:102] = np.asarray(inp["conf_ln_b"][l], f).reshape(2, 128).T
    sdw = np.asarray(inp["short_dw"][l], f)
    for c in range(2):
        vec[:, 102 + c * 3:102 + (c + 1) * 3] = sdw[:, c * 128:(c + 1) * 128].T
    out["vec%d" % l] = vec
    v64 = np.zeros((64, 16), f)
    v64[:, 0:8] = np.asarray(inp["rwkv_ln_g"][l], f).reshape(8, 64).T
    v64[:, 8:16] = np.asarray(inp["rwkv_ln_b"][l], f).reshape(8, 64).T
    out["vec64_%d" % l] = v64
    out["w2a2_%d" % l] = np.ascontiguousarray(np.concatenate([np.asarray(inp["rwkv_w2"][l], f), np.asarray(inp["rwkv_a2"][l], f)], 0))
    out["g2_%d" % l] = np.ascontiguousarray(np.asarray(inp["rwkv_g2"][l], f))
    out["woa%d" % l] = np.ascontiguousarray(np.asarray(inp["rwkv_w_o"][l], f).reshape(8, 64, 1024).transpose(1, 0, 2))
    out["wob%d" % l] = np.ascontiguousarray(np.asarray(inp["conf_w_o"][l], f).reshape(2, 128, 1024).transpose(1, 0, 2))
    out["woc%d" % l] = np.ascontiguousarray(np.asarray(inp["short_w_o"][l], f).reshape(2, 128, 1024).transpose(1, 0, 2))
    out["wout%d" % l] = np.ascontiguousarray(np.asarray(inp["w_out"][l], f).reshape(8, 128, 1024).transpose(1, 0, 2))
    lnp = np.stack([np.asarray(inp[n][l], f) for n in ("ln1_g", "ln1_b", "ln2_g", "ln2_b")], 0)
    out["lnp%d" % l] = np.ascontiguousarray(np.broadcast_to(lnp[None], (128, 4, 1024)))
    if is_moe:
        wg = np.asarray(inp["moe_w_gate"][j], f)
        wu = np.asarray(inp["moe_w_up"][j], f)
        wd = np.asarray(inp["moe_w_down"][j], f)
        out["router%d" % l] = np.ascontiguousarray(np.asarray(inp["moe_router"][j], f).reshape(8, 128, 8).transpose(1, 0, 2))
    else:
        wg = np.asarray(inp["ffn_w_gate"][j], f)[None]
        wu = np.asarray(inp["ffn_w_up"][j], f)[None]
        wd = np.asarray(inp["ffn_w_down"][j], f)[None]
    E = wg.shape[0]
    out["wg%d" % l] = np.ascontiguousarray(wg.reshape(E, 8, 128, NFG, 256).transpose(0, 3, 2, 1, 4))
    out["wu%d" % l] = np.ascontiguousarray(wu.reshape(E, 8, 128, NFG, 256).transpose(0, 3, 2, 1, 4))
    out["wd%d" % l] = np.ascontiguousarray(wd.reshape(E, NFG, 2, 128, 1024).transpose(0, 1, 3, 2, 4))
    return out


_NC_CACHE = {}


def kernel(**inputs):
    x = np.asarray(inputs["x"], np.float32)
    B, S, _ = x.shape
    if S not in _NC_CACHE:
        _NC_CACHE[S] = build(S)
    nc = _NC_CACHE[S]
    shared = {"consts": make_consts()}
    for l in range(2):
        shared.update(prep_layer(inputs, l, l % 2 == 1, l // 2))
    maps = []
    for b in range(B):
        m = dict(shared)
        m["x"] = np.ascontiguousarray(x[b])
        maps.append(m)
    in_maps = [maps[c % B] for c in range(8)]
    res = run_bass_kernel_spmd(nc, in_maps, core_ids=list(range(8)))
    return np.stack([np.asarray(res.results[b]["y"], np.float32) for b in range(B)], 0)
```

```python
import numpy as np
from contextlib import ExitStack
import concourse.bass as bass
import concourse.mybir as mybir
from concourse.bass_utils import run_bass_kernel_spmd

F32 = mybir.dt.float32
BF16 = mybir.dt.bfloat16
AF = mybir.ActivationFunctionType
ALU = mybir.AluOpType
AX = mybir.AxisListType

D = 1024
TG = 128
CH = 64
C0 = float(np.exp(-0.5))
ALPHA = float(4.0 ** 0.25)
LN_EPS = 1e-5
GN_EPS = 64e-5
NF = 2816
NFG = 11
K_ID, K_I2, K_MKA, K_ML, K_OB, K_OF, K_SM, K_END = 0, 128, 192, 320, 384, 512, 640, 1152
NVEC = 108


class Tok:
    __slots__ = ("w", "r", "sem", "cnt", "name")

    def __init__(self, name="t"):
        self.name = name
        self.w = {}
        self.r = {}
        self.sem = None
        self.cnt = 0


class DSem:
    __slots__ = ("h", "cnt", "q")

    def __init__(self, h, q):
        self.h = h
        self.cnt = 0
        self.q = q


class Buf:
    def __init__(self, t, name="b"):
        self.t = t
        self.k = Tok(name)


class Prog:
    def __init__(self, nc, es):
        self.nc = nc
        self.es = es
        self.eng = {"pe": nc.tensor, "act": nc.scalar, "dve": nc.vector, "pool": nc.gpsimd, "sp": nc.sync}
        self.sem = {}
        self.cnt = {}
        self.known = {e: {} for e in self.eng}
        for e in ("pe", "act", "dve", "pool"):
            self.sem[e] = es.enter_context(nc.semaphore("sem_" + e))
            self.cnt[e] = 0
        self.nsem = 0
        self.dsems = []
        self.free_dsems = {"sp": [], "pool": []}
        self.banks = []
        self.bi = 0
        self.bbanks = []
        self.bbi = 0
        self.dummy = None
        self.ninst = 0

    def _wait(self, e, deps):
        kn = self.known[e]
        need = {}
        for (sem, val, owner) in deps:
            if owner is not None:
                if owner == e and e == "pe":
                    continue
                assert val <= self.cnt[owner], "uncovered dependency"
            key = id(sem)
            if kn.get(key, 0) >= val:
                continue
            if key not in need or need[key][1] < val:
                need[key] = (sem, val)
        for key, (sem, val) in need.items():
            self.eng[e].wait_ge(sem, val)
            kn[key] = val
            self.ninst += 1

    @staticmethod
    def _put(d, rec):
        key = id(rec[0])
        if key not in d or d[key][1] < rec[1]:
            d[key] = rec

    def op(self, e, fn, R=(), W=(), inc=True):
        deps = []
        for t in R:
            deps += list(t.w.values())
        for t in W:
            deps += list(t.w.values())
            deps += list(t.r.values())
        self._wait(e, deps)
        ins = fn()
        self.ninst += 1
        if inc:
            self.cnt[e] += 1
            ins.then_inc(self.sem[e], 1)
            rec = (self.sem[e], self.cnt[e], e)
        else:
            rec = (self.sem[e], self.cnt[e] + 1, e)
        for t in R:
            self._put(t.r, rec)
        for t in W:
            t.w = {id(rec[0]): rec}
            t.r = {}
        return ins

    def dma(self, q, out, in_, R=(), W=(), part=False, own=None):
        deps = []
        for t in R:
            deps += list(t.w.values())
        for t in W:
            if not part:
                deps += list(t.w.values())
            deps += list(t.r.values())
        self._wait(q, deps)
        ins = self.eng[q].dma_start(out=out, in_=in_)
        self.ninst += 1
        t0 = own if own is not None else W[0]
        if t0.sem is None:
            if self.free_dsems[q]:
                t0.sem = self.free_dsems[q].pop()
            else:
                t0.sem = DSem(self.es.enter_context(self.nc.semaphore("d%d" % self.nsem)), q)
                self.nsem += 1
                self.dsems.append(t0.sem)
        assert t0.sem.q == q, "token DMA'd from two queue kinds"
        t0.sem.cnt += 16
        ins.then_inc(t0.sem.h, 16)
        rec = (t0.sem.h, t0.sem.cnt, None)
        for t in R:
            self._put(t.r, rec)
        for t in W:
            if part:
                self._put(t.w, rec)
            else:
                t.w = {id(rec[0]): rec}
                t.r = {}
        return ins

    def barrier(self):
        f = "pool"
        deps = [(self.sem[e], self.cnt[e], e) for e in ("pe", "act", "dve")]
        deps += [(d.h, d.cnt, None) for d in self.dsems]
        self._wait(f, deps)
        if self.cnt[f] > 0:
            self.eng[f].wait_ge(self.sem[f], self.cnt[f])
        ins = self.nc.gpsimd.memset(self.dummy.t[0:1, 0:1], 0.0)
        self.cnt[f] += 1
        ins.then_inc(self.sem[f], 1)
        for e in ("pe", "act", "dve", "sp"):
            self.eng[e].wait_ge(self.sem[f], self.cnt[f])
        for e in self.eng:
            kn = self.known[e]
            for c in ("pe", "act", "dve", "pool"):
                kn[id(self.sem[c])] = self.cnt[c]
            for d in self.dsems:
                kn[id(d.h)] = d.cnt

    def release(self, st):
        for b in getattr(st, "_bufs", []):
            if b.k.sem is not None:
                self.free_dsems[b.k.sem.q].append(b.k.sem)
                b.k.sem = None

    def bank(self):
        b = self.banks[self.bi % len(self.banks)]
        self.bi += 1
        return b

    def bbank(self):
        b = self.bbanks[self.bbi % len(self.bbanks)]
        self.bbi += 1
        return b


def build(S, layers=(0, 1), moe=(False, True), NT=1024, debug_xm=False, phases="XRBGF", rstop=0, mdbg=9):
    nc = bass.Bass("TRN2", target_bir_lowering=False)
    es = ExitStack()
    P = Prog(nc, es)
    V, A, T = nc.vector, nc.scalar, nc.tensor
    npass = S // NT
    assert S % NT == 0 and NT % 512 == 0
    nlast = len(layers) - 1

    def din(name, shape):
        return nc.dram_tensor(name, list(shape), F32, kind="ExternalInput")

    x_in = din("x", [S, D])
    y_out = nc.dram_tensor("y", [S, D], F32, kind="ExternalOutput")
    xl1 = nc.dram_tensor("xl1", [S, D], F32)
    if debug_xm:
        xm = nc.dram_tensor("xm", [S, D], F32, kind="ExternalOutput")
    else:
        xm = nc.dram_tensor("xm", [S, D], F32)
    consts_d = din("consts", [128, K_END])
    LW = {}
    for l in layers:
        E = 8 if moe[l] else 1
        LW[l] = dict(
            winA=din("winA%d" % l, [14, 128, 8, 128]), winB=din("winB%d" % l, [34, 128, 8, 128]),
            vec=din("vec%d" % l, [128, NVEC]), vec64=din("vec64_%d" % l, [64, 16]),
            w2a2=din("w2a2_%d" % l, [128, 512]), g2=din("g2_%d" % l, [128, 512]),
            woa=din("woa%d" % l, [64, 8, 1024]), wob=din("wob%d" % l, [128, 2, 1024]),
            woc=din("woc%d" % l, [128, 2, 1024]), wout=din("wout%d" % l, [128, 8, 1024]),
            lnp=din("lnp%d" % l, [128, 4, 1024]),
            wg=din("wg%d" % l, [E, NFG, 128, 8, 256]), wu=din("wu%d" % l, [E, NFG, 128, 8, 256]),
            wd=din("wd%d" % l, [E, NFG, 128, 2, 1024]))
        if moe[l]:
            LW[l]["router"] = din("router%d" % l, [128, 8, 8])
    tok_y = Tok("y")
    tok_xl1 = Tok("xl1")
    tok_xm = Tok("xm")

    uid = [0]

    def sb(st, name, shape, dt):
        uid[0] += 1
        b = Buf(st.enter_context(nc.sbuf_tensor("%s_u%d" % (name, uid[0]), list(shape), dt)), name)
        if not hasattr(st, "_bufs"):
            st._bufs = []
        st._bufs.append(b)
        return b

    def dve(fn, R, W):
        return P.op("dve", fn, R, W)

    def act(fn, R, W):
        return P.op("act", fn, R, W)

    def mm(out, lhsT, rhs, start, stop, R, W, inc):
        return P.op("pe", lambda: T.matmul(out, lhsT, rhs, start=start, stop=stop), R, W, inc)

    for i in range(6):
        P.banks.append(Buf(es.enter_context(nc.psum_tensor("pb%d" % i, [128, 512], F32))))
    for i in range(2):
        P.bbanks.append(Buf(es.enter_context(nc.psum_tensor("pbb%d" % i, [128, 1024], BF16))))
    P.dummy = sb(es, "dummy", [128, 4], F32)
    CON = sb(es, "CON", [128, K_END], F32)
    IDB = sb(es, "IDB", [128, 128], BF16)
    XT = sb(es, "XT", [128, 8, NT], BF16)
    OG = sb(es, "OG", [64, 8, NT], BF16)
    UB = sb(es, "UB", [128, 2, NT], BF16)
    UC = sb(es, "UC", [128, 2, NT], BF16)
    UH = sb(es, "UH", [128, 2, 32 + NT], F32)
    GH = sb(es, "GH", [128, 2, 32 + NT], F32)
    VEC = sb(es, "VEC", [128, NVEC], F32)
    OMM = sb(es, "OMM", [128, 14], F32)
    OMKA = sb(es, "OMKA", [128, 4], F32)
    V64 = sb(es, "V64", [64, 16], F32)
    W2A2 = sb(es, "W2A2", [128, 512], BF16)
    G2 = sb(es, "G2", [128, 512], BF16)
    TAILS = sb(es, "TAILS", [128, 14], F32)
    SRING = [sb(es, "SR%d" % i, [64, 8, 64], F32) for i in range(3)]
    GATE = sb(es, "GATE", [128, NT // 128, 8], F32)
    ROUT = sb(es, "ROUT", [128, 8, 8], F32)
    sci = [0]

    P.dma("sp", CON.t[:], consts_d.ap(), W=[CON.k])
    dve(lambda: V.tensor_copy(IDB.t[:], CON.t[:, K_ID:K_ID + 128]), [CON.k], [IDB.k])
    ID = CON.t[:, K_ID:K_ID + 128]

    def cview(lo, hi, rows=128):
        return CON.t[0:rows, lo:hi]

    def phase_X(src, p):
        with ExitStack() as ph:
            xs = [sb(ph, "xs%d" % i, [128, D], F32) for i in range(2)]
            for i in range(NT // 128):
                xb = xs[i % 2]
                r0 = p * NT + i * 128
                P.dma("sp", xb.t[:], src[r0:r0 + 128, :], W=[xb.k])
                for hf in range(2):
                    bk = P.bank()
                    for q in range(4):
                        kc = hf * 4 + q
                        P.op("pe", lambda: T.transpose(bk.t[:, q * 128:(q + 1) * 128], xb.t[:, kc * 128:(kc + 1) * 128], ID),
                             [xb.k, CON.k], [bk.k], inc=(q == 3))
                    o = XT.t[:, hf * 4:hf * 4 + 4, i * 128:(i + 1) * 128]
                    iv = bk.t[:].rearrange("p (q t) -> p q t", q=4)
                    if hf == 0:
                        act(lambda: A.copy(o, iv), [bk.k], [XT.k])
                    else:
                        dve(lambda: V.tensor_copy(o, iv), [bk.k], [XT.k])
            P.barrier()
            P.release(ph)

    def phase_R(l, p):
        w = LW[l]
        with ExitStack() as ph:
            WA = sb(ph, "WA", [128, 14, 8, 128], BF16)
            for g0 in range(0, 14, 7):
                P.dma("pool", WA.t[:, g0:g0 + 7], w["winA"].ap()[g0:g0 + 7].rearrange("g p k c -> p g k c"),
                      W=[WA.k], part=True)
            sl = {n: sb(ph, "R_" + n, [128, 4, TG], F32) for n in ("r", "k", "v", "A", "S", "T1", "C", "T2", "BV")}
            r_, k_, v_, A_, S_, T1, C_, T2, BV = (sl[n] for n in ("r", "k", "v", "A", "S", "T1", "C", "T2", "BV"))
            DG = sb(ph, "DG", [128, 4, 2, 64], F32)
            GC = sb(ph, "GC", [128, 4, 2], F32)
            PWPA = sb(ph, "PWPA", [128, TG], F32)
            PG = sb(ph, "PG", [128, TG], F32)
            MX = [sb(ph, "MX%d" % i, [128, TG], F32) for i in range(2)]
            TP = sb(ph, "TP", [128, TG], BF16)
            SGB = sb(ph, "SGB", [128, TG], BF16)
            AR = sb(ph, "AR", [128, 4, 2, 2, 64], BF16)
            BK = sb(ph, "BK", [128, 4, 2, 2, 64], BF16)
            VB = sb(ph, "VB", [128, 4, TG], BF16)
            BH = sb(ph, "BH", [128, 4, TG], BF16)
            KH = sb(ph, "KH", [128, 4, TG], BF16)
            VTM = sb(ph, "VTM", [64, 2, 512], BF16)
            BHTM = sb(ph, "BHTM", [64, 2, 512], BF16)
            KHTM = sb(ph, "KHTM", [64, 2, 512], BF16)
            AVA = sb(ph, "AVA", [64, 2, 8, 128], BF16)
            ATA = [sb(ph, "ATA%d" % i, [64, 8, 128], BF16) for i in range(2)]
            ATB = [sb(ph, "ATB%d" % i, [64, 8, 128], BF16) for i in range(2)]
            XXc = [[sb(ph, "XX%d_%d" % (c, i), [64, 8, 64], BF16) for i in range(2)] for c in range(2)]
            YYc = [[sb(ph, "YY%d_%d" % (c, i), [64, 8, 64], BF16) for i in range(2)] for c in range(2)]
            ZZc = [sb(ph, "ZZ%d" % c, [64, 8, 64], BF16) for c in range(2)]
            UW = [sb(ph, "UW%d" % i, [64, 8, 128], BF16) for i in range(2)]
            GT = [sb(ph, "GT%d" % i, [64, 8, 64], F32) for i in range(2)]
            HH = [sb(ph, "HH%d" % i, [64, 8, 64], F32) for i in range(2)]
            OLOC = sb(ph, "OLOC", [64, 8, TG], F32)
            QT = sb(ph, "QT", [64, 8, TG], F32)
            BV64 = sb(ph, "BV64", [64, 8, TG], F32)
            G64 = sb(ph, "G64", [64, 8, TG], F32)
            DN = sb(ph, "DN", [64, 8, TG], F32)
            SQ = sb(ph, "SQ", [64, 8, TG], F32)

            def vb(c0, n=TG):
                return VEC.t[:, c0:c0 + 4].unsqueeze(2).broadcast_to([128, 4, n])

            def cv(b):
                return b.t[:].rearrange("p j (c t) -> p j c t", t=64)

            def fl(b):
                return b.t[:].rearrange("p j t -> p (j t)")

            MKA = cview(K_MKA, K_MKA + 128, 64)
            ML = cview(K_ML, K_ML + 64, 64)
            ONB = cview(K_OB, K_OB + 128)
            ON64 = CON.t[0:64, K_OB:K_OB + 64]
            I2 = cview(K_I2, K_I2 + 64)
            SMK = cview(K_SM, K_SM + 512)

            for tg in range(NT // TG):
                c0 = tg * TG
                for g in range(14):
                    bk = P.bank()
                    for kc in range(8):
                        mm(bk.t[:, 0:TG], WA.t[:, g, kc, :], XT.t[:, kc, c0:c0 + TG], kc == 0, kc == 7,
                           [WA.k, XT.k], [bk.k], kc == 7)
                    if g < 4:
                        dst, dk = r_.t[:, g, :], r_.k
                    elif g < 8:
                        dst, dk = k_.t[:, g - 4, :], k_.k
                    elif g < 12:
                        dst, dk = v_.t[:, g - 8, :], v_.k
                    elif g == 12:
                        dst, dk = PWPA.t[:], PWPA.k
                    else:
                        dst, dk = PG.t[:], PG.k
                    m_ = MX[g % 2]
                    act(lambda: A.activation(out=m_.t[:], in_=bk.t[:, 0:TG], func=AF.Copy, scale=OMM.t[:, g:g + 1]),
                        [bk.k, OMM.k], [m_.k])
                    dve(lambda: V.scalar_tensor_tensor(dst[:, 1:TG], bk.t[:, 0:TG - 1], VEC.t[:, g:g + 1],
                                                       m_.t[:, 1:TG], ALU.mult, ALU.add),
                        [bk.k, VEC.k, m_.k], [dk])
                    dve(lambda: V.scalar_tensor_tensor(dst[:, 0:1], TAILS.t[:, g:g + 1], VEC.t[:, g:g + 1],
                                                       m_.t[:, 0:1], ALU.mult, ALU.add),
                        [TAILS.k, VEC.k, m_.k], [dk])
                    dve(lambda: V.tensor_copy(TAILS.t[:, g:g + 1], bk.t[:, TG - 1:TG]), [bk.k], [TAILS.k])
                if rstop == 1:
                    P.barrier()
                    P.release(ph)
                    return
                act(lambda: A.activation(out=TP.t[0:64, :], in_=PWPA.t[0:64, :], func=AF.Tanh), [PWPA.k], [TP.k])
                dve(lambda: V.tensor_copy(TP.t[64:128, :], PWPA.t[64:128, :]), [PWPA.k], [TP.k])
                act(lambda: A.activation(out=SGB.t[:], in_=PG.t[:], func=AF.Sigmoid), [PG.k], [SGB.k])
                for j in range(4):
                    bk = P.bank()
                    mm(bk.t[:, 0:TG], W2A2.t[0:64, j * 128:(j + 1) * 128], TP.t[0:64, :], True, True,
                       [W2A2.k, TP.k], [bk.k], True)
                    act(lambda: A.activation(out=S_.t[:, j, :], in_=bk.t[:, 0:TG], func=AF.Sigmoid,
                                             bias=VEC.t[:, 14 + j:15 + j]), [bk.k, VEC.k], [S_.k])
                    bk2 = P.bank()
                    mm(bk2.t[:, 0:TG], W2A2.t[64:128, j * 128:(j + 1) * 128], TP.t[64:128, :], True, True,
                       [W2A2.k, TP.k], [bk2.k], True)
                    act(lambda: A.activation(out=A_.t[:, j, :], in_=bk2.t[:, 0:TG], func=AF.Sigmoid,
                                             bias=VEC.t[:, 18 + j:19 + j]), [bk2.k, VEC.k], [A_.k])
                if rstop == 2:
                    P.barrier()
                    P.release(ph)
                    return
                dve(lambda: V.tensor_tensor(T1.t[:], k_.t[:], vb(22), ALU.mult), [k_.k, VEC.k], [T1.k])
                act(lambda: A.activation(out=T2.t[:], in_=T1.t[:], func=AF.Square), [T1.k], [T2.k])
                bk = P.bank()
                mm(bk.t[:, 0:512], ONB, fl(T2), True, True, [CON.k, T2.k], [bk.k], True)
                act(lambda: A.activation(out=fl(T2), in_=bk.t[:, 0:512], func=AF.Ln, bias=1e-24, scale=1.0),
                    [bk.k], [T2.k])
                act(lambda: A.activation(out=T2.t[:], in_=T2.t[:], func=AF.Exp, scale=-0.5), [T2.k], [T2.k])
                dve(lambda: V.tensor_tensor(T1.t[:], T1.t[:], T2.t[:], ALU.mult), [T1.k, T2.k], [T1.k])
                if rstop == 3:
                    P.barrier()
                    P.release(ph)
                    return
                dve(lambda: V.tensor_tensor_scan(fl(C_), SMK, fl(S_), 0.0, ALU.mult, ALU.add), [CON.k, S_.k], [C_.k])
                act(lambda: A.activation(out=T2.t[:], in_=C_.t[:], func=AF.Exp, scale=-C0), [C_.k], [T2.k])
                dve(lambda: V.tensor_tensor(AR.t[:, :, :, 1, :], cv(r_), cv(T2), ALU.mult), [r_.k, T2.k], [AR.k])
                dve(lambda: V.tensor_tensor(T2.t[:], C_.t[:], S_.t[:], ALU.subtract), [C_.k, S_.k], [T2.k])
                act(lambda: A.activation(out=T2.t[:], in_=T2.t[:], func=AF.Exp, scale=-C0), [T2.k], [T2.k])
                dve(lambda: V.scalar_tensor_tensor(AR.t[:, :, :, 0, :], cv(T1), -1.0, cv(T2), ALU.mult, ALU.mult),
                    [T1.k, T2.k], [AR.k])
                act(lambda: A.activation(out=T2.t[:], in_=C_.t[:], func=AF.Exp, scale=C0), [C_.k], [T2.k])
                dve(lambda: V.tensor_tensor(S_.t[:], T1.t[:], A_.t[:], ALU.mult), [T1.k, A_.k], [S_.k])
                dve(lambda: V.tensor_tensor(BK.t[:, :, :, 0, :], cv(S_), cv(T2), ALU.mult), [S_.k, T2.k], [BK.k])
                for j in range(4):
                    dve(lambda: V.tensor_scalar(A_.t[:, j, :], A_.t[:, j, :], VEC.t[:, 26 + j:27 + j],
                                                OMKA.t[:, j:j + 1], ALU.mult, ALU.add), [A_.k, VEC.k, OMKA.k], [A_.k])
                dve(lambda: V.tensor_tensor(k_.t[:], k_.t[:], A_.t[:], ALU.mult), [k_.k, A_.k], [k_.k])
                dve(lambda: V.tensor_tensor(BK.t[:, :, :, 1, :], cv(k_), cv(T2), ALU.mult), [k_.k, T2.k], [BK.k])
                clast = cv(C_)[:, :, :, 63:64]
                dve(lambda: V.tensor_tensor(cv(T2), clast.broadcast_to([128, 4, 2, 64]), cv(C_), ALU.subtract),
                    [C_.k], [T2.k])
                act(lambda: A.activation(out=T2.t[:], in_=T2.t[:], func=AF.Exp, scale=-C0), [T2.k], [T2.k])
                dve(lambda: V.tensor_tensor(BH.t[:], S_.t[:], T2.t[:], ALU.mult), [S_.k, T2.k], [BH.k])
                dve(lambda: V.tensor_tensor(KH.t[:], k_.t[:], T2.t[:], ALU.mult), [k_.k, T2.k], [KH.k])
                act(lambda: A.activation(out=GC.t[:], in_=cv(C_)[:, :, :, 63], func=AF.Exp, scale=-C0), [C_.k], [GC.k])
                dve(lambda: V.tensor_tensor(DG.t[:], I2.unsqueeze(1).unsqueeze(1).broadcast_to([128, 4, 2, 64]),
                                            GC.t[:].unsqueeze(3).broadcast_to([128, 4, 2, 64]), ALU.mult),
                    [CON.k, GC.k], [DG.k])
                if rstop == 4:
                    P.barrier()
                    P.release(ph)
                    return
                dve(lambda: V.tensor_tensor(T2.t[:], r_.t[:], k_.t[:], ALU.mult), [r_.k, k_.k], [T2.k])
                dve(lambda: V.tensor_tensor(T2.t[:], T2.t[:], vb(30), ALU.mult), [T2.k, VEC.k], [T2.k])
                bk = P.bank()
                mm(bk.t[:, 0:512], ONB, fl(T2), True, True, [CON.k, T2.k], [bk.k], True)
                dve(lambda: V.tensor_tensor(fl(BV), bk.t[:, 0:512], fl(v_), ALU.mult), [bk.k, v_.k], [BV.k])
                act(lambda: A.copy(VB.t[:], v_.t[:]), [v_.k], [VB.k])
                for (srcb, dstb, kind) in ((BV, BV64, 0), (SGB, G64, 1)):
                    bks = [P.bank(), P.bank()]
                    for h in range(8):
                        j, hp = h // 2, h % 2
                        rows = slice(64 * hp, 64 * hp + 64)
                        bkx = bks[h // 4]
                        o = bkx.t[0:64, (h % 4) * 128:(h % 4 + 1) * 128]
                        if kind == 0:
                            mm(o, CON.t[:, K_ID + 64 * hp:K_ID + 64 * hp + 64], BV.t[:, j, :], True, True,
                               [CON.k, BV.k], [bkx.k], h % 4 == 3)
                        else:
                            mm(o, G2.t[:, h * 64:(h + 1) * 64], SGB.t[:], True, True, [G2.k, SGB.k], [bkx.k], h % 4 == 3)
                    for q in range(2):
                        o = dstb.t[:, q * 4:q * 4 + 4, :]
                        iv = bks[q].t[0:64, :].rearrange("p (h t) -> p h t", h=4)
                        if q == 0:
                            act(lambda: A.copy(o, iv), [bks[q].k], [dstb.k])
                        else:
                            dve(lambda: V.tensor_copy(o, iv), [bks[q].k], [dstb.k])
                if rstop == 5:
                    P.barrier()
                    P.release(ph)
                    return
                for c in range(2):
                    for si, (srcb, dstb) in enumerate(((VB, VTM), (BH, BHTM), (KH, KHTM), (AR, AVA))):
                        bb = P.bbank()
                        for j in range(4):
                            if srcb is AR:
                                iv = AR.t[:, j, c, 0, :]
                            else:
                                iv = srcb.t[:, j, c * 64:(c + 1) * 64]
                            P.op("pe", lambda: T.transpose(bb.t[0:64, j * 128:(j + 1) * 128], iv, IDB.t[:]),
                                 [srcb.k, IDB.k], [bb.k], inc=(j == 3))
                        if srcb is AR:
                            o = AVA.t[:, c, :, 64:128]
                            iv2 = bb.t[0:64, 0:512].rearrange("p (h t) -> p h t", h=8)
                        else:
                            o = dstb.t[:, c, :]
                            iv2 = bb.t[0:64, 0:512]
                        if si % 2 == 0:
                            act(lambda: A.copy(o, iv2), [bb.k], [dstb.k])
                        else:
                            dve(lambda: V.tensor_copy(o, iv2), [bb.k], [dstb.k])
                if rstop == 6:
                    P.barrier()
                    P.release(ph)
                    return
                CS = (0, 1)
                for c in CS:
                    ata, atb = ATA[c], ATB[c]
                    for which, dstb in ((0, ata), (1, atb)):
                        bks = [P.bank(), P.bank()]
                        for h in range(8):
                            j, hp = h // 2, h % 2
                            rows = slice(64 * hp, 64 * hp + 64)
                            bkx = bks[hp]
                            mm(bkx.t[0:64, j * 128:(j + 1) * 128], BK.t[rows, j, c, which, :],
                               AR.t[rows, j, c, :, :], True, True, [BK.k, AR.k], [bkx.k], h >= 6)
                        for q in range(2):
                            iv = bks[q].t[0:64, :].rearrange("p (h t) -> p h t", h=4)
                            ov = dstb.t[:].rearrange("p (j hp) n -> p hp j n", hp=2)[:, q]
                            dve(lambda: V.tensor_tensor(ov, iv, MKA.unsqueeze(1).broadcast_to([64, 4, 128]), ALU.mult),
                                [bks[q].k, CON.k], [dstb.k])
                            if which == 0:
                                ox = XXc[c][0].t[:].rearrange("p (j hp) n -> p hp j n", hp=2)[:, q]
                                dve(lambda: V.tensor_tensor(ox, iv[:, :, 0:64],
                                                            MKA[:, 0:64].unsqueeze(1).broadcast_to([64, 4, 64]), ALU.mult),
                                    [bks[q].k, CON.k], [XXc[c][0].k])
                    bks = [P.bank(), P.bank()]
                    for h in range(8):
                        j, hp = h // 2, h % 2
                        rows = slice(64 * hp, 64 * hp + 64)
                        mm(bks[hp].t[0:64, j * 64:(j + 1) * 64], AR.t[rows, j, c, 0, :], BK.t[rows, j, c, 0, :], True, True,
                           [AR.k, BK.k], [bks[hp].k], h >= 6)
                    for q in range(2):
                        oy = YYc[c][0].t[:].rearrange("p (j hp) n -> p hp j n", hp=2)[:, q]
                        dve(lambda: V.tensor_tensor(oy, bks[q].t[0:64, 0:256].rearrange("p (h t) -> p h t", h=4),
                                                    ML.unsqueeze(1).broadcast_to([64, 4, 64]), ALU.mult),
                            [bks[q].k, CON.k], [YYc[c][0].k])
                    dve(lambda: V.tensor_tensor(ZZc[c].t[:], XXc[c][0].t[:],
                                                CON.t[0:64, K_ID:K_ID + 64].unsqueeze(1).broadcast_to([64, 8, 64]), ALU.add),
                        [XXc[c][0].k, CON.k], [ZZc[c].k])
                for lvl in range(5):
                    pxs, pys = {}, {}
                    for c in CS:
                        xc, yc = XXc[c][lvl % 2], YYc[c][lvl % 2]
                        if lvl < 4:
                            px = P.bank()
                            for h in range(8):
                                mm(px.t[0:64, h * 64:(h + 1) * 64], yc.t[:, h, :], xc.t[:, h, :], True, True,
                                   [yc.k, xc.k], [px.k], h == 7)
                            pxs[c] = px
                        py = P.bank()
                        for h in range(8):
                            mm(py.t[0:64, h * 64:(h + 1) * 64], xc.t[:, h, :], yc.t[:, h, :], True, True,
                               [yc.k, xc.k], [py.k], h == 7)
                        pys[c] = py
                    for c in CS:
                        xn, yn = XXc[c][(lvl + 1) % 2], YYc[c][(lvl + 1) % 2]
                        if lvl < 4:
                            px = pxs[c]
                            act(lambda: A.copy(xn.t[:], px.t[0:64, :].rearrange("p (h t) -> p h t", h=8)), [px.k], [xn.k])
                        py = pys[c]
                        act(lambda: A.copy(yn.t[:], py.t[0:64, :].rearrange("p (h t) -> p h t", h=8)), [py.k], [yn.k])
                    pzs = {}
                    for c in CS:
                        yn = YYc[c][(lvl + 1) % 2]
                        pz = P.bank()
                        for h in range(8):
                            mm(pz.t[0:64, h * 64:(h + 1) * 64], yn.t[:, h, :], ZZc[c].t[:, h, :], True, True,
                               [yn.k, ZZc[c].k], [pz.k], h == 7)
                        pzs[c] = pz
                    for c in CS:
                        pz = pzs[c]
                        dve(lambda: V.tensor_tensor(ZZc[c].t[:], pz.t[0:64, :].rearrange("p (h t) -> p h t", h=8), ZZc[c].t[:], ALU.add),
                            [pz.k, ZZc[c].k], [ZZc[c].k])
                pvs = {}
                for c in CS:
                    pv = P.bank()
                    for h in range(8):
                        mm(pv.t[0:64, h * 64:(h + 1) * 64], ATB[c].t[:, h, 0:64], VTM.t[:, c, h * 64:(h + 1) * 64], True, True,
                           [ATB[c].k, VTM.k], [pv.k], h == 7)
                    pvs[c] = pv
                for c in CS:
                    pv = pvs[c]
                    if c == 0:
                        act(lambda: A.copy(AVA.t[:, c, :, 0:64], pv.t[0:64, :].rearrange("p (h t) -> p h t", h=8)), [pv.k], [AVA.k])
                    else:
                        dve(lambda: V.tensor_copy(AVA.t[:, c, :, 0:64], pv.t[0:64, :].rearrange("p (h t) -> p h t", h=8)), [pv.k], [AVA.k])
                for c in CS:
                    tt, uw = ZZc[c], UW[c]
                    bks = [P.bank(), P.bank()]
                    for h in range(8):
                        bkx = bks[h // 4]
                        mm(bkx.t[0:64, (h % 4) * 128:(h % 4 + 1) * 128], tt.t[:, h, :], AVA.t[:, c, h, :], True, True,
                           [tt.k, AVA.k], [bkx.k], h % 4 == 3)
                    act(lambda: A.copy(uw.t[:, 0:4, :], bks[0].t[0:64, :].rearrange("p (h t) -> p h t", h=4)), [bks[0].k], [uw.k])
                    dve(lambda: V.tensor_copy(uw.t[:, 4:8, :], bks[1].t[0:64, :].rearrange("p (h t) -> p h t", h=4)), [bks[1].k], [uw.k])
                for c in CS:
                    ata, atb, uw, gt, hh = ATA[c], ATB[c], UW[c], GT[c], HH[c]
                    po = P.bank()
                    for h in range(8):
                        o = po.t[0:64, h * 64:(h + 1) * 64]
                        mm(o, uw.t[:, h, 0:64], ata.t[:, h, 64:128], True, False, [uw.k, ata.k], [po.k], False)
                        mm(o, VTM.t[:, c, h * 64:(h + 1) * 64], atb.t[:, h, 64:128], False, True, [VTM.k, atb.k], [po.k], h == 7)
                    act(lambda: A.copy(OLOC.t[:, :, c * 64:(c + 1) * 64], po.t[0:64, :].rearrange("p (h t) -> p h t", h=8)),
                        [po.k], [OLOC.k])
                    pq = P.bank()
                    for h in range(8):
                        j, hp = h // 2, h % 2
                        o = pq.t[0:64, h * 64:(h + 1) * 64]
                        mm(o, uw.t[:, h, 64:128], ata.t[:, h, 64:128], True, False, [uw.k, ata.k], [pq.k], False)
                        mm(o, IDB.t[:, 64 * hp:64 * hp + 64], AR.t[:, j, c, 1, :], False, True, [IDB.k, AR.k], [pq.k], h == 7)
                    dve(lambda: V.tensor_copy(QT.t[:, :, c * 64:(c + 1) * 64], pq.t[0:64, :].rearrange("p (h t) -> p h t", h=8)),
                        [pq.k], [QT.k])
                    pg_ = P.bank()
                    for h in range(8):
                        j, hp = h // 2, h % 2
                        o = pg_.t[0:64, h * 64:(h + 1) * 64]
                        mm(o, uw.t[:, h, 64:128], BHTM.t[:, c, h * 64:(h + 1) * 64], True, False, [uw.k, BHTM.k], [pg_.k], False)
                        mm(o, CON.t[:, K_ID + 64 * hp:K_ID + 64 * hp + 64], DG.t[:, j, c, :], False, True,
                           [CON.k, DG.k], [pg_.k], h == 7)
                    act(lambda: A.copy(gt.t[:], pg_.t[0:64, :].rearrange("p (h t) -> p h t", h=8)), [pg_.k], [gt.k])
                    phh = P.bank()
                    for h in range(8):
                        o = phh.t[0:64, h * 64:(h + 1) * 64]
                        mm(o, BHTM.t[:, c, h * 64:(h + 1) * 64], uw.t[:, h, 0:64], True, False, [BHTM.k, uw.k], [phh.k], False)
                        mm(o, KHTM.t[:, c, h * 64:(h + 1) * 64], VTM.t[:, c, h * 64:(h + 1) * 64], False, True,
                           [KHTM.k, VTM.k], [phh.k], h == 7)
                    dve(lambda: V.tensor_copy(hh.t[:], phh.t[0:64, :].rearrange("p (h t) -> p h t", h=8)), [phh.k], [hh.k])
                for c in CS:
                    gt, hh = GT[c], HH[c]
                    scur = SRING[sci[0] % 3]
                    snext = SRING[(sci[0] + 1) % 3]
                    sci[0] += 1
                    pO = P.bank()
                    for h in range(8):
                        mm(pO.t[0:64, h * 64:(h + 1) * 64], scur.t[:, h, :], QT.t[:, h, c * 64:(c + 1) * 64], True, True,
                           [scur.k, QT.k], [pO.k], h == 7)
                    dve(lambda: V.tensor_tensor(OLOC.t[:, :, c * 64:(c + 1) * 64],
                                                pO.t[0:64, :].rearrange("p (h t) -> p h t", h=8),
                                                OLOC.t[:, :, c * 64:(c + 1) * 64], ALU.add), [pO.k, OLOC.k], [OLOC.k])
                    pS = P.bank()
                    for h in range(8):
                        mm(pS.t[0:64, h * 64:(h + 1) * 64], gt.t[:, h, :], scur.t[:, h, :], True, True,
                           [gt.k, scur.k], [pS.k], h == 7)
                    dve(lambda: V.tensor_tensor(snext.t[:], pS.t[0:64, :].rearrange("p (h t) -> p h t", h=8), hh.t[:], ALU.add),
                        [pS.k, hh.k], [snext.k])
                ofl = OLOC.t[:].rearrange("p h t -> p (h t)")
                dfl = DN.t[:].rearrange("p h t -> p (h t)")
                sfl = SQ.t[:].rearrange("p h t -> p (h t)")
                for q in range(2):
                    bk = P.bank()
                    mm(bk.t[0:64, :], ON64, ofl[:, q * 512:(q + 1) * 512], True, True, [CON.k, OLOC.k], [bk.k], True)
                    dve(lambda: V.scalar_tensor_tensor(dfl[:, q * 512:(q + 1) * 512], bk.t[0:64, :], -1.0 / 64,
                                                       ofl[:, q * 512:(q + 1) * 512], ALU.mult, ALU.add),
                        [bk.k, OLOC.k], [DN.k])
                act(lambda: A.activation(out=SQ.t[:], in_=DN.t[:], func=AF.Square), [DN.k], [SQ.k])
                for q in range(2):
                    bk = P.bank()
                    mm(bk.t[0:64, :], ON64, sfl[:, q * 512:(q + 1) * 512], True, True, [CON.k, SQ.k], [bk.k], True)
                    act(lambda: A.activation(out=sfl[:, q * 512:(q + 1) * 512], in_=bk.t[0:64, :], func=AF.Ln,
                                             bias=GN_EPS, scale=1.0 / 64), [bk.k], [SQ.k])
                act(lambda: A.activation(out=SQ.t[:], in_=SQ.t[:], func=AF.Exp, scale=-0.5), [SQ.k], [SQ.k])
                dve(lambda: V.tensor_tensor(DN.t[:], DN.t[:], SQ.t[:], ALU.mult), [DN.k, SQ.k], [DN.k])
                dve(lambda: V.tensor_tensor(DN.t[:], DN.t[:], V64.t[:, 0:8].unsqueeze(2).broadcast_to([64, 8, TG]), ALU.mult),
                    [DN.k, V64.k], [DN.k])
                dve(lambda: V.tensor_tensor(DN.t[:], DN.t[:], V64.t[:, 8:16].unsqueeze(2).broadcast_to([64, 8, TG]), ALU.add),
                    [DN.k, V64.k], [DN.k])
                dve(lambda: V.tensor_tensor(DN.t[:], DN.t[:], BV64.t[:], ALU.add), [DN.k, BV64.k], [DN.k])
                dve(lambda: V.tensor_tensor(OG.t[:, :, c0:c0 + TG], DN.t[:], G64.t[:], ALU.mult), [DN.k, G64.k], [OG.k])
            P.barrier()
            P.release(ph)

    def phase_BC(l, p):
        w = LW[l]
        with ExitStack() as ph:
            WB = sb(ph, "WB", [128, 10, 8, 128], BF16)
            P.dma("pool", WB.t[:], w["winB"].ap()[0:10].rearrange("g p k c -> p g k c"), W=[WB.k])
            TMP = [sb(ph, "bcT%d" % i, [128, 512], F32) for i in range(2)]
            ACC = sb(ph, "bcACC", [128, 2, NT], F32)
            DD = sb(ph, "bcD", [128, 2, 512], F32)
            GBB = sb(ph, "bcGB", [128, 2, NT], F32)
            ONF = cview(K_OF, K_OF + 128)
            ntg = NT // 512

            def proj(g, tg):
                bk = P.bank()
                for kc in range(8):
                    mm(bk.t[:], WB.t[:, g, kc, :], XT.t[:, kc, tg * 512:(tg + 1) * 512], kc == 0, kc == 7,
                       [WB.k, XT.k], [bk.k], kc == 7)
                return bk

            for tg in range(ntg):
                for c in range(2):
                    bu = proj(c, tg)
                    bg = proj(2 + c, tg)
                    tm = TMP[c]
                    act(lambda: A.activation(out=tm.t[:], in_=bg.t[:], func=AF.Sigmoid), [bg.k], [tm.k])
                    dve(lambda: V.tensor_tensor(UH.t[:, c, 32 + tg * 512:32 + (tg + 1) * 512], bu.t[:], tm.t[:], ALU.mult),
                        [bu.k, tm.k], [UH.k])
            for c in range(2):
                dve(lambda: V.tensor_scalar(ACC.t[:, c, :], UH.t[:, c, 2:2 + NT], VEC.t[:, 34 + c * 31:35 + c * 31],
                                            VEC.t[:, 96 + c:97 + c], ALU.mult, ALU.add), [UH.k, VEC.k], [ACC.k])
                for jj in range(1, 31):
                    dve(lambda: V.scalar_tensor_tensor(ACC.t[:, c, :], UH.t[:, c, 2 + jj:2 + jj + NT],
                                                       VEC.t[:, 34 + c * 31 + jj:35 + c * 31 + jj], ACC.t[:, c, :],
                                                       ALU.mult, ALU.add), [UH.k, VEC.k, ACC.k], [ACC.k])
            dve(lambda: V.tensor_copy(UH.t[:, :, 0:32], UH.t[:, :, NT:NT + 32]), [UH.k], [UH.k])
            for tg in range(ntg):
                ts_ = slice(tg * 512, (tg + 1) * 512)
                bk = P.bank()
                for c in range(2):
                    mm(bk.t[:], ONF, ACC.t[:, c, ts_], c == 0, c == 1, [CON.k, ACC.k], [bk.k], c == 1)
                for c in range(2):
                    dve(lambda: V.scalar_tensor_tensor(DD.t[:, c, :], bk.t[:], -1.0 / 256, ACC.t[:, c, ts_], ALU.mult, ALU.add),
                        [bk.k, ACC.k], [DD.k])
                    act(lambda: A.activation(out=ACC.t[:, c, ts_], in_=DD.t[:, c, :], func=AF.Square), [DD.k], [ACC.k])
                bk2 = P.bank()
                for c in range(2):
                    mm(bk2.t[:], ONF, ACC.t[:, c, ts_], c == 0, c == 1, [CON.k, ACC.k], [bk2.k], c == 1)
                tm = TMP[0]
                act(lambda: A.activation(out=tm.t[:], in_=bk2.t[:], func=AF.Sqrt, bias=LN_EPS, scale=1.0 / 256), [bk2.k], [tm.k])
                dve(lambda: V.reciprocal(tm.t[:], tm.t[:]), [tm.k], [tm.k])
                for c in range(2):
                    dve(lambda: V.tensor_tensor(DD.t[:, c, :], DD.t[:, c, :], tm.t[:], ALU.mult), [DD.k, tm.k], [DD.k])
                    dve(lambda: V.tensor_scalar(DD.t[:, c, :], DD.t[:, c, :], VEC.t[:, 98 + c:99 + c], VEC.t[:, 100 + c:101 + c],
                                                ALU.mult, ALU.add), [DD.k, VEC.k], [DD.k])
                    act(lambda: A.activation(out=UB.t[:, c, ts_], in_=DD.t[:, c, :], func=AF.Silu), [DD.k], [UB.k])
            for tg in range(ntg):
                for c in range(2):
                    bgb = proj(4 + c, tg)
                    act(lambda: A.copy(GBB.t[:, c, tg * 512:(tg + 1) * 512], bgb.t[:]), [bgb.k], [GBB.k])
                    bgc = proj(6 + c, tg)
                    bh = proj(8 + c, tg)
                    tm = TMP[c]
                    act(lambda: A.copy(tm.t[:], bh.t[:]), [bh.k], [tm.k])
                    dve(lambda: V.tensor_tensor(GH.t[:, c, 32 + tg * 512:32 + (tg + 1) * 512], bgc.t[:], tm.t[:], ALU.mult),
                        [bgc.k, tm.k], [GH.k])
            for c in range(2):
                dve(lambda: V.tensor_scalar(ACC.t[:, c, :], GH.t[:, c, 30:30 + NT], VEC.t[:, 102 + c * 3:103 + c * 3], None,
                                            ALU.mult), [GH.k, VEC.k], [ACC.k])
                for jj in range(1, 3):
                    dve(lambda: V.scalar_tensor_tensor(ACC.t[:, c, :], GH.t[:, c, 30 + jj:30 + jj + NT],
                                                       VEC.t[:, 102 + c * 3 + jj:103 + c * 3 + jj], ACC.t[:, c, :],
                                                       ALU.mult, ALU.add), [GH.k, VEC.k, ACC.k], [ACC.k])
                dve(lambda: V.tensor_tensor(UC.t[:, c, :], GBB.t[:, c, :], ACC.t[:, c, :], ALU.mult), [GBB.k, ACC.k], [UC.k])
            dve(lambda: V.tensor_copy(GH.t[:, :, 0:32], GH.t[:, :, NT:NT + 32]), [GH.k], [GH.k])
            P.barrier()
            P.release(ph)

    def phase_GO(l, p, src):
        w = LW[l]
        with ExitStack() as ph:
            MT = sb(ph, "MT", [128, 8, NT], BF16)
            with ExitStack() as ph2:
                WOA = sb(ph2, "WOA", [64, 8, 1024], BF16)
                WOB = sb(ph2, "WOB", [128, 2, 1024], BF16)
                WOC = sb(ph2, "WOC", [128, 2, 1024], BF16)
                P.dma("pool", WOA.t[:], w["woa"].ap(), W=[WOA.k])
                P.dma("pool", WOB.t[:], w["wob"].ap(), W=[WOB.k])
                P.dma("pool", WOC.t[:], w["woc"].ap(), W=[WOC.k])
                WG = [sb(ph2, "WGt%d" % i, [128, 3, 8, 128], BF16) for i in range(2)]
                GS = [sb(ph2, "GS%d" % i, [128, 512], F32) for i in range(3)]
                MA = sb(ph2, "MA", [128, 512], F32)
                MB = sb(ph2, "MB", [128, 512], F32)
                ntg = NT // 512
                for i in range(8):
                    wgt = WG[i % 2]
                    P.dma("pool", wgt.t[:], w["winB"].ap()[10 + 3 * i:13 + 3 * i].rearrange("g p k c -> p g k c"), W=[wgt.k])
                    for tg in range(ntg):
                        ts_ = slice(tg * 512, (tg + 1) * 512)
                        for br in range(3):
                            bk = P.bank()
                            for kc in range(8):
                                mm(bk.t[:], wgt.t[:, br, kc, :], XT.t[:, kc, ts_], kc == 0, kc == 7, [wgt.k, XT.k], [bk.k], kc == 7)
                            act(lambda: A.activation(out=GS[br].t[:], in_=bk.t[:], func=AF.Sigmoid), [bk.k], [GS[br].k])
                        ba = P.bank()
                        for h in range(8):
                            mm(ba.t[:], WOA.t[:, h, i * 128:(i + 1) * 128], OG.t[:, h, ts_], h == 0, h == 7, [WOA.k, OG.k], [ba.k], h == 7)
                        dve(lambda: V.tensor_tensor(MA.t[:], ba.t[:], GS[0].t[:], ALU.mult), [ba.k, GS[0].k], [MA.k])
                        bb_ = P.bank()
                        for c in range(2):
                            mm(bb_.t[:], WOB.t[:, c, i * 128:(i + 1) * 128], UB.t[:, c, ts_], c == 0, c == 1, [WOB.k, UB.k], [bb_.k], c == 1)
                        dve(lambda: V.tensor_tensor(MB.t[:], bb_.t[:], GS[1].t[:], ALU.mult), [bb_.k, GS[1].k], [MB.k])
                        dve(lambda: V.tensor_tensor(MA.t[:], MA.t[:], MB.t[:], ALU.add), [MA.k, MB.k], [MA.k])
                        bc = P.bank()
                        for c in range(2):
                            mm(bc.t[:], WOC.t[:, c, i * 128:(i + 1) * 128], UC.t[:, c, ts_], c == 0, c == 1, [WOC.k, UC.k], [bc.k], c == 1)
                        dve(lambda: V.tensor_tensor(MB.t[:], bc.t[:], GS[2].t[:], ALU.mult), [bc.k, GS[2].k], [MB.k])
                        dve(lambda: V.tensor_tensor(MT.t[:, i, ts_], MA.t[:], MB.t[:], ALU.add), [MA.k, MB.k], [MT.k])
                P.barrier()
                P.release(ph2)
            WOUT = sb(ph, "WOUT", [128, 8, 1024], BF16)
            P.dma("pool", WOUT.t[:], w["wout"].ap(), W=[WOUT.k])
            LNP = sb(ph, "LNP", [128, 2, 1024], F32)
            P.dma("sp", LNP.t[:], w["lnp"].ap()[:, 0:2, :], W=[LNP.k])
            XR = [sb(ph, "XR%d" % i, [128, D], F32) for i in range(2)]
            ZB = [sb(ph, "ZB%d" % i, [128, D], F32) for i in range(2)]
            ST = sb(ph, "ST", [128, 2, 6], F32)
            MV = sb(ph, "MV", [128, 4], F32)
            XTF = sb(ph, "XTF", [128, 8, 128], F32)
            LG = sb(ph, "LG", [128, 8], F32)
            LG2 = sb(ph, "LG2", [128, 8], F32)
            EQ1 = sb(ph, "EQ1", [128, 8], F32)
            EQ2 = sb(ph, "EQ2", [128, 8], F32)
            SM = sb(ph, "SMx", [128, 8], F32)
            for i in range(NT // 128):
                r0 = p * NT + i * 128
                xr = XR[i % 2]
                zb = ZB[i % 2]
                P.dma("sp", xr.t[:], src[r0:r0 + 128, :], W=[xr.k])
                for hf in range(2):
                    bk = P.bank()
                    for kc in range(8):
                        mm(bk.t[:], MT.t[:, kc, i * 128:(i + 1) * 128], WOUT.t[:, kc, hf * 512:(hf + 1) * 512], kc == 0, kc == 7,
                           [MT.k, WOUT.k], [bk.k], kc == 7)
                    dve(lambda: V.scalar_tensor_tensor(zb.t[:, hf * 512:(hf + 1) * 512], xr.t[:, hf * 512:(hf + 1) * 512], ALPHA,
                                                       bk.t[:], ALU.mult, ALU.add), [xr.k, bk.k], [zb.k])
                layer_norm(zb, LNP, 0, ST, MV)
                P.dma("sp", xm.ap()[r0:r0 + 128, :], zb.t[:], R=[zb.k], W=[tok_xm], part=True, own=zb.k)
                for hf in range(2):
                    bk = P.bank()
                    for q in range(4):
                        kc = hf * 4 + q
                        P.op("pe", lambda: T.transpose(bk.t[:, q * 128:(q + 1) * 128], zb.t[:, kc * 128:(kc + 1) * 128], ID),
                             [zb.k, CON.k], [bk.k], inc=(q == 3))
                    iv = bk.t[:].rearrange("p (q t) -> p q t", q=4)
                    if moe[l]:
                        act(lambda: A.copy(XTF.t[:, hf * 4:hf * 4 + 4, :], iv), [bk.k], [XTF.k])
                        dve(lambda: V.tensor_copy(XT.t[:, hf * 4:hf * 4 + 4, i * 128:(i + 1) * 128], XTF.t[:, hf * 4:hf * 4 + 4, :]),
                            [XTF.k], [XT.k])
                    else:
                        act(lambda: A.copy(XT.t[:, hf * 4:hf * 4 + 4, i * 128:(i + 1) * 128], iv), [bk.k], [XT.k])
                if moe[l] and mdbg >= 2:
                    bk = P.bank()
                    for kc in range(8):
                        mm(bk.t[:, 0:8], XTF.t[:, kc, :], ROUT.t[:, kc, :], kc == 0, kc == 7, [XTF.k, ROUT.k], [bk.k], kc == 7)
                    dve(lambda: V.tensor_copy(LG.t[:], bk.t[:, 0:8]), [bk.k], [LG.k])
                if moe[l] and mdbg >= 3:
                    dve(lambda: V.tensor_reduce(SM.t[:, 0:1], LG.t[:], AX.X, ALU.max), [LG.k], [SM.k])
                    dve(lambda: V.tensor_scalar(EQ1.t[:], LG.t[:], SM.t[:, 0:1], None, ALU.is_equal), [LG.k, SM.k], [EQ1.k])
                    dve(lambda: V.scalar_tensor_tensor(LG2.t[:], EQ1.t[:], -1e30, LG.t[:], ALU.mult, ALU.add), [EQ1.k, LG.k], [LG2.k])
                    dve(lambda: V.tensor_reduce(SM.t[:, 1:2], LG2.t[:], AX.X, ALU.max), [LG2.k], [SM.k])
                    dve(lambda: V.tensor_scalar(EQ2.t[:], LG2.t[:], SM.t[:, 1:2], None, ALU.is_equal), [LG2.k, SM.k], [EQ2.k])
                    dve(lambda: V.tensor_tensor(SM.t[:, 2:3], SM.t[:, 1:2], SM.t[:, 0:1], ALU.subtract), [SM.k], [SM.k])
                    act(lambda: A.activation(out=SM.t[:, 3:4], in_=SM.t[:, 2:3], func=AF.Exp), [SM.k], [SM.k])
                    dve(lambda: V.tensor_scalar(SM.t[:, 4:5], SM.t[:, 3:4], 1.0, None, ALU.add), [SM.k], [SM.k])
                    dve(lambda: V.reciprocal(SM.t[:, 5:6], SM.t[:, 4:5]), [SM.k], [SM.k])
                    dve(lambda: V.tensor_tensor(SM.t[:, 6:7], SM.t[:, 3:4], SM.t[:, 5:6], ALU.mult), [SM.k], [SM.k])
                    dve(lambda: V.tensor_scalar(EQ1.t[:], EQ1.t[:], SM.t[:, 5:6], None, ALU.mult), [EQ1.k, SM.k], [EQ1.k])
                    dve(lambda: V.scalar_tensor_tensor(GATE.t[:, i, :], EQ2.t[:], SM.t[:, 6:7], EQ1.t[:], ALU.mult, ALU.add),
                        [EQ2.k, SM.k, EQ1.k], [GATE.k])
            P.barrier()
            P.release(ph)

    def layer_norm(zb, LNP, gi, ST, MV):
        for hf in range(2):
            dve(lambda: V.bn_stats(ST.t[:, hf, :], zb.t[:, hf * 512:(hf + 1) * 512]), [zb.k], [ST.k])
        dve(lambda: V.bn_aggr(MV.t[:, 0:2], ST.t[:].rearrange("p a b -> p (a b)")), [ST.k], [MV.k])
        act(lambda: A.activation(out=MV.t[:, 2:3], in_=MV.t[:, 1:2], func=AF.Sqrt, bias=LN_EPS, scale=1.0), [MV.k], [MV.k])
        dve(lambda: V.reciprocal(MV.t[:, 3:4], MV.t[:, 2:3]), [MV.k], [MV.k])
        dve(lambda: V.tensor_scalar(zb.t[:], zb.t[:], MV.t[:, 0:1], MV.t[:, 3:4], ALU.subtract, ALU.mult), [zb.k, MV.k], [zb.k])
        dve(lambda: V.tensor_tensor(zb.t[:], zb.t[:], LNP.t[:, gi, :], ALU.mult), [zb.k, LNP.k], [zb.k])
        dve(lambda: V.tensor_tensor(zb.t[:], zb.t[:], LNP.t[:, gi + 1, :], ALU.add), [zb.k, LNP.k], [zb.k])

    def phase_F(l, p, dst, tok_dst):
        w = LW[l]
        E = 8 if moe[l] else 1
        with ExitStack() as ph:
            ACC = sb(ph, "fACC", [128, NT // 128, D], F32)
            WGs = [sb(ph, "fWG%d" % i, [128, 8, 256], BF16) for i in range(2)]
            WUs = [sb(ph, "fWU%d" % i, [128, 8, 256], BF16) for i in range(2)]
            WDs = [sb(ph, "fWD%d" % i, [128, 2, 1024], BF16) for i in range(2)]
            HT = [sb(ph, "fHT%d" % i, [128, 2, 512], BF16) for i in range(2)]
            SGT = [sb(ph, "fSG%d" % i, [128, 512], F32) for i in range(2)]
            LNP = sb(ph, "fLNP", [128, 2, 1024], F32)
            P.dma("sp", LNP.t[:], w["lnp"].ap()[:, 2:4, :], W=[LNP.k])
            ST = sb(ph, "fST", [128, 2, 6], F32)
            MV = sb(ph, "fMV", [128, 4], F32)
            XR = [sb(ph, "fXR%d" % i, [128, D], F32) for i in range(2)]
            ntg = NT // 512
            it = 0
            for e in range(E):
                for g in range(NFG):
                    wg, wu, wd = WGs[it % 2], WUs[it % 2], WDs[it % 2]
                    P.dma("pool", wg.t[:], w["wg"].ap()[e, g], W=[wg.k])
                    P.dma("pool", wu.t[:], w["wu"].ap()[e, g], W=[wu.k])
                    P.dma("pool", wd.t[:], w["wd"].ap()[e, g], W=[wd.k])
                    for tg in range(ntg):
                        ts_ = slice(tg * 512, (tg + 1) * 512)
                        ht = HT[tg % 2]
                        for fc in range(2):
                            bg = P.bank()
                            for kc in range(8):
                                mm(bg.t[:], wg.t[:, kc, fc * 128:(fc + 1) * 128], XT.t[:, kc, ts_], kc == 0, kc == 7,
                                   [wg.k, XT.k], [bg.k], kc == 7)
                            bu = P.bank()
                            for kc in range(8):
                                mm(bu.t[:], wu.t[:, kc, fc * 128:(fc + 1) * 128], XT.t[:, kc, ts_], kc == 0, kc == 7,
                                   [wu.k, XT.k], [bu.k], kc == 7)
                            sg = SGT[fc]
                            act(lambda: A.activation(out=sg.t[:], in_=bg.t[:], func=AF.Silu), [bg.k], [sg.k])
                            dve(lambda: V.tensor_tensor(ht.t[:, fc, :], bu.t[:], sg.t[:], ALU.mult), [bu.k, sg.k], [ht.k])
                        for tt_ in range(4):
                            ti = tg * 4 + tt_
                            for hf in range(2):
                                bk = P.bank()
                                for fc in range(2):
                                    mm(bk.t[:], ht.t[:, fc, tt_ * 128:(tt_ + 1) * 128], wd.t[:, fc, hf * 512:(hf + 1) * 512],
                                       fc == 0, fc == 1, [ht.k, wd.k], [bk.k], fc == 1)
                                o = ACC.t[:, ti, hf * 512:(hf + 1) * 512]
                                if moe[l]:
                                    gsc = GATE.t[:, ti, e:e + 1]
                                    if it == 0:
                                        dve(lambda: V.tensor_scalar(o, bk.t[:], gsc, None, ALU.mult), [bk.k, GATE.k], [ACC.k])
                                    else:
                                        dve(lambda: V.scalar_tensor_tensor(o, bk.t[:], gsc, o, ALU.mult, ALU.add),
                                            [bk.k, GATE.k, ACC.k], [ACC.k])
                                else:
                                    if it == 0:
                                        act(lambda: A.copy(o, bk.t[:]), [bk.k], [ACC.k])
                                    else:
                                        dve(lambda: V.tensor_tensor(o, bk.t[:], o, ALU.add), [bk.k, ACC.k], [ACC.k])
                    it += 1
            for i in range(NT // 128):
                r0 = p * NT + i * 128
                xr = XR[i % 2]
                P.dma("sp", xr.t[:], xm.ap()[r0:r0 + 128, :], W=[xr.k])
                dve(lambda: V.scalar_tensor_tensor(xr.t[:], xr.t[:], ALPHA, ACC.t[:, i, :], ALU.mult, ALU.add), [xr.k, ACC.k], [xr.k])
                layer_norm(xr, LNP, 0, ST, MV)
                P.dma("sp", dst[r0:r0 + 128, :], xr.t[:], R=[xr.k], W=[tok_dst], part=True, own=xr.k)
            P.barrier()
            P.release(ph)

    for li, l in enumerate(layers):
        w = LW[l]
        src = x_in.ap() if li == 0 else xl1.ap()
        if li == nlast:
            dst, tok_dst = y_out.ap(), tok_y
        else:
            dst, tok_dst = xl1.ap(), tok_xl1
        P.dma("sp", VEC.t[:], w["vec"].ap(), W=[VEC.k])
        P.dma("sp", V64.t[:], w["vec64"].ap(), W=[V64.k])
        P.dma("pool", W2A2.t[:], w["w2a2"].ap(), W=[W2A2.k])
        P.dma("pool", G2.t[:], w["g2"].ap(), W=[G2.k])
        if moe[l]:
            P.dma("sp", ROUT.t[:], w["router"].ap(), W=[ROUT.k])
        dve(lambda: V.tensor_scalar(OMM.t[:], VEC.t[:, 0:14], -1.0, 1.0, ALU.mult, ALU.add), [VEC.k], [OMM.k])
        dve(lambda: V.tensor_scalar(OMKA.t[:], VEC.t[:, 26:30], -1.0, 1.0, ALU.mult, ALU.add), [VEC.k], [OMKA.k])
        dve(lambda: V.memset(TAILS.t[:], 0.0), [], [TAILS.k])
        dve(lambda: V.memset(UH.t[:, :, 0:32], 0.0), [], [UH.k])
        dve(lambda: V.memset(GH.t[:, :, 0:32], 0.0), [], [GH.k])
        dve(lambda: V.memset(SRING[sci[0] % 3].t[:], 0.0), [], [SRING[sci[0] % 3].k])
        P.barrier()
        for p in range(npass):
            if "X" in phases:
                phase_X(src, p)
            if "R" in phases:
                phase_R(l, p)
            if "B" in phases:
                phase_BC(l, p)
            if "G" in phases:
                phase_GO(l, p, src)
            if "F" in phases:
                phase_F(l, p, dst, tok_dst)
    P.barrier()
    es.close()
    return nc


def make_consts():
    c = np.zeros((128, K_END), np.float32)
    c[:, K_ID:K_ID + 128] = np.eye(128, dtype=np.float32)
    c[0:64, K_I2:K_I2 + 64] = np.eye(64, dtype=np.float32)
    c[64:128, K_I2:K_I2 + 64] = np.eye(64, dtype=np.float32)
    s = np.arange(64)[:, None]
    n = np.arange(64)[None, :]
    c[0:64, K_MKA:K_MKA + 64] = (s < n)
    c[0:64, K_MKA + 64:K_MKA + 128] = (s <= n)
    c[0:64, K_ML:K_ML + 64] = (n < s)
    c[0:64, K_OB:K_OB + 64] = 1.0
    c[64:128, K_OB + 64:K_OB + 128] = 1.0
    c[:, K_OF:K_OF + 128] = 1.0
    sm = np.ones(512, np.float32)
    sm[::64] = 0.0
    c[:, K_SM:K_SM + 512] = sm[None, :]
    return c


def prep_layer(inp, l, is_moe, j):
    f = np.float32
    out = {}
    w_in = np.asarray(inp["w_in"][l], f)
    W = w_in.reshape(8, 128, 48, 128).transpose(2, 1, 0, 3)
    out["winA%d" % l] = np.ascontiguousarray(W[0:14])
    order = list(range(14, 24)) + [24 + br * 8 + i for i in range(8) for br in range(3)]
    out["winB%d" % l] = np.ascontiguousarray(W[order])
    vec = np.zeros((128, NVEC), f)
    vec[:, 0:14] = np.asarray(inp["rwkv_mu"][l], f).reshape(14, 128).T
    for c0, name in ((14, "rwkv_w0"), (18, "rwkv_a0"), (22, "rwkv_k_k"), (26, "rwkv_k_a"), (30, "rwkv_r_k")):
        vec[:, c0:c0 + 4] = np.asarray(inp[name][l], f).reshape(4, 128).T
    cdw = np.asarray(inp["conf_dw"][l], f)
    for c in range(2):
        vec[:, 34 + c * 31:34 + (c + 1) * 31] = cdw[:, c * 128:(c + 1) * 128].T
    vec[:, 96:98] = np.asarray(inp["conf_dw_b"][l], f).reshape(2, 128).T
    vec[:, 98:100] = np.asarray(inp["conf_ln_g"][l], f).reshape(2, 128).T
    vec[:, 100:102] = np.asarray(inp["conf_ln_b"][l], f).reshape(2, 128).T
    sdw = np.asarray(inp["short_dw"][l], f)
    for c in range(2):
        vec[:, 102 + c * 3:102 + (c + 1) * 3] = sdw[:, c * 128:(c + 1) * 128].T
    out["vec%d" % l] = vec
    v64 = np.zeros((64, 16), f)
    v64[:, 0:8] = np.asarray(inp["rwkv_ln_g"][l], f).reshape(8, 64).T
    v64[:, 8:16] = np.asarray(inp["rwkv_ln_b"][l], f).reshape(8, 64).T
    out["vec64_%d" % l] = v64
    out["w2a2_%d" % l] = np.ascontiguousarray(np.concatenate([np.asarray(inp["rwkv_w2"][l], f), np.asarray(inp["rwkv_a2"][l], f)], 0))
    out["g2_%d" % l] = np.ascontiguousarray(np.asarray(inp["rwkv_g2"][l], f))
    out["woa%d" % l] = np.ascontiguousarray(np.asarray(inp["rwkv_w_o"][l], f).reshape(8, 64, 1024).transpose(1, 0, 2))
    out["wob%d" % l] = np.ascontiguousarray(np.asarray(inp["conf_w_o"][l], f).reshape(2, 128, 1024).transpose(1, 0, 2))
    out["woc%d" % l] = np.ascontiguousarray(np.asarray(inp["short_w_o"][l], f).reshape(2, 128, 1024).transpose(1, 0, 2))
    out["wout%d" % l] = np.ascontiguousarray(np.asarray(inp["w_out"][l], f).reshape(8, 128, 1024).transpose(1, 0, 2))
    lnp = np.stack([np.asarray(inp[n][l], f) for n in ("ln1_g", "ln1_b", "ln2_g", "ln2_b")], 0)
    out["lnp%d" % l] = np.ascontiguousarray(np.broadcast_to(lnp[None], (128, 4, 1024)))
    if is_moe:
        wg = np.asarray(inp["moe_w_gate"][j], f)
        wu = np.asarray(inp["moe_w_up"][j], f)
        wd = np.asarray(inp["moe_w_down"][j], f)
        out["router%d" % l] = np.ascontiguousarray(np.asarray(inp["moe_router"][j], f).reshape(8, 128, 8).transpose(1, 0, 2))
    else:
        wg = np.asarray(inp["ffn_w_gate"][j], f)[None]
        wu = np.asarray(inp["ffn_w_up"][j], f)[None]
        wd = np.asarray(inp["ffn_w_down"][j], f)[None]
    E = wg.shape[0]
    out["wg%d" % l] = np.ascontiguousarray(wg.reshape(E, 8, 128, NFG, 256).transpose(0, 3, 2, 1, 4))
    out["wu%d" % l] = np.ascontiguousarray(wu.reshape(E, 8, 128, NFG, 256).transpose(0, 3, 2, 1, 4))
    out["wd%d" % l] = np.ascontiguousarray(wd.reshape(E, NFG, 2, 128, 1024).transpose(0, 1, 3, 2, 4))
    return out


_NC_CACHE = {}


def kernel(**inputs):
    x = np.asarray(inputs["x"], np.float32)
    B, S, _ = x.shape
    if S not in _NC_CACHE:
        _NC_CACHE[S] = build(S)
    nc = _NC_CACHE[S]
    shared = {"consts": make_consts()}
    for l in range(2):
        shared.update(prep_layer(inputs, l, l % 2 == 1, l // 2))
    maps = []
    for b in range(B):
        m = dict(shared)
        m["x"] = np.ascontiguousarray(x[b])
        maps.append(m)
    in_maps = [maps[c % B] for c in range(8)]
    res = run_bass_kernel_spmd(nc, in_maps, core_ids=list(range(8)))
    return np.stack([np.asarray(res.results[b]["y"], np.float32) for b in range(B)], 0)
```

```python
import numpy as np
from contextlib import ExitStack
import concourse.bass as bass
import concourse.mybir as mybir
from concourse.bass_utils import run_bass_kernel_spmd

F32 = mybir.dt.float32
BF16 = mybir.dt.bfloat16
AF = mybir.ActivationFunctionType
ALU = mybir.AluOpType
AX = mybir.AxisListType

D = 1024
TG = 128
CH = 64
C0 = float(np.exp(-0.5))
ALPHA = float(4.0 ** 0.25)
LN_EPS = 1e-5
GN_EPS = 64e-5
NF = 2816
NFG = 11
K_ID, K_I2, K_MKA, K_ML, K_OB, K_OF, K_SM, K_END = 0, 128, 192, 320, 384, 512, 640, 1152
NVEC = 108


class Tok:
    __slots__ = ("w", "r", "sem", "cnt", "name")

    def __init__(self, name="t"):
        self.name = name
        self.w = {}
        self.r = {}
        self.sem = None
        self.cnt = 0


class DSem:
    __slots__ = ("h", "cnt", "q")

    def __init__(self, h, q):
        self.h = h
        self.cnt = 0
        self.q = q


class Buf:
    def __init__(self, t, name="b"):
        self.t = t
        self.k = Tok(name)


class Prog:
    def __init__(self, nc, es):
        self.nc = nc
        self.es = es
        self.eng = {"pe": nc.tensor, "act": nc.scalar, "dve": nc.vector, "pool": nc.gpsimd, "sp": nc.sync}
        self.sem = {}
        self.cnt = {}
        self.known = {e: {} for e in self.eng}
        for e in ("pe", "act", "dve", "pool"):
            self.sem[e] = es.enter_context(nc.semaphore("sem_" + e))
            self.cnt[e] = 0
        self.nsem = 0
        self.dsems = []
        self.free_dsems = {"sp": [], "pool": []}
        self.banks = []
        self.bi = 0
        self.bbanks = []
        self.bbi = 0
        self.dummy = None
        self.ninst = 0

    def _wait(self, e, deps):
        kn = self.known[e]
        need = {}
        for (sem, val, owner) in deps:
            if owner is not None:
                if owner == e and e == "pe":
                    continue
                assert val <= self.cnt[owner], "uncovered dependency"
            key = id(sem)
            if kn.get(key, 0) >= val:
                continue
            if key not in need or need[key][1] < val:
                need[key] = (sem, val)
        for key, (sem, val) in need.items():
            self.eng[e].wait_ge(sem, val)
            kn[key] = val
            self.ninst += 1

    @staticmethod
    def _put(d, rec):
        key = id(rec[0])
        if key not in d or d[key][1] < rec[1]:
            d[key] = rec

    def op(self, e, fn, R=(), W=(), inc=True):
        deps = []
        for t in R:
            deps += list(t.w.values())
        for t in W:
            deps += list(t.w.values())
            deps += list(t.r.values())
        self._wait(e, deps)
        ins = fn()
        self.ninst += 1
        if inc:
            self.cnt[e] += 1
            ins.then_inc(self.sem[e], 1)
            rec = (self.sem[e], self.cnt[e], e)
        else:
            rec = (self.sem[e], self.cnt[e] + 1, e)
        for t in R:
            self._put(t.r, rec)
        for t in W:
            t.w = {id(rec[0]): rec}
            t.r = {}
        return ins

    def dma(self, q, out, in_, R=(), W=(), part=False, own=None):
        deps = []
        for t in R:
            deps += list(t.w.values())
        for t in W:
            if not part:
                deps += list(t.w.values())
            deps += list(t.r.values())
        self._wait(q, deps)
        ins = self.eng[q].dma_start(out=out, in_=in_)
        self.ninst += 1
        t0 = own if own is not None else W[0]
        if t0.sem is None:
            if self.free_dsems[q]:
                t0.sem = self.free_dsems[q].pop()
            else:
                t0.sem = DSem(self.es.enter_context(self.nc.semaphore("d%d" % self.nsem)), q)
                self.nsem += 1
                self.dsems.append(t0.sem)
        assert t0.sem.q == q, "token DMA'd from two queue kinds"
        t0.sem.cnt += 16
        ins.then_inc(t0.sem.h, 16)
        rec = (t0.sem.h, t0.sem.cnt, None)
        for t in R:
            self._put(t.r, rec)
        for t in W:
            if part:
                self._put(t.w, rec)
            else:
                t.w = {id(rec[0]): rec}
                t.r = {}
        return ins

    def barrier(self):
        f = "pool"
        deps = [(self.sem[e], self.cnt[e], e) for e in ("pe", "act", "dve")]
        deps += [(d.h, d.cnt, None) for d in self.dsems]
        self._wait(f, deps)
        if self.cnt[f] > 0:
            self.eng[f].wait_ge(self.sem[f], self.cnt[f])
        ins = self.nc.gpsimd.memset(self.dummy.t[0:1, 0:1], 0.0)
        self.cnt[f] += 1
        ins.then_inc(self.sem[f], 1)
        for e in ("pe", "act", "dve", "sp"):
            self.eng[e].wait_ge(self.sem[f], self.cnt[f])
        for e in self.eng:
            kn = self.known[e]
            for c in ("pe", "act", "dve", "pool"):
                kn[id(self.sem[c])] = self.cnt[c]
            for d in self.dsems:
                kn[id(d.h)] = d.cnt

    def release(self, st):
        for b in getattr(st, "_bufs", []):
            if b.k.sem is not None:
                self.free_dsems[b.k.sem.q].append(b.k.sem)
                b.k.sem = None

    def bank(self):
        b = self.banks[self.bi % len(self.banks)]
        self.bi += 1
        return b

    def bbank(self):
        b = self.bbanks[self.bbi % len(self.bbanks)]
        self.bbi += 1
        return b


def build(S, layers=(0, 1), moe=(False, True), NT=1024, debug_xm=False, phases="XRBGF", rstop=0, mdbg=9):
    nc = bass.Bass("TRN2", target_bir_lowering=False)
    es = ExitStack()
    P = Prog(nc, es)
    V, A, T = nc.vector, nc.scalar, nc.tensor
    npass = S // NT
    assert S % NT == 0 and NT % 512 == 0
    nlast = len(layers) - 1

    def din(name, shape):
        return nc.dram_tensor(name, list(shape), F32, kind="ExternalInput")

    x_in = din("x", [S, D])
    y_out = nc.dram_tensor("y", [S, D], F32, kind="ExternalOutput")
    xl1 = nc.dram_tensor("xl1", [S, D], F32)
    if debug_xm:
        xm = nc.dram_tensor("xm", [S, D], F32, kind="ExternalOutput")
    else:
        xm = nc.dram_tensor("xm", [S, D], F32)
    consts_d = din("consts", [128, K_END])
    LW = {}
    for l in layers:
        E = 8 if moe[l] else 1
        LW[l] = dict(
            winA=din("winA%d" % l, [14, 128, 8, 128]), winB=din("winB%d" % l, [34, 128, 8, 128]),
            vec=din("vec%d" % l, [128, NVEC]), vec64=din("vec64_%d" % l, [64, 16]),
            w2a2=din("w2a2_%d" % l, [128, 512]), g2=din("g2_%d" % l, [128, 512]),
            woa=din("woa%d" % l, [64, 8, 1024]), wob=din("wob%d" % l, [128, 2, 1024]),
            woc=din("woc%d" % l, [128, 2, 1024]), wout=din("wout%d" % l, [128, 8, 1024]),
            lnp=din("lnp%d" % l, [128, 4, 1024]),
            wg=din("wg%d" % l, [E, NFG, 128, 8, 256]), wu=din("wu%d" % l, [E, NFG, 128, 8, 256]),
            wd=din("wd%d" % l, [E, NFG, 128, 2, 1024]))
        if moe[l]:
            LW[l]["router"] = din("router%d" % l, [128, 8, 8])
    tok_y = Tok("y")
    tok_xl1 = Tok("xl1")
    tok_xm = Tok("xm")

    uid = [0]

    def sb(st, name, shape, dt):
        uid[0] += 1
        b = Buf(st.enter_context(nc.sbuf_tensor("%s_u%d" % (name, uid[0]), list(shape), dt)), name)
        if not hasattr(st, "_bufs"):
            st._bufs = []
        st._bufs.append(b)
        return b

    def dve(fn, R, W):
        return P.op("dve", fn, R, W)

    def act(fn, R, W):
        return P.op("act", fn, R, W)

    def mm(out, lhsT, rhs, start, stop, R, W, inc):
        return P.op("pe", lambda: T.matmul(out, lhsT, rhs, start=start, stop=stop), R, W, inc)

    for i in range(6):
        P.banks.append(Buf(es.enter_context(nc.psum_tensor("pb%d" % i, [128, 512], F32))))
    for i in range(2):
        P.bbanks.append(Buf(es.enter_context(nc.psum_tensor("pbb%d" % i, [128, 1024], BF16))))
    P.dummy = sb(es, "dummy", [128, 4], F32)
    CON = sb(es, "CON", [128, K_END], F32)
    IDB = sb(es, "IDB", [128, 128], BF16)
    XT = sb(es, "XT", [128, 8, NT], BF16)
    OG = sb(es, "OG", [64, 8, NT], BF16)
    UB = sb(es, "UB", [128, 2, NT], BF16)
    UC = sb(es, "UC", [128, 2, NT], BF16)
    UH = sb(es, "UH", [128, 2, 32 + NT], F32)
    GH = sb(es, "GH", [128, 2, 32 + NT], F32)
    VEC = sb(es, "VEC", [128, NVEC], F32)
    OMM = sb(es, "OMM", [128, 14], F32)
    OMKA = sb(es, "OMKA", [128, 4], F32)
    V64 = sb(es, "V64", [64, 16], F32)
    W2A2 = sb(es, "W2A2", [128, 512], BF16)
    G2 = sb(es, "G2", [128, 512], BF16)
    TAILS = sb(es, "TAILS", [128, 14], F32)
    SRING = [sb(es, "SR%d" % i, [64, 8, 64], F32) for i in range(3)]
    GATE = sb(es, "GATE", [128, NT // 128, 8], F32)
    ROUT = sb(es, "ROUT", [128, 8, 8], F32)
    sci = [0]

    P.dma("sp", CON.t[:], consts_d.ap(), W=[CON.k])
    dve(lambda: V.tensor_copy(IDB.t[:], CON.t[:, K_ID:K_ID + 128]), [CON.k], [IDB.k])
    ID = CON.t[:, K_ID:K_ID + 128]

    def cview(lo, hi, rows=128):
        return CON.t[0:rows, lo:hi]

    def phase_X(src, p):
        with ExitStack() as ph:
            xs = [sb(ph, "xs%d" % i, [128, D], F32) for i in range(2)]
            for i in range(NT // 128):
                xb = xs[i % 2]
                r0 = p * NT + i * 128
                P.dma("sp", xb.t[:], src[r0:r0 + 128, :], W=[xb.k])
                for hf in range(2):
                    bk = P.bank()
                    for q in range(4):
                        kc = hf * 4 + q
                        P.op("pe", lambda: T.transpose(bk.t[:, q * 128:(q + 1) * 128], xb.t[:, kc * 128:(kc + 1) * 128], ID),
                             [xb.k, CON.k], [bk.k], inc=(q == 3))
                    o = XT.t[:, hf * 4:hf * 4 + 4, i * 128:(i + 1) * 128]
                    iv = bk.t[:].rearrange("p (q t) -> p q t", q=4)
                    if hf == 0:
                        act(lambda: A.copy(o, iv), [bk.k], [XT.k])
                    else:
                        dve(lambda: V.tensor_copy(o, iv), [bk.k], [XT.k])
            P.barrier()
            P.release(ph)

    def phase_R(l, p):
        w = LW[l]
        with ExitStack() as ph:
            WA = sb(ph, "WA", [128, 14, 8, 128], BF16)
            for g0 in range(0, 14, 7):
                P.dma("pool", WA.t[:, g0:g0 + 7], w["winA"].ap()[g0:g0 + 7].rearrange("g p k c -> p g k c"),
                      W=[WA.k], part=True)
            sl = {n: sb(ph, "R_" + n, [128, 4, TG], F32) for n in ("r", "k", "v", "A", "S", "T1", "C", "T2", "BV")}
            r_, k_, v_, A_, S_, T1, C_, T2, BV = (sl[n] for n in ("r", "k", "v", "A", "S", "T1", "C", "T2", "BV"))
            DG = sb(ph, "DG", [128, 4, 2, 64], F32)
            GC = sb(ph, "GC", [128, 4, 2], F32)
            PWPA = sb(ph, "PWPA", [128, TG], F32)
            PG = sb(ph, "PG", [128, TG], F32)
            MX = [sb(ph, "MX%d" % i, [128, TG], F32) for i in range(2)]
            TP = sb(ph, "TP", [128, TG], BF16)
            SGB = sb(ph, "SGB", [128, TG], BF16)
            AR = sb(ph, "AR", [128, 4, 2, 2, 64], BF16)
            BK = sb(ph, "BK", [128, 4, 2, 2, 64], BF16)
            VB = sb(ph, "VB", [128, 4, TG], BF16)
            BH = sb(ph, "BH", [128, 4, TG], BF16)
            KH = sb(ph, "KH", [128, 4, TG], BF16)
            VTM = sb(ph, "VTM", [64, 2, 512], BF16)
            BHTM = sb(ph, "BHTM", [64, 2, 512], BF16)
            KHTM = sb(ph, "KHTM", [64, 2, 512], BF16)
            AVA = sb(ph, "AVA", [64, 2, 8, 128], BF16)
            ATA = [sb(ph, "ATA%d" % i, [64, 8, 128], BF16) for i in range(2)]
            ATB = [sb(ph, "ATB%d" % i, [64, 8, 128], BF16) for i in range(2)]
            XXc = [[sb(ph, "XX%d_%d" % (c, i), [64, 8, 64], BF16) for i in range(2)] for c in range(2)]
            YYc = [[sb(ph, "YY%d_%d" % (c, i), [64, 8, 64], BF16) for i in range(2)] for c in range(2)]
            ZZc = [sb(ph, "ZZ%d" % c, [64, 8, 64], BF16) for c in range(2)]
            UW = [sb(ph, "UW%d" % i, [64, 8, 128], BF16) for i in range(2)]
            GT = [sb(ph, "GT%d" % i, [64, 8, 64], F32) for i in range(2)]
            HH = [sb(ph, "HH%d" % i, [64, 8, 64], F32) for i in range(2)]
            OLOC = sb(ph, "OLOC", [64, 8, TG], F32)
            QT = sb(ph, "QT", [64, 8, TG], F32)
            BV64 = sb(ph, "BV64", [64, 8, TG], F32)
            G64 = sb(ph, "G64", [64, 8, TG], F32)
            DN = sb(ph, "DN", [64, 8, TG], F32)
            SQ = sb(ph, "SQ", [64, 8, TG], F32)

            def vb(c0, n=TG):
                return VEC.t[:, c0:c0 + 4].unsqueeze(2).broadcast_to([128, 4, n])

            def cv(b):
                return b.t[:].rearrange("p j (c t) -> p j c t", t=64)

            def fl(b):
                return b.t[:].rearrange("p j t -> p (j t)")

            MKA = cview(K_MKA, K_MKA + 128, 64)
            ML = cview(K_ML, K_ML + 64, 64)
            ONB = cview(K_OB, K_OB + 128)
            ON64 = CON.t[0:64, K_OB:K_OB + 64]
            I2 = cview(K_I2, K_I2 + 64)
            SMK = cview(K_SM, K_SM + 512)

            for tg in range(NT // TG):
                c0 = tg * TG
                for g in range(14):
                    bk = P.bank()
                    for kc in range(8):
                        mm(bk.t[:, 0:TG], WA.t[:, g, kc, :], XT.t[:, kc, c0:c0 + TG], kc == 0, kc == 7,
                           [WA.k, XT.k], [bk.k], kc == 7)
                    if g < 4:
                        dst, dk = r_.t[:, g, :], r_.k
                    elif g < 8:
                        dst, dk = k_.t[:, g - 4, :], k_.k
                    elif g < 12:
                        dst, dk = v_.t[:, g - 8, :], v_.k
                    elif g == 12:
                        dst, dk = PWPA.t[:], PWPA.k
                    else:
                        dst, dk = PG.t[:], PG.k
                    m_ = MX[g % 2]
                    act(lambda: A.activation(out=m_.t[:], in_=bk.t[:, 0:TG], func=AF.Copy, scale=OMM.t[:, g:g + 1]),
                        [bk.k, OMM.k], [m_.k])
                    dve(lambda: V.scalar_tensor_tensor(dst[:, 1:TG], bk.t[:, 0:TG - 1], VEC.t[:, g:g + 1],
                                                       m_.t[:, 1:TG], ALU.mult, ALU.add),
                        [bk.k, VEC.k, m_.k], [dk])
                    dve(lambda: V.scalar_tensor_tensor(dst[:, 0:1], TAILS.t[:, g:g + 1], VEC.t[:, g:g + 1],
                                                       m_.t[:, 0:1], ALU.mult, ALU.add),
                        [TAILS.k, VEC.k, m_.k], [dk])
                    dve(lambda: V.tensor_copy(TAILS.t[:, g:g + 1], bk.t[:, TG - 1:TG]), [bk.k], [TAILS.k])
                if rstop == 1:
                    P.barrier()
                    P.release(ph)
                    return
                act(lambda: A.activation(out=TP.t[0:64, :], in_=PWPA.t[0:64, :], func=AF.Tanh), [PWPA.k], [TP.k])
                dve(lambda: V.tensor_copy(TP.t[64:128, :], PWPA.t[64:128, :]), [PWPA.k], [TP.k])
                act(lambda: A.activation(out=SGB.t[:], in_=PG.t[:], func=AF.Sigmoid), [PG.k], [SGB.k])
                for j in range(4):
                    bk = P.bank()
                    mm(bk.t[:, 0:TG], W2A2.t[0:64, j * 128:(j + 1) * 128], TP.t[0:64, :], True, True,
                       [W2A2.k, TP.k], [bk.k], True)
                    act(lambda: A.activation(out=S_.t[:, j, :], in_=bk.t[:, 0:TG], func=AF.Sigmoid,
                                             bias=VEC.t[:, 14 + j:15 + j]), [bk.k, VEC.k], [S_.k])
                    bk2 = P.bank()
                    mm(bk2.t[:, 0:TG], W2A2.t[64:128, j * 128:(j + 1) * 128], TP.t[64:128, :], True, True,
                       [W2A2.k, TP.k], [bk2.k], True)
                    act(lambda: A.activation(out=A_.t[:, j, :], in_=bk2.t[:, 0:TG], func=AF.Sigmoid,
                                             bias=VEC.t[:, 18 + j:19 + j]), [bk2.k, VEC.k], [A_.k])
                if rstop == 2:
                    P.barrier()
                    P.release(ph)
                    return
                dve(lambda: V.tensor_tensor(T1.t[:], k_.t[:], vb(22), ALU.mult), [k_.k, VEC.k], [T1.k])
                act(lambda: A.activation(out=T2.t[:], in_=T1.t[:], func=AF.Square), [T1.k], [T2.k])
                bk = P.bank()
                mm(bk.t[:, 0:512], ONB, fl(T2), True, True, [CON.k, T2.k], [bk.k], True)
                act(lambda: A.activation(out=fl(T2), in_=bk.t[:, 0:512], func=AF.Ln, bias=1e-24, scale=1.0),
                    [bk.k], [T2.k])
                act(lambda: A.activation(out=T2.t[:], in_=T2.t[:], func=AF.Exp, scale=-0.5), [T2.k], [T2.k])
                dve(lambda: V.tensor_tensor(T1.t[:], T1.t[:], T2.t[:], ALU.mult), [T1.k, T2.k], [T1.k])
                if rstop == 3:
                    P.barrier()
                    P.release(ph)
                    return
                dve(lambda: V.tensor_tensor_scan(fl(C_), SMK, fl(S_), 0.0, ALU.mult, ALU.add), [CON.k, S_.k], [C_.k])
                act(lambda: A.activation(out=T2.t[:], in_=C_.t[:], func=AF.Exp, scale=-C0), [C_.k], [T2.k])
                dve(lambda: V.tensor_tensor(AR.t[:, :, :, 1, :], cv(r_), cv(T2), ALU.mult), [r_.k, T2.k], [AR.k])
                dve(lambda: V.tensor_tensor(T2.t[:], C_.t[:], S_.t[:], ALU.subtract), [C_.k, S_.k], [T2.k])
                act(lambda: A.activation(out=T2.t[:], in_=T2.t[:], func=AF.Exp, scale=-C0), [T2.k], [T2.k])
                dve(lambda: V.scalar_tensor_tensor(AR.t[:, :, :, 0, :], cv(T1), -1.0, cv(T2), ALU.mult, ALU.mult),
                    [T1.k, T2.k], [AR.k])
                act(lambda: A.activation(out=T2.t[:], in_=C_.t[:], func=AF.Exp, scale=C0), [C_.k], [T2.k])
                dve(lambda: V.tensor_tensor(S_.t[:], T1.t[:], A_.t[:], ALU.mult), [T1.k, A_.k], [S_.k])
                dve(lambda: V.tensor_tensor(BK.t[:, :, :, 0, :], cv(S_), cv(T2), ALU.mult), [S_.k, T2.k], [BK.k])
                for j in range(4):
                    dve(lambda: V.tensor_scalar(A_.t[:, j, :], A_.t[:, j, :], VEC.t[:, 26 + j:27 + j],
                                                OMKA.t[:, j:j + 1], ALU.mult, ALU.add), [A_.k, VEC.k, OMKA.k], [A_.k])
                dve(lambda: V.tensor_tensor(k_.t[:], k_.t[:], A_.t[:], ALU.mult), [k_.k, A_.k], [k_.k])
                dve(lambda: V.tensor_tensor(BK.t[:, :, :, 1, :], cv(k_), cv(T2), ALU.mult), [k_.k, T2.k], [BK.k])
                clast = cv(C_)[:, :, :, 63:64]
                dve(lambda: V.tensor_tensor(cv(T2), clast.broadcast_to([128, 4, 2, 64]), cv(C_), ALU.subtract),
                    [C_.k], [T2.k])
                act(lambda: A.activation(out=T2.t[:], in_=T2.t[:], func=AF.Exp, scale=-C0), [T2.k], [T2.k])
                dve(lambda: V.tensor_tensor(BH.t[:], S_.t[:], T2.t[:], ALU.mult), [S_.k, T2.k], [BH.k])
                dve(lambda: V.tensor_tensor(KH.t[:], k_.t[:], T2.t[:], ALU.mult), [k_.k, T2.k], [KH.k])
                act(lambda: A.activation(out=GC.t[:], in_=cv(C_)[:, :, :, 63], func=AF.Exp, scale=-C0), [C_.k], [GC.k])
                dve(lambda: V.tensor_tensor(DG.t[:], I2.unsqueeze(1).unsqueeze(1).broadcast_to([128, 4, 2, 64]),
                                            GC.t[:].unsqueeze(3).broadcast_to([128, 4, 2, 64]), ALU.mult),
                    [CON.k, GC.k], [DG.k])
                if rstop == 4:
                    P.barrier()
                    P.release(ph)
                    return
                dve(lambda: V.tensor_tensor(T2.t[:], r_.t[:], k_.t[:], ALU.mult), [r_.k, k_.k], [T2.k])
                dve(lambda: V.tensor_tensor(T2.t[:], T2.t[:], vb(30), ALU.mult), [T2.k, VEC.k], [T2.k])
                bk = P.bank()
                mm(bk.t[:, 0:512], ONB, fl(T2), True, True, [CON.k, T2.k], [bk.k], True)
                dve(lambda: V.tensor_tensor(fl(BV), bk.t[:, 0:512], fl(v_), ALU.mult), [bk.k, v_.k], [BV.k])
                act(lambda: A.copy(VB.t[:], v_.t[:]), [v_.k], [VB.k])
                for (srcb, dstb, kind) in ((BV, BV64, 0), (SGB, G64, 1)):
                    bks = [P.bank(), P.bank()]
                    for h in range(8):
                        j, hp = h // 2, h % 2
                        rows = slice(64 * hp, 64 * hp + 64)
                        bkx = bks[h // 4]
                        o = bkx.t[0:64, (h % 4) * 128:(h % 4 + 1) * 128]
                        if kind == 0:
                            mm(o, CON.t[:, K_ID + 64 * hp:K_ID + 64 * hp + 64], BV.t[:, j, :], True, True,
                               [CON.k, BV.k], [bkx.k], h % 4 == 3)
                        else:
                            mm(o, G2.t[:, h * 64:(h + 1) * 64], SGB.t[:], True, True, [G2.k, SGB.k], [bkx.k], h % 4 == 3)
                    for q in range(2):
                        o = dstb.t[:, q * 4:q * 4 + 4, :]
                        iv = bks[q].t[0:64, :].rearrange("p (h t) -> p h t", h=4)
                        if q == 0:
                            act(lambda: A.copy(o, iv), [bks[q].k], [dstb.k])
                        else:
                            dve(lambda: V.tensor_copy(o, iv), [bks[q].k], [dstb.k])
                if rstop == 5:
                    P.barrier()
                    P.release(ph)
                    return
                for c in range(2):
                    for si, (srcb, dstb) in enumerate(((VB, VTM), (BH, BHTM), (KH, KHTM), (AR, AVA))):
                        bb = P.bbank()
                        for j in range(4):
                            if srcb is AR:
                                iv = AR.t[:, j, c, 0, :]
                            else:
                                iv = srcb.t[:, j, c * 64:(c + 1) * 64]
                            P.op("pe", lambda: T.transpose(bb.t[0:64, j * 128:(j + 1) * 128], iv, IDB.t[:]),
                                 [srcb.k, IDB.k], [bb.k], inc=(j == 3))
                        if srcb is AR:
                            o = AVA.t[:, c, :, 64:128]
                            iv2 = bb.t[0:64, 0:512].rearrange("p (h t) -> p h t", h=8)
                        else:
                            o = dstb.t[:, c, :]
                            iv2 = bb.t[0:64, 0:512]
                        if si % 2 == 0:
                            act(lambda: A.copy(o, iv2), [bb.k], [dstb.k])
                        else:
                            dve(lambda: V.tensor_copy(o, iv2), [bb.k], [dstb.k])
                if rstop == 6:
                    P.barrier()
                    P.release(ph)
                    return
                CS = (0, 1)
                for c in CS:
                    ata, atb = ATA[c], ATB[c]
                    for which, dstb in ((0, ata), (1, atb)):
                        bks = [P.bank(), P.bank()]
                        for h in range(8):
                            j, hp = h // 2, h % 2
                            rows = slice(64 * hp, 64 * hp + 64)
                            bkx = bks[hp]
                            mm(bkx.t[0:64, j * 128:(j + 1) * 128], BK.t[rows, j, c, which, :],
                               AR.t[rows, j, c, :, :], True, True, [BK.k, AR.k], [bkx.k], h >= 6)
                        for q in range(2):
                            iv = bks[q].t[0:64, :].rearrange("p (h t) -> p h t", h=4)
                            ov = dstb.t[:].rearrange("p (j hp) n -> p hp j n", hp=2)[:, q]
                            dve(lambda: V.tensor_tensor(ov, iv, MKA.unsqueeze(1).broadcast_to([64, 4, 128]), ALU.mult),
                                [bks[q].k, CON.k], [dstb.k])
                            if which == 0:
                                ox = XXc[c][0].t[:].rearrange("p (j hp) n -> p hp j n", hp=2)[:, q]
                                dve(lambda: V.tensor_tensor(ox, iv[:, :, 0:64],
                                                            MKA[:, 0:64].unsqueeze(1).broadcast_to([64, 4, 64]), ALU.mult),
                                    [bks[q].k, CON.k], [XXc[c][0].k])
                    bks = [P.bank(), P.bank()]
                    for h in range(8):
                        j, hp = h // 2, h % 2
                        rows = slice(64 * hp, 64 * hp + 64)
                        mm(bks[hp].t[0:64, j * 64:(j + 1) * 64], AR.t[rows, j, c, 0, :], BK.t[rows, j, c, 0, :], True, True,
                           [AR.k, BK.k], [bks[hp].k], h >= 6)
                    for q in range(2):
                        oy = YYc[c][0].t[:].rearrange("p (j hp) n -> p hp j n", hp=2)[:, q]
                        dve(lambda: V.tensor_tensor(oy, bks[q].t[0:64, 0:256].rearrange("p (h t) -> p h t", h=4),
                                                    ML.unsqueeze(1).broadcast_to([64, 4, 64]), ALU.mult),
                            [bks[q].k, CON.k], [YYc[c][0].k])
                    dve(lambda: V.tensor_tensor(ZZc[c].t[:], XXc[c][0].t[:],
                                                CON.t[0:64, K_ID:K_ID + 64].unsqueeze(1).broadcast_to([64, 8, 64]), ALU.add),
                        [XXc[c][0].k, CON.k], [ZZc[c].k])
                for lvl in range(5):
                    pxs, pys = {}, {}
                    for c in CS:
                        xc, yc = XXc[c][lvl % 2], YYc[c][lvl % 2]
                        if lvl < 4:
                            px = P.bank()
                            for h in range(8):
                                mm(px.t[0:64, h * 64:(h + 1) * 64], yc.t[:, h, :], xc.t[:, h, :], True, True,
                                   [yc.k, xc.k], [px.k], h == 7)
                            pxs[c] = px
                        py = P.bank()
                        for h in range(8):
                            mm(py.t[0:64, h * 64:(h + 1) * 64], xc.t[:, h, :], yc.t[:, h, :], True, True,
                               [yc.k, xc.k], [py.k], h == 7)
                        pys[c] = py
                    for c in CS:
                        xn, yn = XXc[c][(lvl + 1) % 2], YYc[c][(lvl + 1) % 2]
                        if lvl < 4:
                            px = pxs[c]
                            act(lambda: A.copy(xn.t[:], px.t[0:64, :].rearrange("p (h t) -> p h t", h=8)), [px.k], [xn.k])
                        py = pys[c]
                        act(lambda: A.copy(yn.t[:], py.t[0:64, :].rearrange("p (h t) -> p h t", h=8)), [py.k], [yn.k])
                    pzs = {}
                    for c in CS:
                        yn = YYc[c][(lvl + 1) % 2]
                        pz = P.bank()
                        for h in range(8):
                            mm(pz.t[0:64, h * 64:(h + 1) * 64], yn.t[:, h, :], ZZc[c].t[:, h, :], True, True,
                               [yn.k, ZZc[c].k], [pz.k], h == 7)
                        pzs[c] = pz
                    for c in CS:
                        pz = pzs[c]
                        dve(lambda: V.tensor_tensor(ZZc[c].t[:], pz.t[0:64, :].rearrange("p (h t) -> p h t", h=8), ZZc[c].t[:], ALU.add),
                            [pz.k, ZZc[c].k], [ZZc[c].k])
                pvs = {}
                for c in CS:
                    pv = P.bank()
                    for h in range(8):
                        mm(pv.t[0:64, h * 64:(h + 1) * 64], ATB[c].t[:, h, 0:64], VTM.t[:, c, h * 64:(h + 1) * 64], True, True,
                           [ATB[c].k, VTM.k], [pv.k], h == 7)
                    pvs[c] = pv
                for c in CS:
                    pv = pvs[c]
                    if c == 0:
                        act(lambda: A.copy(AVA.t[:, c, :, 0:64], pv.t[0:64, :].rearrange("p (h t) -> p h t", h=8)), [pv.k], [AVA.k])
                    else:
                        dve(lambda: V.tensor_copy(AVA.t[:, c, :, 0:64], pv.t[0:64, :].rearrange("p (h t) -> p h t", h=8)), [pv.k], [AVA.k])
                for c in CS:
                    tt, uw = ZZc[c], UW[c]
                    bks = [P.bank(), P.bank()]
                    for h in range(8):
                        bkx = bks[h // 4]
                        mm(bkx.t[0:64, (h % 4) * 128:(h % 4 + 1) * 128], tt.t[:, h, :], AVA.t[:, c, h, :], True, True,
                           [tt.k, AVA.k], [bkx.k], h % 4 == 3)
                    act(lambda: A.copy(uw.t[:, 0:4, :], bks[0].t[0:64, :].rearrange("p (h t) -> p h t", h=4)), [bks[0].k], [uw.k])
                    dve(lambda: V.tensor_copy(uw.t[:, 4:8, :], bks[1].t[0:64, :].rearrange("p (h t) -> p h t", h=4)), [bks[1].k], [uw.k])
                for c in CS:
                    ata, atb, uw, gt, hh = ATA[c], ATB[c], UW[c], GT[c], HH[c]
                    po = P.bank()
                    for h in range(8):
                        o = po.t[0:64, h * 64:(h + 1) * 64]
                        mm(o, uw.t[:, h, 0:64], ata.t[:, h, 64:128], True, False, [uw.k, ata.k], [po.k], False)
                        mm(o, VTM.t[:, c, h * 64:(h + 1) * 64], atb.t[:, h, 64:128], False, True, [VTM.k, atb.k], [po.k], h == 7)
                    act(lambda: A.copy(OLOC.t[:, :, c * 64:(c + 1) * 64], po.t[0:64, :].rearrange("p (h t) -> p h t", h=8)),
                        [po.k], [OLOC.k])
                    pq = P.bank()
                    for h in range(8):
                        j, hp = h // 2, h % 2
                        o = pq.t[0:64, h * 64:(h + 1) * 64]
                        mm(o, uw.t[:, h, 64:128], ata.t[:, h, 64:128], True, False, [uw.k, ata.k], [pq.k], False)
                        mm(o, IDB.t[:, 64 * hp:64 * hp + 64], AR.t[:, j, c, 1, :], False, True, [IDB.k, AR.k], [pq.k], h == 7)
                    dve(lambda: V.tensor_copy(QT.t[:, :, c * 64:(c + 1) * 64], pq.t[0:64, :].rearrange("p (h t) -> p h t", h=8)),
                        [pq.k], [QT.k])
                    pg_ = P.bank()
                    for h in range(8):
                        j, hp = h // 2, h % 2
                        o = pg_.t[0:64, h * 64:(h + 1) * 64]
                        mm(o, uw.t[:, h, 64:128], BHTM.t[:, c, h * 64:(h + 1) * 64], True, False, [uw.k, BHTM.k], [pg_.k], False)
                        mm(o, CON.t[:, K_ID + 64 * hp:K_ID + 64 * hp + 64], DG.t[:, j, c, :], False, True,
                           [CON.k, DG.k], [pg_.k], h == 7)
                    act(lambda: A.copy(gt.t[:], pg_.t[0:64, :].rearrange("p (h t) -> p h t", h=8)), [pg_.k], [gt.k])
                    phh = P.bank()
                    for h in range(8):
                        o = phh.t[0:64, h * 64:(h + 1) * 64]
                        mm(o, BHTM.t[:, c, h * 64:(h + 1) * 64], uw.t[:, h, 0:64], True, False, [BHTM.k, uw.k], [phh.k], False)
                        mm(o, KHTM.t[:, c, h * 64:(h + 1) * 64], VTM.t[:, c, h * 64:(h + 1) * 64], False, True,
                           [KHTM.k, VTM.k], [phh.k], h == 7)
                    dve(lambda: V.tensor_copy(hh.t[:], phh.t[0:64, :].rearrange("p (h t) -> p h t", h=8)), [phh.k], [hh.k])
                for c in CS:
                    gt, hh = GT[c], HH[c]
                    scur = SRING[sci[0] % 3]
                    snext = SRING[(sci[0] + 1) % 3]
                    sci[0] += 1
                    pO = P.bank()
                    for h in range(8):
                        mm(pO.t[0:64, h * 64:(h + 1) * 64], scur.t[:, h, :], QT.t[:, h, c * 64:(c + 1) * 64], True, True,
                           [scur.k, QT.k], [pO.k], h == 7)
                    dve(lambda: V.tensor_tensor(OLOC.t[:, :, c * 64:(c + 1) * 64],
                                                pO.t[0:64, :].rearrange("p (h t) -> p h t", h=8),
                                                OLOC.t[:, :, c * 64:(c + 1) * 64], ALU.add), [pO.k, OLOC.k], [OLOC.k])
                    pS = P.bank()
                    for h in range(8):
                        mm(pS.t[0:64, h * 64:(h + 1) * 64], gt.t[:, h, :], scur.t[:, h, :], True, True,
                           [gt.k, scur.k], [pS.k], h == 7)
                    dve(lambda: V.tensor_tensor(snext.t[:], pS.t[0:64, :].rearrange("p (h t) -> p h t", h=8), hh.t[:], ALU.add),
                        [pS.k, hh.k], [snext.k])
                ofl = OLOC.t[:].rearrange("p h t -> p (h t)")
                dfl = DN.t[:].rearrange("p h t -> p (h t)")
                sfl = SQ.t[:].rearrange("p h t -> p (h t)")
                for q in range(2):
                    bk = P.bank()
                    mm(bk.t[0:64, :], ON64, ofl[:, q * 512:(q + 1) * 512], True, True, [CON.k, OLOC.k], [bk.k], True)
                    dve(lambda: V.scalar_tensor_tensor(dfl[:, q * 512:(q + 1) * 512], bk.t[0:64, :], -1.0 / 64,
                                                       ofl[:, q * 512:(q + 1) * 512], ALU.mult, ALU.add),
                        [bk.k, OLOC.k], [DN.k])
                act(lambda: A.activation(out=SQ.t[:], in_=DN.t[:], func=AF.Square), [DN.k], [SQ.k])
                for q in range(2):
                    bk = P.bank()
                    mm(bk.t[0:64, :], ON64, sfl[:, q * 512:(q + 1) * 512], True, True, [CON.k, SQ.k], [bk.k], True)
                    act(lambda: A.activation(out=sfl[:, q * 512:(q + 1) * 512], in_=bk.t[0:64, :], func=AF.Ln,
                                             bias=GN_EPS, scale=1.0 / 64), [bk.k], [SQ.k])
                act(lambda: A.activation(out=SQ.t[:], in_=SQ.t[:], func=AF.Exp, scale=-0.5), [SQ.k], [SQ.k])
                dve(lambda: V.tensor_tensor(DN.t[:], DN.t[:], SQ.t[:], ALU.mult), [DN.k, SQ.k], [DN.k])
                dve(lambda: V.tensor_tensor(DN.t[:], DN.t[:], V64.t[:, 0:8].unsqueeze(2).broadcast_to([64, 8, TG]), ALU.mult),
                    [DN.k, V64.k], [DN.k])
                dve(lambda: V.tensor_tensor(DN.t[:], DN.t[:], V64.t[:, 8:16].unsqueeze(2).broadcast_to([64, 8, TG]), ALU.add),
                    [DN.k, V64.k], [DN.k])
                dve(lambda: V.tensor_tensor(DN.t[:], DN.t[:], BV64.t[:], ALU.add), [DN.k, BV64.k], [DN.k])
                dve(lambda: V.tensor_tensor(OG.t[:, :, c0:c0 + TG], DN.t[:], G64.t[:], ALU.mult), [DN.k, G64.k], [OG.k])
            P.barrier()
            P.release(ph)

    def phase_BC(l, p):
        w = LW[l]
        with ExitStack() as ph:
            WB = sb(ph, "WB", [128, 10, 8, 128], BF16)
            P.dma("pool", WB.t[:], w["winB"].ap()[0:10].rearrange("g p k c -> p g k c"), W=[WB.k])
            TMP = [sb(ph, "bcT%d" % i, [128, 512], F32) for i in range(2)]
            ACC = sb(ph, "bcACC", [128, 2, NT], F32)
            ACCc = [Tok("acc0"), Tok("acc1")]
            DD = sb(ph, "bcD", [128, 2, 512], F32)
            GBB = sb(ph, "bcGB", [128, 2, NT], F32)
            ONF = cview(K_OF, K_OF + 128)
            ntg = NT // 512

            def proj(g, tg):
                bk = P.bank()
                for kc in range(8):
                    mm(bk.t[:], WB.t[:, g, kc, :], XT.t[:, kc, tg * 512:(tg + 1) * 512], kc == 0, kc == 7,
                       [WB.k, XT.k], [bk.k], kc == 7)
                return bk

            for tg in range(ntg):
                for c in range(2):
                    bu = proj(c, tg)
                    bg = proj(2 + c, tg)
                    tm = TMP[c]
                    act(lambda: A.activation(out=tm.t[:], in_=bg.t[:], func=AF.Sigmoid), [bg.k], [tm.k])
                    dve(lambda: V.tensor_tensor(UH.t[:, c, 32 + tg * 512:32 + (tg + 1) * 512], bu.t[:], tm.t[:], ALU.mult),
                        [bu.k, tm.k], [UH.k])
            for c in range(2):
                dve(lambda: V.tensor_scalar(ACC.t[:, c, :], UH.t[:, c, 2:2 + NT], VEC.t[:, 34 + c * 31:35 + c * 31],
                                            VEC.t[:, 96 + c:97 + c], ALU.mult, ALU.add), [UH.k, VEC.k], [ACCc[c]])
            for jj in range(1, 31):
                for c in range(2):
                    dve(lambda: V.scalar_tensor_tensor(ACC.t[:, c, :], UH.t[:, c, 2 + jj:2 + jj + NT],
                                                       VEC.t[:, 34 + c * 31 + jj:35 + c * 31 + jj], ACC.t[:, c, :],
                                                       ALU.mult, ALU.add), [UH.k, VEC.k, ACCc[c]], [ACCc[c]])
            dve(lambda: V.tensor_copy(ACC.t[:, 0, 0:1], ACC.t[:, 0, 0:1]), [ACCc[0], ACCc[1]], [ACC.k, ACCc[0], ACCc[1]])
            dve(lambda: V.tensor_copy(UH.t[:, :, 0:32], UH.t[:, :, NT:NT + 32]), [UH.k], [UH.k])
            for tg in range(ntg):
                ts_ = slice(tg * 512, (tg + 1) * 512)
                bk = P.bank()
                for c in range(2):
                    mm(bk.t[:], ONF, ACC.t[:, c, ts_], c == 0, c == 1, [CON.k, ACC.k], [bk.k], c == 1)
                for c in range(2):
                    dve(lambda: V.scalar_tensor_tensor(DD.t[:, c, :], bk.t[:], -1.0 / 256, ACC.t[:, c, ts_], ALU.mult, ALU.add),
                        [bk.k, ACC.k], [DD.k])
                    act(lambda: A.activation(out=ACC.t[:, c, ts_], in_=DD.t[:, c, :], func=AF.Square), [DD.k], [ACC.k])
                bk2 = P.bank()
                for c in range(2):
                    mm(bk2.t[:], ONF, ACC.t[:, c, ts_], c == 0, c == 1, [CON.k, ACC.k], [bk2.k], c == 1)
                tm = TMP[0]
                act(lambda: A.activation(out=tm.t[:], in_=bk2.t[:], func=AF.Sqrt, bias=LN_EPS, scale=1.0 / 256), [bk2.k], [tm.k])
                dve(lambda: V.reciprocal(tm.t[:], tm.t[:]), [tm.k], [tm.k])
                for c in range(2):
                    dve(lambda: V.tensor_tensor(DD.t[:, c, :], DD.t[:, c, :], tm.t[:], ALU.mult), [DD.k, tm.k], [DD.k])
                    dve(lambda: V.tensor_scalar(DD.t[:, c, :], DD.t[:, c, :], VEC.t[:, 98 + c:99 + c], VEC.t[:, 100 + c:101 + c],
                                                ALU.mult, ALU.add), [DD.k, VEC.k], [DD.k])
                    act(lambda: A.activation(out=UB.t[:, c, ts_], in_=DD.t[:, c, :], func=AF.Silu), [DD.k], [UB.k])
            for tg in range(ntg):
                for c in range(2):
                    bgb = proj(4 + c, tg)
                    act(lambda: A.copy(GBB.t[:, c, tg * 512:(tg + 1) * 512], bgb.t[:]), [bgb.k], [GBB.k])
                    bgc = proj(6 + c, tg)
                    bh = proj(8 + c, tg)
                    tm = TMP[c]
                    act(lambda: A.copy(tm.t[:], bh.t[:]), [bh.k], [tm.k])
                    dve(lambda: V.tensor_tensor(GH.t[:, c, 32 + tg * 512:32 + (tg + 1) * 512], bgc.t[:], tm.t[:], ALU.mult),
                        [bgc.k, tm.k], [GH.k])
            for c in range(2):
                dve(lambda: V.tensor_scalar(ACC.t[:, c, :], GH.t[:, c, 30:30 + NT], VEC.t[:, 102 + c * 3:103 + c * 3], None,
                                            ALU.mult), [GH.k, VEC.k], [ACC.k])
                for jj in range(1, 3):
                    dve(lambda: V.scalar_tensor_tensor(ACC.t[:, c, :], GH.t[:, c, 30 + jj:30 + jj + NT],
                                                       VEC.t[:, 102 + c * 3 + jj:103 + c * 3 + jj], ACC.t[:, c, :],
                                                       ALU.mult, ALU.add), [GH.k, VEC.k, ACC.k], [ACC.k])
                dve(lambda: V.tensor_tensor(UC.t[:, c, :], GBB.t[:, c, :], ACC.t[:, c, :], ALU.mult), [GBB.k, ACC.k], [UC.k])
            dve(lambda: V.tensor_copy(GH.t[:, :, 0:32], GH.t[:, :, NT:NT + 32]), [GH.k], [GH.k])
            P.barrier()
            P.release(ph)

    def phase_GO(l, p, src):
        w = LW[l]
        with ExitStack() as ph:
            MT = sb(ph, "MT", [128, 8, NT], BF16)
            with ExitStack() as ph2:
                WOA = sb(ph2, "WOA", [64, 8, 1024], BF16)
                WOB = sb(ph2, "WOB", [128, 2, 1024], BF16)
                WOC = sb(ph2, "WOC", [128, 2, 1024], BF16)
                P.dma("pool", WOA.t[:], w["woa"].ap(), W=[WOA.k])
                P.dma("pool", WOB.t[:], w["wob"].ap(), W=[WOB.k])
                P.dma("pool", WOC.t[:], w["woc"].ap(), W=[WOC.k])
                WG = [sb(ph2, "WGt%d" % i, [128, 3, 8, 128], BF16) for i in range(2)]
                GS = [sb(ph2, "GS%d" % i, [128, 512], F32) for i in range(3)]
                MA = sb(ph2, "MA", [128, 512], F32)
                MB = sb(ph2, "MB", [128, 512], F32)
                ntg = NT // 512
                for i in range(8):
                    wgt = WG[i % 2]
                    P.dma("pool", wgt.t[:], w["winB"].ap()[10 + 3 * i:13 + 3 * i].rearrange("g p k c -> p g k c"), W=[wgt.k])
                    for tg in range(ntg):
                        ts_ = slice(tg * 512, (tg + 1) * 512)
                        for br in range(3):
                            bk = P.bank()
                            for kc in range(8):
                                mm(bk.t[:], wgt.t[:, br, kc, :], XT.t[:, kc, ts_], kc == 0, kc == 7, [wgt.k, XT.k], [bk.k], kc == 7)
                            act(lambda: A.activation(out=GS[br].t[:], in_=bk.t[:], func=AF.Sigmoid), [bk.k], [GS[br].k])
                        ba = P.bank()
                        for h in range(8):
                            mm(ba.t[:], WOA.t[:, h, i * 128:(i + 1) * 128], OG.t[:, h, ts_], h == 0, h == 7, [WOA.k, OG.k], [ba.k], h == 7)
                        dve(lambda: V.tensor_tensor(MA.t[:], ba.t[:], GS[0].t[:], ALU.mult), [ba.k, GS[0].k], [MA.k])
                        bb_ = P.bank()
                        for c in range(2):
                            mm(bb_.t[:], WOB.t[:, c, i * 128:(i + 1) * 128], UB.t[:, c, ts_], c == 0, c == 1, [WOB.k, UB.k], [bb_.k], c == 1)
                        dve(lambda: V.tensor_tensor(MB.t[:], bb_.t[:], GS[1].t[:], ALU.mult), [bb_.k, GS[1].k], [MB.k])
                        dve(lambda: V.tensor_tensor(MA.t[:], MA.t[:], MB.t[:], ALU.add), [MA.k, MB.k], [MA.k])
                        bc = P.bank()
                        for c in range(2):
                            mm(bc.t[:], WOC.t[:, c, i * 128:(i + 1) * 128], UC.t[:, c, ts_], c == 0, c == 1, [WOC.k, UC.k], [bc.k], c == 1)
                        dve(lambda: V.tensor_tensor(MB.t[:], bc.t[:], GS[2].t[:], ALU.mult), [bc.k, GS[2].k], [MB.k])
                        dve(lambda: V.tensor_tensor(MT.t[:, i, ts_], MA.t[:], MB.t[:], ALU.add), [MA.k, MB.k], [MT.k])
                P.barrier()
                P.release(ph2)
            WOUT = sb(ph, "WOUT", [128, 8, 1024], BF16)
            P.dma("pool", WOUT.t[:], w["wout"].ap(), W=[WOUT.k])
            LNP = sb(ph, "LNP", [128, 2, 1024], F32)
            P.dma("sp", LNP.t[:], w["lnp"].ap()[:, 0:2, :], W=[LNP.k])
            XR = [sb(ph, "XR%d" % i, [128, D], F32) for i in range(2)]
            ZB = [sb(ph, "ZB%d" % i, [128, D], F32) for i in range(2)]
            ST = sb(ph, "ST", [128, 2, 6], F32)
            MV = sb(ph, "MV", [128, 4], F32)
            XTF = sb(ph, "XTF", [128, 8, 128], F32)
            LG = sb(ph, "LG", [128, 8], F32)
            LG2 = sb(ph, "LG2", [128, 8], F32)
            EQ1 = sb(ph, "EQ1", [128, 8], F32)
            EQ2 = sb(ph, "EQ2", [128, 8], F32)
            SM = sb(ph, "SMx", [128, 8], F32)
            def stage_a(i):
                r0 = p * NT + i * 128
                xr = XR[i % 2]
                zb = ZB[i % 2]
                P.dma("sp", xr.t[:], src[r0:r0 + 128, :], W=[xr.k])
                for hf in range(2):
                    bk = P.bank()
                    for kc in range(8):
                        mm(bk.t[:], MT.t[:, kc, i * 128:(i + 1) * 128], WOUT.t[:, kc, hf * 512:(hf + 1) * 512], kc == 0, kc == 7,
                           [MT.k, WOUT.k], [bk.k], kc == 7)
                    dve(lambda: V.scalar_tensor_tensor(zb.t[:, hf * 512:(hf + 1) * 512], xr.t[:, hf * 512:(hf + 1) * 512], ALPHA,
                                                       bk.t[:], ALU.mult, ALU.add), [xr.k, bk.k], [zb.k])
                layer_norm(zb, LNP, 0, ST, MV)
                P.dma("sp", xm.ap()[r0:r0 + 128, :], zb.t[:], R=[zb.k], W=[tok_xm], part=True, own=zb.k)

            stage_a(0)
            for i in range(NT // 128):
                if i + 1 < NT // 128:
                    stage_a(i + 1)
                zb = ZB[i % 2]
                for hf in range(2):
                    bk = P.bank()
                    for q in range(4):
                        kc = hf * 4 + q
                        P.op("pe", lambda: T.transpose(bk.t[:, q * 128:(q + 1) * 128], zb.t[:, kc * 128:(kc + 1) * 128], ID),
                             [zb.k, CON.k], [bk.k], inc=(q == 3))
                    iv = bk.t[:].rearrange("p (q t) -> p q t", q=4)
                    if moe[l]:
                        act(lambda: A.copy(XTF.t[:, hf * 4:hf * 4 + 4, :], iv), [bk.k], [XTF.k])
                        dve(lambda: V.tensor_copy(XT.t[:, hf * 4:hf * 4 + 4, i * 128:(i + 1) * 128], XTF.t[:, hf * 4:hf * 4 + 4, :]),
                            [XTF.k], [XT.k])
                    else:
                        act(lambda: A.copy(XT.t[:, hf * 4:hf * 4 + 4, i * 128:(i + 1) * 128], iv), [bk.k], [XT.k])
                if moe[l] and mdbg >= 2:
                    bk = P.bank()
                    for kc in range(8):
                        mm(bk.t[:, 0:8], XTF.t[:, kc, :], ROUT.t[:, kc, :], kc == 0, kc == 7, [XTF.k, ROUT.k], [bk.k], kc == 7)
                    dve(lambda: V.tensor_copy(LG.t[:], bk.t[:, 0:8]), [bk.k], [LG.k])
                if moe[l] and mdbg >= 3:
                    dve(lambda: V.tensor_reduce(SM.t[:, 0:1], LG.t[:], AX.X, ALU.max), [LG.k], [SM.k])
                    dve(lambda: V.tensor_scalar(EQ1.t[:], LG.t[:], SM.t[:, 0:1], None, ALU.is_equal), [LG.k, SM.k], [EQ1.k])
                    dve(lambda: V.scalar_tensor_tensor(LG2.t[:], EQ1.t[:], -1e30, LG.t[:], ALU.mult, ALU.add), [EQ1.k, LG.k], [LG2.k])
                    dve(lambda: V.tensor_reduce(SM.t[:, 1:2], LG2.t[:], AX.X, ALU.max), [LG2.k], [SM.k])
                    dve(lambda: V.tensor_scalar(EQ2.t[:], LG2.t[:], SM.t[:, 1:2], None, ALU.is_equal), [LG2.k, SM.k], [EQ2.k])
                    dve(lambda: V.tensor_tensor(SM.t[:, 2:3], SM.t[:, 1:2], SM.t[:, 0:1], ALU.subtract), [SM.k], [SM.k])
                    act(lambda: A.activation(out=SM.t[:, 3:4], in_=SM.t[:, 2:3], func=AF.Exp), [SM.k], [SM.k])
                    dve(lambda: V.tensor_scalar(SM.t[:, 4:5], SM.t[:, 3:4], 1.0, None, ALU.add), [SM.k], [SM.k])
                    dve(lambda: V.reciprocal(SM.t[:, 5:6], SM.t[:, 4:5]), [SM.k], [SM.k])
                    dve(lambda: V.tensor_tensor(SM.t[:, 6:7], SM.t[:, 3:4], SM.t[:, 5:6], ALU.mult), [SM.k], [SM.k])
                    dve(lambda: V.tensor_scalar(EQ1.t[:], EQ1.t[:], SM.t[:, 5:6], None, ALU.mult), [EQ1.k, SM.k], [EQ1.k])
                    dve(lambda: V.scalar_tensor_tensor(GATE.t[:, i, :], EQ2.t[:], SM.t[:, 6:7], EQ1.t[:], ALU.mult, ALU.add),
                        [EQ2.k, SM.k, EQ1.k], [GATE.k])
            P.barrier()
            P.release(ph)

    def layer_norm(zb, LNP, gi, ST, MV):
        for hf in range(2):
            dve(lambda: V.bn_stats(ST.t[:, hf, :], zb.t[:, hf * 512:(hf + 1) * 512]), [zb.k], [ST.k])
        dve(lambda: V.bn_aggr(MV.t[:, 0:2], ST.t[:].rearrange("p a b -> p (a b)")), [ST.k], [MV.k])
        act(lambda: A.activation(out=MV.t[:, 2:3], in_=MV.t[:, 1:2], func=AF.Sqrt, bias=LN_EPS, scale=1.0), [MV.k], [MV.k])
        dve(lambda: V.reciprocal(MV.t[:, 3:4], MV.t[:, 2:3]), [MV.k], [MV.k])
        dve(lambda: V.tensor_scalar(zb.t[:], zb.t[:], MV.t[:, 0:1], MV.t[:, 3:4], ALU.subtract, ALU.mult), [zb.k, MV.k], [zb.k])
        dve(lambda: V.tensor_tensor(zb.t[:], zb.t[:], LNP.t[:, gi, :], ALU.mult), [zb.k, LNP.k], [zb.k])
        dve(lambda: V.tensor_tensor(zb.t[:], zb.t[:], LNP.t[:, gi + 1, :], ALU.add), [zb.k, LNP.k], [zb.k])

    def phase_F(l, p, dst, tok_dst):
        w = LW[l]
        E = 8 if moe[l] else 1
        with ExitStack() as ph:
            ACC = sb(ph, "fACC", [128, NT // 128, D], F32)
            WGs = [sb(ph, "fWG%d" % i, [128, 8, 256], BF16) for i in range(2)]
            WUs = [sb(ph, "fWU%d" % i, [128, 8, 256], BF16) for i in range(2)]
            WDs = [sb(ph, "fWD%d" % i, [128, 2, 1024], BF16) for i in range(2)]
            HT = [sb(ph, "fHT%d" % i, [128, 2, 512], BF16) for i in range(2)]
            SGT = [sb(ph, "fSG%d" % i, [128, 512], F32) for i in range(2)]
            LNP = sb(ph, "fLNP", [128, 2, 1024], F32)
            P.dma("sp", LNP.t[:], w["lnp"].ap()[:, 2:4, :], W=[LNP.k])
            ST = sb(ph, "fST", [128, 2, 6], F32)
            MV = sb(ph, "fMV", [128, 4], F32)
            XR = [sb(ph, "fXR%d" % i, [128, D], F32) for i in range(2)]
            ntg = NT // 512
            it = 0
            for e in range(E):
                for g in range(NFG):
                    wg, wu, wd = WGs[it % 2], WUs[it % 2], WDs[it % 2]
                    P.dma("pool", wg.t[:], w["wg"].ap()[e, g], W=[wg.k])
                    P.dma("pool", wu.t[:], w["wu"].ap()[e, g], W=[wu.k])
                    P.dma("pool", wd.t[:], w["wd"].ap()[e, g], W=[wd.k])
                    for tg in range(ntg):
                        ts_ = slice(tg * 512, (tg + 1) * 512)
                        ht = HT[tg % 2]
                        for fc in range(2):
                            bg = P.bank()
                            for kc in range(8):
                                mm(bg.t[:], wg.t[:, kc, fc * 128:(fc + 1) * 128], XT.t[:, kc, ts_], kc == 0, kc == 7,
                                   [wg.k, XT.k], [bg.k], kc == 7)
                            bu = P.bank()
                            for kc in range(8):
                                mm(bu.t[:], wu.t[:, kc, fc * 128:(fc + 1) * 128], XT.t[:, kc, ts_], kc == 0, kc == 7,
                                   [wu.k, XT.k], [bu.k], kc == 7)
                            sg = SGT[fc]
                            act(lambda: A.activation(out=sg.t[:], in_=bg.t[:], func=AF.Silu), [bg.k], [sg.k])
                            dve(lambda: V.tensor_tensor(ht.t[:, fc, :], bu.t[:], sg.t[:], ALU.mult), [bu.k, sg.k], [ht.k])
                        for tt_ in range(4):
                            ti = tg * 4 + tt_
                            for hf in range(2):
                                bk = P.bank()
                                for fc in range(2):
                                    mm(bk.t[:], ht.t[:, fc, tt_ * 128:(tt_ + 1) * 128], wd.t[:, fc, hf * 512:(hf + 1) * 512],
                                       fc == 0, fc == 1, [ht.k, wd.k], [bk.k], fc == 1)
                                o = ACC.t[:, ti, hf * 512:(hf + 1) * 512]
                                if moe[l]:
                                    gsc = GATE.t[:, ti, e:e + 1]
                                    if it == 0:
                                        dve(lambda: V.tensor_scalar(o, bk.t[:], gsc, None, ALU.mult), [bk.k, GATE.k], [ACC.k])
                                    else:
                                        dve(lambda: V.scalar_tensor_tensor(o, bk.t[:], gsc, o, ALU.mult, ALU.add),
                                            [bk.k, GATE.k, ACC.k], [ACC.k])
                                else:
                                    if it == 0:
                                        act(lambda: A.copy(o, bk.t[:]), [bk.k], [ACC.k])
                                    else:
                                        dve(lambda: V.tensor_tensor(o, bk.t[:], o, ALU.add), [bk.k, ACC.k], [ACC.k])
                    it += 1
            for i in range(NT // 128):
                r0 = p * NT + i * 128
                xr = XR[i % 2]
                P.dma("sp", xr.t[:], xm.ap()[r0:r0 + 128, :], W=[xr.k])
                dve(lambda: V.scalar_tensor_tensor(xr.t[:], xr.t[:], ALPHA, ACC.t[:, i, :], ALU.mult, ALU.add), [xr.k, ACC.k], [xr.k])
                layer_norm(xr, LNP, 0, ST, MV)
                P.dma("sp", dst[r0:r0 + 128, :], xr.t[:], R=[xr.k], W=[tok_dst], part=True, own=xr.k)
            P.barrier()
            P.release(ph)

    for li, l in enumerate(layers):
        w = LW[l]
        src = x_in.ap() if li == 0 else xl1.ap()
        if li == nlast:
            dst, tok_dst = y_out.ap(), tok_y
        else:
            dst, tok_dst = xl1.ap(), tok_xl1
        P.dma("sp", VEC.t[:], w["vec"].ap(), W=[VEC.k])
        P.dma("sp", V64.t[:], w["vec64"].ap(), W=[V64.k])
        P.dma("pool", W2A2.t[:], w["w2a2"].ap(), W=[W2A2.k])
        P.dma("pool", G2.t[:], w["g2"].ap(), W=[G2.k])
        if moe[l]:
            P.dma("sp", ROUT.t[:], w["router"].ap(), W=[ROUT.k])
        dve(lambda: V.tensor_scalar(OMM.t[:], VEC.t[:, 0:14], -1.0, 1.0, ALU.mult, ALU.add), [VEC.k], [OMM.k])
        dve(lambda: V.tensor_scalar(OMKA.t[:], VEC.t[:, 26:30], -1.0, 1.0, ALU.mult, ALU.add), [VEC.k], [OMKA.k])
        dve(lambda: V.memset(TAILS.t[:], 0.0), [], [TAILS.k])
        dve(lambda: V.memset(UH.t[:, :, 0:32], 0.0), [], [UH.k])
        dve(lambda: V.memset(GH.t[:, :, 0:32], 0.0), [], [GH.k])
        dve(lambda: V.memset(SRING[sci[0] % 3].t[:], 0.0), [], [SRING[sci[0] % 3].k])
        P.barrier()
        for p in range(npass):
            if "X" in phases:
                phase_X(src, p)
            if "R" in phases:
                phase_R(l, p)
            if "B" in phases:
                phase_BC(l, p)
            if "G" in phases:
                phase_GO(l, p, src)
            if "F" in phases:
                phase_F(l, p, dst, tok_dst)
    P.barrier()
    es.close()
    return nc


def make_consts():
    c = np.zeros((128, K_END), np.float32)
    c[:, K_ID:K_ID + 128] = np.eye(128, dtype=np.float32)
    c[0:64, K_I2:K_I2 + 64] = np.eye(64, dtype=np.float32)
    c[64:128, K_I2:K_I2 + 64] = np.eye(64, dtype=np.float32)
    s = np.arange(64)[:, None]
    n = np.arange(64)[None, :]
    c[0:64, K_MKA:K_MKA + 64] = (s < n)
    c[0:64, K_MKA + 64:K_MKA + 128] = (s <= n)
    c[0:64, K_ML:K_ML + 64] = (n < s)
    c[0:64, K_OB:K_OB + 64] = 1.0
    c[64:128, K_OB + 64:K_OB + 128] = 1.0
    c[:, K_OF:K_OF + 128] = 1.0
    sm = np.ones(512, np.float32)
    sm[::64] = 0.0
    c[:, K_SM:K_SM + 512] = sm[None, :]
    return c


def prep_layer(inp, l, is_moe, j):
    f = np.float32
    out = {}
    w_in = np.asarray(inp["w_in"][l], f)
    W = w_in.reshape(8, 128, 48, 128).transpose(2, 1, 0, 3)
    out["winA%d" % l] = np.ascontiguousarray(W[0:14])
    order = list(range(14, 24)) + [24 + br * 8 + i for i in range(8) for br in range(3)]
    out["winB%d" % l] = np.ascontiguousarray(W[order])
    vec = np.zeros((128, NVEC), f)
    vec[:, 0:14] = np.asarray(inp["rwkv_mu"][l], f).reshape(14, 128).T
    for c0, name in ((14, "rwkv_w0"), (18, "rwkv_a0"), (22, "rwkv_k_k"), (26, "rwkv_k_a"), (30, "rwkv_r_k")):
        vec[:, c0:c0 + 4] = np.asarray(inp[name][l], f).reshape(4, 128).T
    cdw = np.asarray(inp["conf_dw"][l], f)
    for c in range(2):
        vec[:, 34 + c * 31:34 + (c + 1) * 31] = cdw[:, c * 128:(c + 1) * 128].T
    vec[:, 96:98] = np.asarray(inp["conf_dw_b"][l], f).reshape(2, 128).T
    vec[:, 98:100] = np.asarray(inp["conf_ln_g"][l], f).reshape(2, 128).T
    vec[:, 100:102] = np.asarray(inp["conf_ln_b"][l], f).reshape(2, 128).T
    sdw = np.asarray(inp["short_dw"][l], f)
    for c in range(2):
        vec[:, 102 + c * 3:102 + (c + 1) * 3] = sdw[:, c * 128:(c + 1) * 128].T
    out["vec%d" % l] = vec
    v64 = np.zeros((64, 16), f)
    v64[:, 0:8] = np.asarray(inp["rwkv_ln_g"][l], f).reshape(8, 64).T
    v64[:, 8:16] = np.asarray(inp["rwkv_ln_b"][l], f).reshape(8, 64).T
    out["vec64_%d" % l] = v64
    out["w2a2_%d" % l] = np.ascontiguousarray(np.concatenate([np.asarray(inp["rwkv_w2"][l], f), np.asarray(inp["rwkv_a2"][l], f)], 0))
    out["g2_%d" % l] = np.ascontiguousarray(np.asarray(inp["rwkv_g2"][l], f))
    out["woa%d" % l] = np.ascontiguousarray(np.asarray(inp["rwkv_w_o"][l], f).reshape(8, 64, 1024).transpose(1, 0, 2))
    out["wob%d" % l] = np.ascontiguousarray(np.asarray(inp["conf_w_o"][l], f).reshape(2, 128, 1024).transpose(1, 0, 2))
    out["woc%d" % l] = np.ascontiguousarray(np.asarray(inp["short_w_o"][l], f).reshape(2, 128, 1024).transpose(1, 0, 2))
    out["wout%d" % l] = np.ascontiguousarray(np.asarray(inp["w_out"][l], f).reshape(8, 128, 1024).transpose(1, 0, 2))
    lnp = np.stack([np.asarray(inp[n][l], f) for n in ("ln1_g", "ln1_b", "ln2_g", "ln2_b")], 0)
    out["lnp%d" % l] = np.ascontiguousarray(np.broadcast_to(lnp[None], (128, 4, 1024)))
    if is_moe:
        wg = np.asarray(inp["moe_w_gate"][j], f)
        wu = np.asarray(inp["moe_w_up"][j], f)
        wd = np.asarray(inp["moe_w_down"][j], f)
        out["router%d" % l] = np.ascontiguousarray(np.asarray(inp["moe_router"][j], f).reshape(8, 128, 8).transpose(1, 0, 2))
    else:
        wg = np.asarray(inp["ffn_w_gate"][j], f)[None]
        wu = np.asarray(inp["ffn_w_up"][j], f)[None]
        wd = np.asarray(inp["ffn_w_down"][j], f)[None]
    E = wg.shape[0]
    out["wg%d" % l] = np.ascontiguousarray(wg.reshape(E, 8, 128, NFG, 256).transpose(0, 3, 2, 1, 4))
    out["wu%d" % l] = np.ascontiguousarray(wu.reshape(E, 8, 128, NFG, 256).transpose(0, 3, 2, 1, 4))
    out["wd%d" % l] = np.ascontiguousarray(wd.reshape(E, NFG, 2, 128, 1024).transpose(0, 1, 3, 2, 4))
    return out


_NC_CACHE = {}


def kernel(**inputs):
    x = np.asarray(inputs["x"], np.float32)
    B, S, _ = x.shape
    if S not in _NC_CACHE:
        _NC_CACHE[S] = build(S)
    nc = _NC_CACHE[S]
    shared = {"consts": make_consts()}
    for l in range(2):
        shared.update(prep_layer(inputs, l, l % 2 == 1, l // 2))
    maps = []
    for b in range(B):
        m = dict(shared)
        m["x"] = np.ascontiguousarray(x[b])
        maps.append(m)
    in_maps = [maps[c % B] for c in range(8)]
    res = run_bass_kernel_spmd(nc, in_maps, core_ids=list(range(8)))
    return np.stack([np.asarray(res.results[b]["y"], np.float32) for b in range(B)], 0)
```

```python
import numpy as np
from contextlib import ExitStack
import concourse.bass as bass
import concourse.mybir as mybir
from concourse.bass_utils import run_bass_kernel_spmd

F32 = mybir.dt.float32
BF16 = mybir.dt.bfloat16
AF = mybir.ActivationFunctionType
ALU = mybir.AluOpType
AX = mybir.AxisListType

D = 1024
TG = 128
CH = 64
C0 = float(np.exp(-0.5))
ALPHA = float(4.0 ** 0.25)
LN_EPS = 1e-5
GN_EPS = 64e-5
NF = 2816
NFG = 11
K_ID, K_I2, K_MKA, K_ML, K_OB, K_OF, K_SM, K_END = 0, 128, 192, 320, 384, 512, 640, 1152
NVEC = 108


class Tok:
    __slots__ = ("w", "r", "sem", "cnt", "name")

    def __init__(self, name="t"):
        self.name = name
        self.w = {}
        self.r = {}
        self.sem = None
        self.cnt = 0


class DSem:
    __slots__ = ("h", "cnt", "q")

    def __init__(self, h, q):
        self.h = h
        self.cnt = 0
        self.q = q


class Buf:
    def __init__(self, t, name="b"):
        self.t = t
        self.k = Tok(name)


class Prog:
    def __init__(self, nc, es):
        self.nc = nc
        self.es = es
        self.eng = {"pe": nc.tensor, "act": nc.scalar, "dve": nc.vector, "pool": nc.gpsimd, "sp": nc.sync}
        self.sem = {}
        self.cnt = {}
        self.known = {e: {} for e in self.eng}
        for e in ("pe", "act", "dve", "pool"):
            self.sem[e] = es.enter_context(nc.semaphore("sem_" + e))
            self.cnt[e] = 0
        self.nsem = 0
        self.dsems = []
        self.free_dsems = {"sp": [], "pool": []}
        self.banks = []
        self.bi = 0
        self.bbanks = []
        self.bbi = 0
        self.dummy = None
        self.ninst = 0

    def _wait(self, e, deps):
        kn = self.known[e]
        need = {}
        for (sem, val, owner) in deps:
            if owner is not None:
                if owner == e and e == "pe":
                    continue
                assert val <= self.cnt[owner], "uncovered dependency"
            key = id(sem)
            if kn.get(key, 0) >= val:
                continue
            if key not in need or need[key][1] < val:
                need[key] = (sem, val)
        for key, (sem, val) in need.items():
            self.eng[e].wait_ge(sem, val)
            kn[key] = val
            self.ninst += 1

    @staticmethod
    def _put(d, rec):
        key = id(rec[0])
        if key not in d or d[key][1] < rec[1]:
            d[key] = rec

    def op(self, e, fn, R=(), W=(), inc=True):
        deps = []
        for t in R:
            deps += list(t.w.values())
        for t in W:
            deps += list(t.w.values())
            deps += list(t.r.values())
        self._wait(e, deps)
        ins = fn()
        self.ninst += 1
        if inc:
            self.cnt[e] += 1
            ins.then_inc(self.sem[e], 1)
            rec = (self.sem[e], self.cnt[e], e)
        else:
            rec = (self.sem[e], self.cnt[e] + 1, e)
        for t in R:
            self._put(t.r, rec)
        for t in W:
            t.w = {id(rec[0]): rec}
            t.r = {}
        return ins

    def dma(self, q, out, in_, R=(), W=(), part=False, own=None):
        deps = []
        for t in R:
            deps += list(t.w.values())
        for t in W:
            if not part:
                deps += list(t.w.values())
            deps += list(t.r.values())
        self._wait(q, deps)
        ins = self.eng[q].dma_start(out=out, in_=in_)
        self.ninst += 1
        t0 = own if own is not None else W[0]
        if t0.sem is None:
            if self.free_dsems[q]:
                t0.sem = self.free_dsems[q].pop()
            else:
                t0.sem = DSem(self.es.enter_context(self.nc.semaphore("d%d" % self.nsem)), q)
                self.nsem += 1
                self.dsems.append(t0.sem)
        assert t0.sem.q == q, "token DMA'd from two queue kinds"
        t0.sem.cnt += 16
        ins.then_inc(t0.sem.h, 16)
        rec = (t0.sem.h, t0.sem.cnt, None)
        for t in R:
            self._put(t.r, rec)
        for t in W:
            if part:
                self._put(t.w, rec)
            else:
                t.w = {id(rec[0]): rec}
                t.r = {}
        return ins

    def barrier(self):
        f = "pool"
        deps = [(self.sem[e], self.cnt[e], e) for e in ("pe", "act", "dve")]
        deps += [(d.h, d.cnt, None) for d in self.dsems]
        self._wait(f, deps)
        if self.cnt[f] > 0:
            self.eng[f].wait_ge(self.sem[f], self.cnt[f])
        ins = self.nc.gpsimd.memset(self.dummy.t[0:1, 0:1], 0.0)
        self.cnt[f] += 1
        ins.then_inc(self.sem[f], 1)
        for e in ("pe", "act", "dve", "sp"):
            self.eng[e].wait_ge(self.sem[f], self.cnt[f])
        for e in self.eng:
            kn = self.known[e]
            for c in ("pe", "act", "dve", "pool"):
                kn[id(self.sem[c])] = self.cnt[c]
            for d in self.dsems:
                kn[id(d.h)] = d.cnt

    def release(self, st):
        for b in getattr(st, "_bufs", []):
            if b.k.sem is not None:
                self.free_dsems[b.k.sem.q].append(b.k.sem)
                b.k.sem = None

    def bank(self):
        b = self.banks[self.bi % len(self.banks)]
        self.bi += 1
        return b

    def bbank(self):
        b = self.bbanks[self.bbi % len(self.bbanks)]
        self.bbi += 1
        return b


def build(S, layers=(0, 1), moe=(False, True), NT=1024, debug_xm=False, phases="XRBGF", rstop=0, mdbg=9):
    nc = bass.Bass("TRN2", target_bir_lowering=False)
    es = ExitStack()
    P = Prog(nc, es)
    V, A, T = nc.vector, nc.scalar, nc.tensor
    npass = S // NT
    assert S % NT == 0 and NT % 512 == 0
    nlast = len(layers) - 1

    def din(name, shape):
        return nc.dram_tensor(name, list(shape), F32, kind="ExternalInput")

    x_in = din("x", [S, D])
    y_out = nc.dram_tensor("y", [S, D], F32, kind="ExternalOutput")
    xl1 = nc.dram_tensor("xl1", [S, D], F32)
    if debug_xm:
        xm = nc.dram_tensor("xm", [S, D], F32, kind="ExternalOutput")
    else:
        xm = nc.dram_tensor("xm", [S, D], F32)
    consts_d = din("consts", [128, K_END])
    LW = {}
    for l in layers:
        E = 8 if moe[l] else 1
        LW[l] = dict(
            winA=din("winA%d" % l, [14, 128, 8, 128]), winB=din("winB%d" % l, [34, 128, 8, 128]),
            vec=din("vec%d" % l, [128, NVEC]), vec64=din("vec64_%d" % l, [64, 16]),
            w2a2=din("w2a2_%d" % l, [128, 512]), g2=din("g2_%d" % l, [128, 512]),
            woa=din("woa%d" % l, [64, 8, 1024]), wob=din("wob%d" % l, [128, 2, 1024]),
            woc=din("woc%d" % l, [128, 2, 1024]), wout=din("wout%d" % l, [128, 8, 1024]),
            lnp=din("lnp%d" % l, [128, 4, 1024]),
            wg=din("wg%d" % l, [E, NFG, 128, 8, 256]), wu=din("wu%d" % l, [E, NFG, 128, 8, 256]),
            wd=din("wd%d" % l, [E, NFG, 128, 2, 1024]))
        if moe[l]:
            LW[l]["router"] = din("router%d" % l, [128, 8, 8])
    tok_y = Tok("y")
    tok_xl1 = Tok("xl1")
    tok_xm = Tok("xm")

    uid = [0]

    def sb(st, name, shape, dt):
        uid[0] += 1
        b = Buf(st.enter_context(nc.sbuf_tensor("%s_u%d" % (name, uid[0]), list(shape), dt)), name)
        if not hasattr(st, "_bufs"):
            st._bufs = []
        st._bufs.append(b)
        return b

    def dve(fn, R, W):
        return P.op("dve", fn, R, W)

    def act(fn, R, W):
        return P.op("act", fn, R, W)

    def mm(out, lhsT, rhs, start, stop, R, W, inc):
        return P.op("pe", lambda: T.matmul(out, lhsT, rhs, start=start, stop=stop), R, W, inc)

    for i in range(6):
        P.banks.append(Buf(es.enter_context(nc.psum_tensor("pb%d" % i, [128, 512], F32))))
    for i in range(2):
        P.bbanks.append(Buf(es.enter_context(nc.psum_tensor("pbb%d" % i, [128, 1024], BF16))))
    P.dummy = sb(es, "dummy", [128, 4], F32)
    CON = sb(es, "CON", [128, K_END], F32)
    IDB = sb(es, "IDB", [128, 128], BF16)
    XT = sb(es, "XT", [128, 8, NT], BF16)
    OG = sb(es, "OG", [64, 8, NT], BF16)
    UB = sb(es, "UB", [128, 2, NT], BF16)
    UC = sb(es, "UC", [128, 2, NT], BF16)
    UH = sb(es, "UH", [128, 2, 32 + NT], F32)
    GH = sb(es, "GH", [128, 2, 32 + NT], F32)
    VEC = sb(es, "VEC", [128, NVEC], F32)
    OMM = sb(es, "OMM", [128, 14], F32)
    OMKA = sb(es, "OMKA", [128, 4], F32)
    V64 = sb(es, "V64", [64, 16], F32)
    W2A2 = sb(es, "W2A2", [128, 512], BF16)
    G2 = sb(es, "G2", [128, 512], BF16)
    TAILS = sb(es, "TAILS", [128, 14], F32)
    SRING = [sb(es, "SR%d" % i, [64, 8, 64], F32) for i in range(3)]
    GATE = sb(es, "GATE", [128, NT // 128, 8], F32)
    ROUT = sb(es, "ROUT", [128, 8, 8], F32)
    sci = [0]

    P.dma("sp", CON.t[:], consts_d.ap(), W=[CON.k])
    dve(lambda: V.tensor_copy(IDB.t[:], CON.t[:, K_ID:K_ID + 128]), [CON.k], [IDB.k])
    ID = CON.t[:, K_ID:K_ID + 128]

    def cview(lo, hi, rows=128):
        return CON.t[0:rows, lo:hi]

    def phase_X(src, p):
        with ExitStack() as ph:
            xs = [sb(ph, "xs%d" % i, [128, D], F32) for i in range(2)]
            for i in range(NT // 128):
                xb = xs[i % 2]
                r0 = p * NT + i * 128
                P.dma("sp", xb.t[:], src[r0:r0 + 128, :], W=[xb.k])
                for hf in range(2):
                    bk = P.bank()
                    for q in range(4):
                        kc = hf * 4 + q
                        P.op("pe", lambda: T.transpose(bk.t[:, q * 128:(q + 1) * 128], xb.t[:, kc * 128:(kc + 1) * 128], ID),
                             [xb.k, CON.k], [bk.k], inc=(q == 3))
                    o = XT.t[:, hf * 4:hf * 4 + 4, i * 128:(i + 1) * 128]
                    iv = bk.t[:].rearrange("p (q t) -> p q t", q=4)
                    if hf == 0:
                        act(lambda: A.copy(o, iv), [bk.k], [XT.k])
                    else:
                        dve(lambda: V.tensor_copy(o, iv), [bk.k], [XT.k])
            P.barrier()
            P.release(ph)

    def x_body(src, p, ph):
        xs = [sb(ph, "xs%d" % i, [128, D], F32) for i in range(2)]
        for i in range(NT // 128):
            xb = xs[i % 2]
            r0 = p * NT + i * 128
            P.dma("sp", xb.t[:], src[r0:r0 + 128, :], W=[xb.k])
            for hf in range(2):
                bk = P.bank()
                for q in range(4):
                    kc = hf * 4 + q
                    P.op("pe", lambda: T.transpose(bk.t[:, q * 128:(q + 1) * 128], xb.t[:, kc * 128:(kc + 1) * 128], ID),
                         [xb.k, CON.k], [bk.k], inc=(q == 3))
                o = XT.t[:, hf * 4:hf * 4 + 4, i * 128:(i + 1) * 128]
                iv = bk.t[:].rearrange("p (q t) -> p q t", q=4)
                if hf == 0:
                    act(lambda: A.copy(o, iv), [bk.k], [XT.k])
                else:
                    dve(lambda: V.tensor_copy(o, iv), [bk.k], [XT.k])

    def phase_R(l, p, src=None):
        w = LW[l]
        with ExitStack() as ph:
            WA = sb(ph, "WA", [128, 14, 8, 128], BF16)
            for g0 in range(0, 14, 7):
                P.dma("pool", WA.t[:, g0:g0 + 7], w["winA"].ap()[g0:g0 + 7].rearrange("g p k c -> p g k c"),
                      W=[WA.k], part=True)
            if src is not None:
                x_body(src, p, ph)
            sl = {n: sb(ph, "R_" + n, [128, 4, TG], F32) for n in ("r", "k", "v", "A", "S", "T1", "C", "T2", "BV")}
            r_, k_, v_, A_, S_, T1, C_, T2, BV = (sl[n] for n in ("r", "k", "v", "A", "S", "T1", "C", "T2", "BV"))
            DG = sb(ph, "DG", [128, 4, 2, 64], F32)
            GC = sb(ph, "GC", [128, 4, 2], F32)
            PWPA = sb(ph, "PWPA", [128, TG], F32)
            PG = sb(ph, "PG", [128, TG], F32)
            MX = [sb(ph, "MX%d" % i, [128, TG], F32) for i in range(2)]
            TP = sb(ph, "TP", [128, TG], BF16)
            SGB = sb(ph, "SGB", [128, TG], BF16)
            AR = sb(ph, "AR", [128, 4, 2, 2, 64], BF16)
            BK = sb(ph, "BK", [128, 4, 2, 2, 64], BF16)
            VB = sb(ph, "VB", [128, 4, TG], BF16)
            BH = sb(ph, "BH", [128, 4, TG], BF16)
            KH = sb(ph, "KH", [128, 4, TG], BF16)
            VTM = sb(ph, "VTM", [64, 2, 512], BF16)
            BHTM = sb(ph, "BHTM", [64, 2, 512], BF16)
            KHTM = sb(ph, "KHTM", [64, 2, 512], BF16)
            AVA = sb(ph, "AVA", [64, 2, 8, 128], BF16)
            ATA = [sb(ph, "ATA%d" % i, [64, 8, 128], BF16) for i in range(2)]
            ATB = [sb(ph, "ATB%d" % i, [64, 8, 128], BF16) for i in range(2)]
            XXc = [[sb(ph, "XX%d_%d" % (c, i), [64, 8, 64], BF16) for i in range(2)] for c in range(2)]
            YYc = [[sb(ph, "YY%d_%d" % (c, i), [64, 8, 64], BF16) for i in range(2)] for c in range(2)]
            ZZc = [sb(ph, "ZZ%d" % c, [64, 8, 64], BF16) for c in range(2)]
            UW = [sb(ph, "UW%d" % i, [64, 8, 128], BF16) for i in range(2)]
            GT = [sb(ph, "GT%d" % i, [64, 8, 64], F32) for i in range(2)]
            HH = [sb(ph, "HH%d" % i, [64, 8, 64], F32) for i in range(2)]
            OLOC = sb(ph, "OLOC", [64, 8, TG], F32)
            QT = sb(ph, "QT", [64, 8, TG], F32)
            BV64 = sb(ph, "BV64", [64, 8, TG], F32)
            G64 = sb(ph, "G64", [64, 8, TG], F32)
            DN = sb(ph, "DN", [64, 8, TG], F32)
            SQ = sb(ph, "SQ", [64, 8, TG], F32)

            def vb(c0, n=TG):
                return VEC.t[:, c0:c0 + 4].unsqueeze(2).broadcast_to([128, 4, n])

            def cv(b):
                return b.t[:].rearrange("p j (c t) -> p j c t", t=64)

            def fl(b):
                return b.t[:].rearrange("p j t -> p (j t)")

            MKA = cview(K_MKA, K_MKA + 128, 64)
            ML = cview(K_ML, K_ML + 64, 64)
            ONB = cview(K_OB, K_OB + 128)
            ON64 = CON.t[0:64, K_OB:K_OB + 64]
            I2 = cview(K_I2, K_I2 + 64)
            SMK = cview(K_SM, K_SM + 512)

            for tg in range(NT // TG):
                c0 = tg * TG
                for g in range(14):
                    bk = P.bank()
                    for kc in range(8):
                        mm(bk.t[:, 0:TG], WA.t[:, g, kc, :], XT.t[:, kc, c0:c0 + TG], kc == 0, kc == 7,
                           [WA.k, XT.k], [bk.k], kc == 7)
                    if g < 4:
                        dst, dk = r_.t[:, g, :], r_.k
                    elif g < 8:
                        dst, dk = k_.t[:, g - 4, :], k_.k
                    elif g < 12:
                        dst, dk = v_.t[:, g - 8, :], v_.k
                    elif g == 12:
                        dst, dk = PWPA.t[:], PWPA.k
                    else:
                        dst, dk = PG.t[:], PG.k
                    m_ = MX[g % 2]
                    act(lambda: A.activation(out=m_.t[:], in_=bk.t[:, 0:TG], func=AF.Copy, scale=OMM.t[:, g:g + 1]),
                        [bk.k, OMM.k], [m_.k])
                    dve(lambda: V.scalar_tensor_tensor(dst[:, 1:TG], bk.t[:, 0:TG - 1], VEC.t[:, g:g + 1],
                                                       m_.t[:, 1:TG], ALU.mult, ALU.add),
                        [bk.k, VEC.k, m_.k], [dk])
                    dve(lambda: V.scalar_tensor_tensor(dst[:, 0:1], TAILS.t[:, g:g + 1], VEC.t[:, g:g + 1],
                                                       m_.t[:, 0:1], ALU.mult, ALU.add),
                        [TAILS.k, VEC.k, m_.k], [dk])
                    dve(lambda: V.tensor_copy(TAILS.t[:, g:g + 1], bk.t[:, TG - 1:TG]), [bk.k], [TAILS.k])
                if rstop == 1:
                    P.barrier()
                    P.release(ph)
                    return
                act(lambda: A.activation(out=TP.t[0:64, :], in_=PWPA.t[0:64, :], func=AF.Tanh), [PWPA.k], [TP.k])
                dve(lambda: V.tensor_copy(TP.t[64:128, :], PWPA.t[64:128, :]), [PWPA.k], [TP.k])
                act(lambda: A.activation(out=SGB.t[:], in_=PG.t[:], func=AF.Sigmoid), [PG.k], [SGB.k])
                for j in range(4):
                    bk = P.bank()
                    mm(bk.t[:, 0:TG], W2A2.t[0:64, j * 128:(j + 1) * 128], TP.t[0:64, :], True, True,
                       [W2A2.k, TP.k], [bk.k], True)
                    act(lambda: A.activation(out=S_.t[:, j, :], in_=bk.t[:, 0:TG], func=AF.Sigmoid,
                                             bias=VEC.t[:, 14 + j:15 + j]), [bk.k, VEC.k], [S_.k])
                    bk2 = P.bank()
                    mm(bk2.t[:, 0:TG], W2A2.t[64:128, j * 128:(j + 1) * 128], TP.t[64:128, :], True, True,
                       [W2A2.k, TP.k], [bk2.k], True)
                    act(lambda: A.activation(out=A_.t[:, j, :], in_=bk2.t[:, 0:TG], func=AF.Sigmoid,
                                             bias=VEC.t[:, 18 + j:19 + j]), [bk2.k, VEC.k], [A_.k])
                if rstop == 2:
                    P.barrier()
                    P.release(ph)
                    return
                dve(lambda: V.tensor_tensor(T1.t[:], k_.t[:], vb(22), ALU.mult), [k_.k, VEC.k], [T1.k])
                act(lambda: A.activation(out=T2.t[:], in_=T1.t[:], func=AF.Square), [T1.k], [T2.k])
                bk = P.bank()
                mm(bk.t[:, 0:512], ONB, fl(T2), True, True, [CON.k, T2.k], [bk.k], True)
                act(lambda: A.activation(out=fl(T2), in_=bk.t[:, 0:512], func=AF.Ln, bias=1e-24, scale=1.0),
                    [bk.k], [T2.k])
                act(lambda: A.activation(out=T2.t[:], in_=T2.t[:], func=AF.Exp, scale=-0.5), [T2.k], [T2.k])
                dve(lambda: V.tensor_tensor(T1.t[:], T1.t[:], T2.t[:], ALU.mult), [T1.k, T2.k], [T1.k])
                if rstop == 3:
                    P.barrier()
                    P.release(ph)
                    return
                dve(lambda: V.tensor_tensor_scan(fl(C_), SMK, fl(S_), 0.0, ALU.mult, ALU.add), [CON.k, S_.k], [C_.k])
                act(lambda: A.activation(out=T2.t[:], in_=C_.t[:], func=AF.Exp, scale=-C0), [C_.k], [T2.k])
                dve(lambda: V.tensor_tensor(AR.t[:, :, :, 1, :], cv(r_), cv(T2), ALU.mult), [r_.k, T2.k], [AR.k])
                dve(lambda: V.tensor_tensor(T2.t[:], C_.t[:], S_.t[:], ALU.subtract), [C_.k, S_.k], [T2.k])
                act(lambda: A.activation(out=T2.t[:], in_=T2.t[:], func=AF.Exp, scale=-C0), [T2.k], [T2.k])
                dve(lambda: V.scalar_tensor_tensor(AR.t[:, :, :, 0, :], cv(T1), -1.0, cv(T2), ALU.mult, ALU.mult),
                    [T1.k, T2.k], [AR.k])
                act(lambda: A.activation(out=T2.t[:], in_=C_.t[:], func=AF.Exp, scale=C0), [C_.k], [T2.k])
                dve(lambda: V.tensor_tensor(S_.t[:], T1.t[:], A_.t[:], ALU.mult), [T1.k, A_.k], [S_.k])
                dve(lambda: V.tensor_tensor(BK.t[:, :, :, 0, :], cv(S_), cv(T2), ALU.mult), [S_.k, T2.k], [BK.k])
                for j in range(4):
                    dve(lambda: V.tensor_scalar(A_.t[:, j, :], A_.t[:, j, :], VEC.t[:, 26 + j:27 + j],
                                                OMKA.t[:, j:j + 1], ALU.mult, ALU.add), [A_.k, VEC.k, OMKA.k], [A_.k])
                dve(lambda: V.tensor_tensor(k_.t[:], k_.t[:], A_.t[:], ALU.mult), [k_.k, A_.k], [k_.k])
                dve(lambda: V.tensor_tensor(BK.t[:, :, :, 1, :], cv(k_), cv(T2), ALU.mult), [k_.k, T2.k], [BK.k])
                clast = cv(C_)[:, :, :, 63:64]
                dve(lambda: V.tensor_tensor(cv(T2), clast.broadcast_to([128, 4, 2, 64]), cv(C_), ALU.subtract),
                    [C_.k], [T2.k])
                act(lambda: A.activation(out=T2.t[:], in_=T2.t[:], func=AF.Exp, scale=-C0), [T2.k], [T2.k])
                dve(lambda: V.tensor_tensor(BH.t[:], S_.t[:], T2.t[:], ALU.mult), [S_.k, T2.k], [BH.k])
                dve(lambda: V.tensor_tensor(KH.t[:], k_.t[:], T2.t[:], ALU.mult), [k_.k, T2.k], [KH.k])
                act(lambda: A.activation(out=GC.t[:], in_=cv(C_)[:, :, :, 63], func=AF.Exp, scale=-C0), [C_.k], [GC.k])
                dve(lambda: V.tensor_tensor(DG.t[:], I2.unsqueeze(1).unsqueeze(1).broadcast_to([128, 4, 2, 64]),
                                            GC.t[:].unsqueeze(3).broadcast_to([128, 4, 2, 64]), ALU.mult),
                    [CON.k, GC.k], [DG.k])
                if rstop == 4:
                    P.barrier()
                    P.release(ph)
                    return
                dve(lambda: V.tensor_tensor(T2.t[:], r_.t[:], k_.t[:], ALU.mult), [r_.k, k_.k], [T2.k])
                dve(lambda: V.tensor_tensor(T2.t[:], T2.t[:], vb(30), ALU.mult), [T2.k, VEC.k], [T2.k])
                bk = P.bank()
                mm(bk.t[:, 0:512], ONB, fl(T2), True, True, [CON.k, T2.k], [bk.k], True)
                dve(lambda: V.tensor_tensor(fl(BV), bk.t[:, 0:512], fl(v_), ALU.mult), [bk.k, v_.k], [BV.k])
                act(lambda: A.copy(VB.t[:], v_.t[:]), [v_.k], [VB.k])
                for (srcb, dstb, kind) in ((BV, BV64, 0), (SGB, G64, 1)):
                    bks = [P.bank(), P.bank()]
                    for h in range(8):
                        j, hp = h // 2, h % 2
                        rows = slice(64 * hp, 64 * hp + 64)
                        bkx = bks[h // 4]
                        o = bkx.t[0:64, (h % 4) * 128:(h % 4 + 1) * 128]
                        if kind == 0:
                            mm(o, CON.t[:, K_ID + 64 * hp:K_ID + 64 * hp + 64], BV.t[:, j, :], True, True,
                               [CON.k, BV.k], [bkx.k], h % 4 == 3)
                        else:
                            mm(o, G2.t[:, h * 64:(h + 1) * 64], SGB.t[:], True, True, [G2.k, SGB.k], [bkx.k], h % 4 == 3)
                    for q in range(2):
                        o = dstb.t[:, q * 4:q * 4 + 4, :]
                        iv = bks[q].t[0:64, :].rearrange("p (h t) -> p h t", h=4)
                        if q == 0:
                            act(lambda: A.copy(o, iv), [bks[q].k], [dstb.k])
                        else:
                            dve(lambda: V.tensor_copy(o, iv), [bks[q].k], [dstb.k])
                if rstop == 5:
                    P.barrier()
                    P.release(ph)
                    return
                for c in range(2):
                    for si, (srcb, dstb) in enumerate(((VB, VTM), (BH, BHTM), (KH, KHTM), (AR, AVA))):
                        bb = P.bbank()
                        for j in range(4):
                            if srcb is AR:
                                iv = AR.t[:, j, c, 0, :]
                            else:
                                iv = srcb.t[:, j, c * 64:(c + 1) * 64]
                            P.op("pe", lambda: T.transpose(bb.t[0:64, j * 128:(j + 1) * 128], iv, IDB.t[:]),
                                 [srcb.k, IDB.k], [bb.k], inc=(j == 3))
                        if srcb is AR:
                            o = AVA.t[:, c, :, 64:128]
                            iv2 = bb.t[0:64, 0:512].rearrange("p (h t) -> p h t", h=8)
                        else:
                            o = dstb.t[:, c, :]
                            iv2 = bb.t[0:64, 0:512]
                        if si % 2 == 0:
                            act(lambda: A.copy(o, iv2), [bb.k], [dstb.k])
                        else:
                            dve(lambda: V.tensor_copy(o, iv2), [bb.k], [dstb.k])
                if rstop == 6:
                    P.barrier()
                    P.release(ph)
                    return
                CS = (0, 1)
                for c in CS:
                    ata, atb = ATA[c], ATB[c]
                    for which, dstb in ((0, ata), (1, atb)):
                        bks = [P.bank(), P.bank()]
                        for h in range(8):
                            j, hp = h // 2, h % 2
                            rows = slice(64 * hp, 64 * hp + 64)
                            bkx = bks[hp]
                            mm(bkx.t[0:64, j * 128:(j + 1) * 128], BK.t[rows, j, c, which, :],
                               AR.t[rows, j, c, :, :], True, True, [BK.k, AR.k], [bkx.k], h >= 6)
                        for q in range(2):
                            iv = bks[q].t[0:64, :].rearrange("p (h t) -> p h t", h=4)
                            ov = dstb.t[:].rearrange("p (j hp) n -> p hp j n", hp=2)[:, q]
                            dve(lambda: V.tensor_tensor(ov, iv, MKA.unsqueeze(1).broadcast_to([64, 4, 128]), ALU.mult),
                                [bks[q].k, CON.k], [dstb.k])
                            if which == 0:
                                ox = XXc[c][0].t[:].rearrange("p (j hp) n -> p hp j n", hp=2)[:, q]
                                dve(lambda: V.tensor_tensor(ox, iv[:, :, 0:64],
                                                            MKA[:, 0:64].unsqueeze(1).broadcast_to([64, 4, 64]), ALU.mult),
                                    [bks[q].k, CON.k], [XXc[c][0].k])
                    bks = [P.bank(), P.bank()]
                    for h in range(8):
                        j, hp = h // 2, h % 2
                        rows = slice(64 * hp, 64 * hp + 64)
                        mm(bks[hp].t[0:64, j * 64:(j + 1) * 64], AR.t[rows, j, c, 0, :], BK.t[rows, j, c, 0, :], True, True,
                           [AR.k, BK.k], [bks[hp].k], h >= 6)
                    for q in range(2):
                        oy = YYc[c][0].t[:].rearrange("p (j hp) n -> p hp j n", hp=2)[:, q]
                        dve(lambda: V.tensor_tensor(oy, bks[q].t[0:64, 0:256].rearrange("p (h t) -> p h t", h=4),
                                                    ML.unsqueeze(1).broadcast_to([64, 4, 64]), ALU.mult),
                            [bks[q].k, CON.k], [YYc[c][0].k])
                    dve(lambda: V.tensor_tensor(ZZc[c].t[:], XXc[c][0].t[:],
                                                CON.t[0:64, K_ID:K_ID + 64].unsqueeze(1).broadcast_to([64, 8, 64]), ALU.add),
                        [XXc[c][0].k, CON.k], [ZZc[c].k])
                for lvl in range(5):
                    pxs, pys = {}, {}
                    for c in CS:
                        xc, yc = XXc[c][lvl % 2], YYc[c][lvl % 2]
                        if lvl < 4:
                            px = P.bank()
                            for h in range(8):
                                mm(px.t[0:64, h * 64:(h + 1) * 64], yc.t[:, h, :], xc.t[:, h, :], True, True,
                                   [yc.k, xc.k], [px.k], h == 7)
                            pxs[c] = px
                        py = P.bank()
                        for h in range(8):
                            mm(py.t[0:64, h * 64:(h + 1) * 64], xc.t[:, h, :], yc.t[:, h, :], True, True,
                               [yc.k, xc.k], [py.k], h == 7)
                        pys[c] = py
                    for c in CS:
                        xn, yn = XXc[c][(lvl + 1) % 2], YYc[c][(lvl + 1) % 2]
                        if lvl < 4:
                            px = pxs[c]
                            act(lambda: A.copy(xn.t[:], px.t[0:64, :].rearrange("p (h t) -> p h t", h=8)), [px.k], [xn.k])
                        py = pys[c]
                        act(lambda: A.copy(yn.t[:], py.t[0:64, :].rearrange("p (h t) -> p h t", h=8)), [py.k], [yn.k])
                    pzs = {}
                    for c in CS:
                        yn = YYc[c][(lvl + 1) % 2]
                        pz = P.bank()
                        for h in range(8):
                            mm(pz.t[0:64, h * 64:(h + 1) * 64], yn.t[:, h, :], ZZc[c].t[:, h, :], True, True,
                               [yn.k, ZZc[c].k], [pz.k], h == 7)
                        pzs[c] = pz
                    for c in CS:
                        pz = pzs[c]
                        dve(lambda: V.tensor_tensor(ZZc[c].t[:], pz.t[0:64, :].rearrange("p (h t) -> p h t", h=8), ZZc[c].t[:], ALU.add),
                            [pz.k, ZZc[c].k], [ZZc[c].k])
                pvs = {}
                for c in CS:
                    pv = P.bank()
                    for h in range(8):
                        mm(pv.t[0:64, h * 64:(h + 1) * 64], ATB[c].t[:, h, 0:64], VTM.t[:, c, h * 64:(h + 1) * 64], True, True,
                           [ATB[c].k, VTM.k], [pv.k], h == 7)
                    pvs[c] = pv
                for c in CS:
                    pv = pvs[c]
                    if c == 0:
                        act(lambda: A.copy(AVA.t[:, c, :, 0:64], pv.t[0:64, :].rearrange("p (h t) -> p h t", h=8)), [pv.k], [AVA.k])
                    else:
                        dve(lambda: V.tensor_copy(AVA.t[:, c, :, 0:64], pv.t[0:64, :].rearrange("p (h t) -> p h t", h=8)), [pv.k], [AVA.k])
                for c in CS:
                    tt, uw = ZZc[c], UW[c]
                    bks = [P.bank(), P.bank()]
                    for h in range(8):
                        bkx = bks[h // 4]
                        mm(bkx.t[0:64, (h % 4) * 128:(h % 4 + 1) * 128], tt.t[:, h, :], AVA.t[:, c, h, :], True, True,
                           [tt.k, AVA.k], [bkx.k], h % 4 == 3)
                    act(lambda: A.copy(uw.t[:, 0:4, :], bks[0].t[0:64, :].rearrange("p (h t) -> p h t", h=4)), [bks[0].k], [uw.k])
                    dve(lambda: V.tensor_copy(uw.t[:, 4:8, :], bks[1].t[0:64, :].rearrange("p (h t) -> p h t", h=4)), [bks[1].k], [uw.k])
                for c in CS:
                    ata, atb, uw, gt, hh = ATA[c], ATB[c], UW[c], GT[c], HH[c]
                    po = P.bank()
                    for h in range(8):
                        o = po.t[0:64, h * 64:(h + 1) * 64]
                        mm(o, uw.t[:, h, 0:64], ata.t[:, h, 64:128], True, False, [uw.k, ata.k], [po.k], False)
                        mm(o, VTM.t[:, c, h * 64:(h + 1) * 64], atb.t[:, h, 64:128], False, True, [VTM.k, atb.k], [po.k], h == 7)
                    act(lambda: A.copy(OLOC.t[:, :, c * 64:(c + 1) * 64], po.t[0:64, :].rearrange("p (h t) -> p h t", h=8)),
                        [po.k], [OLOC.k])
                    pq = P.bank()
                    for h in range(8):
                        j, hp = h // 2, h % 2
                        o = pq.t[0:64, h * 64:(h + 1) * 64]
                        mm(o, uw.t[:, h, 64:128], ata.t[:, h, 64:128], True, False, [uw.k, ata.k], [pq.k], False)
                        mm(o, IDB.t[:, 64 * hp:64 * hp + 64], AR.t[:, j, c, 1, :], False, True, [IDB.k, AR.k], [pq.k], h == 7)
                    dve(lambda: V.tensor_copy(QT.t[:, :, c * 64:(c + 1) * 64], pq.t[0:64, :].rearrange("p (h t) -> p h t", h=8)),
                        [pq.k], [QT.k])
                    pg_ = P.bank()
                    for h in range(8):
                        j, hp = h // 2, h % 2
                        o = pg_.t[0:64, h * 64:(h + 1) * 64]
                        mm(o, uw.t[:, h, 64:128], BHTM.t[:, c, h * 64:(h + 1) * 64], True, False, [uw.k, BHTM.k], [pg_.k], False)
                        mm(o, CON.t[:, K_ID + 64 * hp:K_ID + 64 * hp + 64], DG.t[:, j, c, :], False, True,
                           [CON.k, DG.k], [pg_.k], h == 7)
                    act(lambda: A.copy(gt.t[:], pg_.t[0:64, :].rearrange("p (h t) -> p h t", h=8)), [pg_.k], [gt.k])
                    phh = P.bank()
                    for h in range(8):
                        o = phh.t[0:64, h * 64:(h + 1) * 64]
                        mm(o, BHTM.t[:, c, h * 64:(h + 1) * 64], uw.t[:, h, 0:64], True, False, [BHTM.k, uw.k], [phh.k], False)
                        mm(o, KHTM.t[:, c, h * 64:(h + 1) * 64], VTM.t[:, c, h * 64:(h + 1) * 64], False, True,
                           [KHTM.k, VTM.k], [phh.k], h == 7)
                    dve(lambda: V.tensor_copy(hh.t[:], phh.t[0:64, :].rearrange("p (h t) -> p h t", h=8)), [phh.k], [hh.k])
                for c in CS:
                    gt, hh = GT[c], HH[c]
                    scur = SRING[sci[0] % 3]
                    snext = SRING[(sci[0] + 1) % 3]
                    sci[0] += 1
                    pO = P.bank()
                    for h in range(8):
                        mm(pO.t[0:64, h * 64:(h + 1) * 64], scur.t[:, h, :], QT.t[:, h, c * 64:(c + 1) * 64], True, True,
                           [scur.k, QT.k], [pO.k], h == 7)
                    dve(lambda: V.tensor_tensor(OLOC.t[:, :, c * 64:(c + 1) * 64],
                                                pO.t[0:64, :].rearrange("p (h t) -> p h t", h=8),
                                                OLOC.t[:, :, c * 64:(c + 1) * 64], ALU.add), [pO.k, OLOC.k], [OLOC.k])
                    pS = P.bank()
                    for h in range(8):
                        mm(pS.t[0:64, h * 64:(h + 1) * 64], gt.t[:, h, :], scur.t[:, h, :], True, True,
                           [gt.k, scur.k], [pS.k], h == 7)
                    dve(lambda: V.tensor_tensor(snext.t[:], pS.t[0:64, :].rearrange("p (h t) -> p h t", h=8), hh.t[:], ALU.add),
                        [pS.k, hh.k], [snext.k])
                ofl = OLOC.t[:].rearrange("p h t -> p (h t)")
                dfl = DN.t[:].rearrange("p h t -> p (h t)")
                sfl = SQ.t[:].rearrange("p h t -> p (h t)")
                for q in range(2):
                    bk = P.bank()
                    mm(bk.t[0:64, :], ON64, ofl[:, q * 512:(q + 1) * 512], True, True, [CON.k, OLOC.k], [bk.k], True)
                    dve(lambda: V.scalar_tensor_tensor(dfl[:, q * 512:(q + 1) * 512], bk.t[0:64, :], -1.0 / 64,
                                                       ofl[:, q * 512:(q + 1) * 512], ALU.mult, ALU.add),
                        [bk.k, OLOC.k], [DN.k])
                act(lambda: A.activation(out=SQ.t[:], in_=DN.t[:], func=AF.Square), [DN.k], [SQ.k])
                for q in range(2):
                    bk = P.bank()
                    mm(bk.t[0:64, :], ON64, sfl[:, q * 512:(q + 1) * 512], True, True, [CON.k, SQ.k], [bk.k], True)
                    act(lambda: A.activation(out=sfl[:, q * 512:(q + 1) * 512], in_=bk.t[0:64, :], func=AF.Ln,
                                             bias=GN_EPS, scale=1.0 / 64), [bk.k], [SQ.k])
                act(lambda: A.activation(out=SQ.t[:], in_=SQ.t[:], func=AF.Exp, scale=-0.5), [SQ.k], [SQ.k])
                dve(lambda: V.tensor_tensor(DN.t[:], DN.t[:], SQ.t[:], ALU.mult), [DN.k, SQ.k], [DN.k])
                dve(lambda: V.tensor_tensor(DN.t[:], DN.t[:], V64.t[:, 0:8].unsqueeze(2).broadcast_to([64, 8, TG]), ALU.mult),
                    [DN.k, V64.k], [DN.k])
                dve(lambda: V.tensor_tensor(DN.t[:], DN.t[:], V64.t[:, 8:16].unsqueeze(2).broadcast_to([64, 8, TG]), ALU.add),
                    [DN.k, V64.k], [DN.k])
                dve(lambda: V.tensor_tensor(DN.t[:], DN.t[:], BV64.t[:], ALU.add), [DN.k, BV64.k], [DN.k])
                dve(lambda: V.tensor_tensor(OG.t[:, :, c0:c0 + TG], DN.t[:], G64.t[:], ALU.mult), [DN.k, G64.k], [OG.k])
            P.barrier()
            P.release(ph)

    def phase_BC(l, p):
        w = LW[l]
        with ExitStack() as ph:
            WB = sb(ph, "WB", [128, 10, 8, 128], BF16)
            P.dma("pool", WB.t[:], w["winB"].ap()[0:10].rearrange("g p k c -> p g k c"), W=[WB.k])
            TMP = [sb(ph, "bcT%d" % i, [128, 512], F32) for i in range(2)]
            ACC = sb(ph, "bcACC", [128, 2, NT], F32)
            ACCc = [Tok("acc0"), Tok("acc1")]
            DD = sb(ph, "bcD", [128, 2, 512], F32)
            GBB = sb(ph, "bcGB", [128, 2, NT], F32)
            ONF = cview(K_OF, K_OF + 128)
            ntg = NT // 512

            def proj(g, tg):
                bk = P.bank()
                for kc in range(8):
                    mm(bk.t[:], WB.t[:, g, kc, :], XT.t[:, kc, tg * 512:(tg + 1) * 512], kc == 0, kc == 7,
                       [WB.k, XT.k], [bk.k], kc == 7)
                return bk

            for tg in range(ntg):
                for c in range(2):
                    bu = proj(c, tg)
                    bg = proj(2 + c, tg)
                    tm = TMP[c]
                    act(lambda: A.activation(out=tm.t[:], in_=bg.t[:], func=AF.Sigmoid), [bg.k], [tm.k])
                    dve(lambda: V.tensor_tensor(UH.t[:, c, 32 + tg * 512:32 + (tg + 1) * 512], bu.t[:], tm.t[:], ALU.mult),
                        [bu.k, tm.k], [UH.k])
            for c in range(2):
                dve(lambda: V.tensor_scalar(ACC.t[:, c, :], UH.t[:, c, 2:2 + NT], VEC.t[:, 34 + c * 31:35 + c * 31],
                                            VEC.t[:, 96 + c:97 + c], ALU.mult, ALU.add), [UH.k, VEC.k], [ACCc[c]])
            for jj in range(1, 31):
                for c in range(2):
                    dve(lambda: V.scalar_tensor_tensor(ACC.t[:, c, :], UH.t[:, c, 2 + jj:2 + jj + NT],
                                                       VEC.t[:, 34 + c * 31 + jj:35 + c * 31 + jj], ACC.t[:, c, :],
                                                       ALU.mult, ALU.add), [UH.k, VEC.k, ACCc[c]], [ACCc[c]])
            dve(lambda: V.tensor_copy(ACC.t[:, 0, 0:1], ACC.t[:, 0, 0:1]), [ACCc[0], ACCc[1]], [ACC.k, ACCc[0], ACCc[1]])
            dve(lambda: V.tensor_copy(UH.t[:, :, 0:32], UH.t[:, :, NT:NT + 32]), [UH.k], [UH.k])
            for tg in range(ntg):
                ts_ = slice(tg * 512, (tg + 1) * 512)
                bk = P.bank()
                for c in range(2):
                    mm(bk.t[:], ONF, ACC.t[:, c, ts_], c == 0, c == 1, [CON.k, ACC.k], [bk.k], c == 1)
                for c in range(2):
                    dve(lambda: V.scalar_tensor_tensor(DD.t[:, c, :], bk.t[:], -1.0 / 256, ACC.t[:, c, ts_], ALU.mult, ALU.add),
                        [bk.k, ACC.k], [DD.k])
                    act(lambda: A.activation(out=ACC.t[:, c, ts_], in_=DD.t[:, c, :], func=AF.Square), [DD.k], [ACC.k])
                bk2 = P.bank()
                for c in range(2):
                    mm(bk2.t[:], ONF, ACC.t[:, c, ts_], c == 0, c == 1, [CON.k, ACC.k], [bk2.k], c == 1)
                tm = TMP[0]
                act(lambda: A.activation(out=tm.t[:], in_=bk2.t[:], func=AF.Sqrt, bias=LN_EPS, scale=1.0 / 256), [bk2.k], [tm.k])
                dve(lambda: V.reciprocal(tm.t[:], tm.t[:]), [tm.k], [tm.k])
                for c in range(2):
                    dve(lambda: V.tensor_tensor(DD.t[:, c, :], DD.t[:, c, :], tm.t[:], ALU.mult), [DD.k, tm.k], [DD.k])
                    dve(lambda: V.tensor_scalar(DD.t[:, c, :], DD.t[:, c, :], VEC.t[:, 98 + c:99 + c], VEC.t[:, 100 + c:101 + c],
                                                ALU.mult, ALU.add), [DD.k, VEC.k], [DD.k])
                    act(lambda: A.activation(out=UB.t[:, c, ts_], in_=DD.t[:, c, :], func=AF.Silu), [DD.k], [UB.k])
            for tg in range(ntg):
                for c in range(2):
                    bgb = proj(4 + c, tg)
                    act(lambda: A.copy(GBB.t[:, c, tg * 512:(tg + 1) * 512], bgb.t[:]), [bgb.k], [GBB.k])
                    bgc = proj(6 + c, tg)
                    bh = proj(8 + c, tg)
                    tm = TMP[c]
                    act(lambda: A.copy(tm.t[:], bh.t[:]), [bh.k], [tm.k])
                    dve(lambda: V.tensor_tensor(GH.t[:, c, 32 + tg * 512:32 + (tg + 1) * 512], bgc.t[:], tm.t[:], ALU.mult),
                        [bgc.k, tm.k], [GH.k])
            for c in range(2):
                dve(lambda: V.tensor_scalar(ACC.t[:, c, :], GH.t[:, c, 30:30 + NT], VEC.t[:, 102 + c * 3:103 + c * 3], None,
                                            ALU.mult), [GH.k, VEC.k], [ACC.k])
                for jj in range(1, 3):
                    dve(lambda: V.scalar_tensor_tensor(ACC.t[:, c, :], GH.t[:, c, 30 + jj:30 + jj + NT],
                                                       VEC.t[:, 102 + c * 3 + jj:103 + c * 3 + jj], ACC.t[:, c, :],
                                                       ALU.mult, ALU.add), [GH.k, VEC.k, ACC.k], [ACC.k])
                dve(lambda: V.tensor_tensor(UC.t[:, c, :], GBB.t[:, c, :], ACC.t[:, c, :], ALU.mult), [GBB.k, ACC.k], [UC.k])
            dve(lambda: V.tensor_copy(GH.t[:, :, 0:32], GH.t[:, :, NT:NT + 32]), [GH.k], [GH.k])
            P.barrier()
            P.release(ph)

    def phase_GO(l, p, src):
        w = LW[l]
        with ExitStack() as ph:
            MT = sb(ph, "MT", [128, 8, NT], BF16)
            with ExitStack() as ph2:
                WOA = sb(ph2, "WOA", [64, 8, 1024], BF16)
                WOB = sb(ph2, "WOB", [128, 2, 1024], BF16)
                WOC = sb(ph2, "WOC", [128, 2, 1024], BF16)
                P.dma("pool", WOA.t[:], w["woa"].ap(), W=[WOA.k])
                P.dma("pool", WOB.t[:], w["wob"].ap(), W=[WOB.k])
                P.dma("pool", WOC.t[:], w["woc"].ap(), W=[WOC.k])
                WG = [sb(ph2, "WGt%d" % i, [128, 3, 8, 128], BF16) for i in range(2)]
                GS = [sb(ph2, "GS%d" % i, [128, 512], F32) for i in range(3)]
                MA = sb(ph2, "MA", [128, 512], F32)
                MB = sb(ph2, "MB", [128, 512], F32)
                ntg = NT // 512
                for i in range(8):
                    wgt = WG[i % 2]
                    P.dma("pool", wgt.t[:], w["winB"].ap()[10 + 3 * i:13 + 3 * i].rearrange("g p k c -> p g k c"), W=[wgt.k])
                    for tg in range(ntg):
                        ts_ = slice(tg * 512, (tg + 1) * 512)
                        for br in range(3):
                            bk = P.bank()
                            for kc in range(8):
                                mm(bk.t[:], wgt.t[:, br, kc, :], XT.t[:, kc, ts_], kc == 0, kc == 7, [wgt.k, XT.k], [bk.k], kc == 7)
                            act(lambda: A.activation(out=GS[br].t[:], in_=bk.t[:], func=AF.Sigmoid), [bk.k], [GS[br].k])
                        ba = P.bank()
                        for h in range(8):
                            mm(ba.t[:], WOA.t[:, h, i * 128:(i + 1) * 128], OG.t[:, h, ts_], h == 0, h == 7, [WOA.k, OG.k], [ba.k], h == 7)
                        dve(lambda: V.tensor_tensor(MA.t[:], ba.t[:], GS[0].t[:], ALU.mult), [ba.k, GS[0].k], [MA.k])
                        bb_ = P.bank()
                        for c in range(2):
                            mm(bb_.t[:], WOB.t[:, c, i * 128:(i + 1) * 128], UB.t[:, c, ts_], c == 0, c == 1, [WOB.k, UB.k], [bb_.k], c == 1)
                        dve(lambda: V.tensor_tensor(MB.t[:], bb_.t[:], GS[1].t[:], ALU.mult), [bb_.k, GS[1].k], [MB.k])
                        dve(lambda: V.tensor_tensor(MA.t[:], MA.t[:], MB.t[:], ALU.add), [MA.k, MB.k], [MA.k])
                        bc = P.bank()
                        for c in range(2):
                            mm(bc.t[:], WOC.t[:, c, i * 128:(i + 1) * 128], UC.t[:, c, ts_], c == 0, c == 1, [WOC.k, UC.k], [bc.k], c == 1)
                        dve(lambda: V.tensor_tensor(MB.t[:], bc.t[:], GS[2].t[:], ALU.mult), [bc.k, GS[2].k], [MB.k])
                        dve(lambda: V.tensor_tensor(MT.t[:, i, ts_], MA.t[:], MB.t[:], ALU.add), [MA.k, MB.k], [MT.k])
                P.barrier()
                P.release(ph2)
            WOUT = sb(ph, "WOUT", [128, 8, 1024], BF16)
            P.dma("pool", WOUT.t[:], w["wout"].ap(), W=[WOUT.k])
            LNP = sb(ph, "LNP", [128, 2, 1024], F32)
            P.dma("sp", LNP.t[:], w["lnp"].ap()[:, 0:2, :], W=[LNP.k])
            XR = [sb(ph, "XR%d" % i, [128, D], F32) for i in range(2)]
            ZB = [sb(ph, "ZB%d" % i, [128, D], F32) for i in range(2)]
            ST = sb(ph, "ST", [128, 2, 6], F32)
            MV = sb(ph, "MV", [128, 4], F32)
            XTF = sb(ph, "XTF", [128, 8, 128], F32)
            LG = sb(ph, "LG", [128, 8], F32)
            LG2 = sb(ph, "LG2", [128, 8], F32)
            EQ1 = sb(ph, "EQ1", [128, 8], F32)
            EQ2 = sb(ph, "EQ2", [128, 8], F32)
            SM = sb(ph, "SMx", [128, 8], F32)
            def stage_a(i):
                r0 = p * NT + i * 128
                xr = XR[i % 2]
                zb = ZB[i % 2]
                P.dma("sp", xr.t[:], src[r0:r0 + 128, :], W=[xr.k])
                for hf in range(2):
                    bk = P.bank()
                    for kc in range(8):
                        mm(bk.t[:], MT.t[:, kc, i * 128:(i + 1) * 128], WOUT.t[:, kc, hf * 512:(hf + 1) * 512], kc == 0, kc == 7,
                           [MT.k, WOUT.k], [bk.k], kc == 7)
                    dve(lambda: V.scalar_tensor_tensor(zb.t[:, hf * 512:(hf + 1) * 512], xr.t[:, hf * 512:(hf + 1) * 512], ALPHA,
                                                       bk.t[:], ALU.mult, ALU.add), [xr.k, bk.k], [zb.k])
                layer_norm(zb, LNP, 0, ST, MV)
                P.dma("sp", xm.ap()[r0:r0 + 128, :], zb.t[:], R=[zb.k], W=[tok_xm], part=True, own=zb.k)

            stage_a(0)
            for i in range(NT // 128):
                if i + 1 < NT // 128:
                    stage_a(i + 1)
                zb = ZB[i % 2]
                for hf in range(2):
                    bk = P.bank()
                    for q in range(4):
                        kc = hf * 4 + q
                        P.op("pe", lambda: T.transpose(bk.t[:, q * 128:(q + 1) * 128], zb.t[:, kc * 128:(kc + 1) * 128], ID),
                             [zb.k, CON.k], [bk.k], inc=(q == 3))
                    iv = bk.t[:].rearrange("p (q t) -> p q t", q=4)
                    if moe[l]:
                        act(lambda: A.copy(XTF.t[:, hf * 4:hf * 4 + 4, :], iv), [bk.k], [XTF.k])
                        dve(lambda: V.tensor_copy(XT.t[:, hf * 4:hf * 4 + 4, i * 128:(i + 1) * 128], XTF.t[:, hf * 4:hf * 4 + 4, :]),
                            [XTF.k], [XT.k])
                    else:
                        act(lambda: A.copy(XT.t[:, hf * 4:hf * 4 + 4, i * 128:(i + 1) * 128], iv), [bk.k], [XT.k])
                if moe[l] and mdbg >= 2:
                    bk = P.bank()
                    for kc in range(8):
                        mm(bk.t[:, 0:8], XTF.t[:, kc, :], ROUT.t[:, kc, :], kc == 0, kc == 7, [XTF.k, ROUT.k], [bk.k], kc == 7)
                    dve(lambda: V.tensor_copy(LG.t[:], bk.t[:, 0:8]), [bk.k], [LG.k])
                if moe[l] and mdbg >= 3:
                    dve(lambda: V.tensor_reduce(SM.t[:, 0:1], LG.t[:], AX.X, ALU.max), [LG.k], [SM.k])
                    dve(lambda: V.tensor_scalar(EQ1.t[:], LG.t[:], SM.t[:, 0:1], None, ALU.is_equal), [LG.k, SM.k], [EQ1.k])
                    dve(lambda: V.scalar_tensor_tensor(LG2.t[:], EQ1.t[:], -1e30, LG.t[:], ALU.mult, ALU.add), [EQ1.k, LG.k], [LG2.k])
                    dve(lambda: V.tensor_reduce(SM.t[:, 1:2], LG2.t[:], AX.X, ALU.max), [LG2.k], [SM.k])
                    dve(lambda: V.tensor_scalar(EQ2.t[:], LG2.t[:], SM.t[:, 1:2], None, ALU.is_equal), [LG2.k, SM.k], [EQ2.k])
                    dve(lambda: V.tensor_tensor(SM.t[:, 2:3], SM.t[:, 1:2], SM.t[:, 0:1], ALU.subtract), [SM.k], [SM.k])
                    act(lambda: A.activation(out=SM.t[:, 3:4], in_=SM.t[:, 2:3], func=AF.Exp), [SM.k], [SM.k])
                    dve(lambda: V.tensor_scalar(SM.t[:, 4:5], SM.t[:, 3:4], 1.0, None, ALU.add), [SM.k], [SM.k])
                    dve(lambda: V.reciprocal(SM.t[:, 5:6], SM.t[:, 4:5]), [SM.k], [SM.k])
                    dve(lambda: V.tensor_tensor(SM.t[:, 6:7], SM.t[:, 3:4], SM.t[:, 5:6], ALU.mult), [SM.k], [SM.k])
                    dve(lambda: V.tensor_scalar(EQ1.t[:], EQ1.t[:], SM.t[:, 5:6], None, ALU.mult), [EQ1.k, SM.k], [EQ1.k])
                    dve(lambda: V.scalar_tensor_tensor(GATE.t[:, i, :], EQ2.t[:], SM.t[:, 6:7], EQ1.t[:], ALU.mult, ALU.add),
                        [EQ2.k, SM.k, EQ1.k], [GATE.k])
            P.barrier()
            P.release(ph)

    def layer_norm(zb, LNP, gi, ST, MV):
        for hf in range(2):
            dve(lambda: V.bn_stats(ST.t[:, hf, :], zb.t[:, hf * 512:(hf + 1) * 512]), [zb.k], [ST.k])
        dve(lambda: V.bn_aggr(MV.t[:, 0:2], ST.t[:].rearrange("p a b -> p (a b)")), [ST.k], [MV.k])
        act(lambda: A.activation(out=MV.t[:, 2:3], in_=MV.t[:, 1:2], func=AF.Sqrt, bias=LN_EPS, scale=1.0), [MV.k], [MV.k])
        dve(lambda: V.reciprocal(MV.t[:, 3:4], MV.t[:, 2:3]), [MV.k], [MV.k])
        dve(lambda: V.tensor_scalar(zb.t[:], zb.t[:], MV.t[:, 0:1], MV.t[:, 3:4], ALU.subtract, ALU.mult), [zb.k, MV.k], [zb.k])
        dve(lambda: V.tensor_tensor(zb.t[:], zb.t[:], LNP.t[:, gi, :], ALU.mult), [zb.k, LNP.k], [zb.k])
        dve(lambda: V.tensor_tensor(zb.t[:], zb.t[:], LNP.t[:, gi + 1, :], ALU.add), [zb.k, LNP.k], [zb.k])

    def phase_F(l, p, dst, tok_dst):
        w = LW[l]
        E = 8 if moe[l] else 1
        with ExitStack() as ph:
            ACC = sb(ph, "fACC", [128, NT // 128, D], F32)
            WGs = [sb(ph, "fWG%d" % i, [128, 8, 256], BF16) for i in range(2)]
            WUs = [sb(ph, "fWU%d" % i, [128, 8, 256], BF16) for i in range(2)]
            WDs = [sb(ph, "fWD%d" % i, [128, 2, 1024], BF16) for i in range(2)]
            HT = [sb(ph, "fHT%d" % i, [128, 2, 512], BF16) for i in range(2)]
            SGT = [sb(ph, "fSG%d" % i, [128, 512], F32) for i in range(2)]
            LNP = sb(ph, "fLNP", [128, 2, 1024], F32)
            P.dma("sp", LNP.t[:], w["lnp"].ap()[:, 2:4, :], W=[LNP.k])
            ST = sb(ph, "fST", [128, 2, 6], F32)
            MV = sb(ph, "fMV", [128, 4], F32)
            XR = [sb(ph, "fXR%d" % i, [128, D], F32) for i in range(2)]
            ntg = NT // 512
            it = 0
            for e in range(E):
                for g in range(NFG):
                    wg, wu, wd = WGs[it % 2], WUs[it % 2], WDs[it % 2]
                    P.dma("pool", wg.t[:], w["wg"].ap()[e, g], W=[wg.k])
                    P.dma("pool", wu.t[:], w["wu"].ap()[e, g], W=[wu.k])
                    P.dma("pool", wd.t[:], w["wd"].ap()[e, g], W=[wd.k])
                    for tg in range(ntg):
                        ts_ = slice(tg * 512, (tg + 1) * 512)
                        ht = HT[tg % 2]
                        for fc in range(2):
                            bg = P.bank()
                            for kc in range(8):
                                mm(bg.t[:], wg.t[:, kc, fc * 128:(fc + 1) * 128], XT.t[:, kc, ts_], kc == 0, kc == 7,
                                   [wg.k, XT.k], [bg.k], kc == 7)
                            bu = P.bank()
                            for kc in range(8):
                                mm(bu.t[:], wu.t[:, kc, fc * 128:(fc + 1) * 128], XT.t[:, kc, ts_], kc == 0, kc == 7,
                                   [wu.k, XT.k], [bu.k], kc == 7)
                            sg = SGT[fc]
                            act(lambda: A.activation(out=sg.t[:], in_=bg.t[:], func=AF.Silu), [bg.k], [sg.k])
                            dve(lambda: V.tensor_tensor(ht.t[:, fc, :], bu.t[:], sg.t[:], ALU.mult), [bu.k, sg.k], [ht.k])
                        for tt_ in range(4):
                            ti = tg * 4 + tt_
                            for hf in range(2):
                                bk = P.bank()
                                for fc in range(2):
                                    mm(bk.t[:], ht.t[:, fc, tt_ * 128:(tt_ + 1) * 128], wd.t[:, fc, hf * 512:(hf + 1) * 512],
                                       fc == 0, fc == 1, [ht.k, wd.k], [bk.k], fc == 1)
                                o = ACC.t[:, ti, hf * 512:(hf + 1) * 512]
                                if moe[l]:
                                    gsc = GATE.t[:, ti, e:e + 1]
                                    if it == 0:
                                        dve(lambda: V.tensor_scalar(o, bk.t[:], gsc, None, ALU.mult), [bk.k, GATE.k], [ACC.k])
                                    else:
                                        dve(lambda: V.scalar_tensor_tensor(o, bk.t[:], gsc, o, ALU.mult, ALU.add),
                                            [bk.k, GATE.k, ACC.k], [ACC.k])
                                else:
                                    if it == 0:
                                        act(lambda: A.copy(o, bk.t[:]), [bk.k], [ACC.k])
                                    else:
                                        dve(lambda: V.tensor_tensor(o, bk.t[:], o, ALU.add), [bk.k, ACC.k], [ACC.k])
                    it += 1
            for i in range(NT // 128):
                r0 = p * NT + i * 128
                xr = XR[i % 2]
                P.dma("sp", xr.t[:], xm.ap()[r0:r0 + 128, :], W=[xr.k])
                dve(lambda: V.scalar_tensor_tensor(xr.t[:], xr.t[:], ALPHA, ACC.t[:, i, :], ALU.mult, ALU.add), [xr.k, ACC.k], [xr.k])
                layer_norm(xr, LNP, 0, ST, MV)
                P.dma("sp", dst[r0:r0 + 128, :], xr.t[:], R=[xr.k], W=[tok_dst], part=True, own=xr.k)
            P.barrier()
            P.release(ph)

    for li, l in enumerate(layers):
        w = LW[l]
        src = x_in.ap() if li == 0 else xl1.ap()
        if li == nlast:
            dst, tok_dst = y_out.ap(), tok_y
        else:
            dst, tok_dst = xl1.ap(), tok_xl1
        P.dma("sp", VEC.t[:], w["vec"].ap(), W=[VEC.k])
        P.dma("sp", V64.t[:], w["vec64"].ap(), W=[V64.k])
        P.dma("pool", W2A2.t[:], w["w2a2"].ap(), W=[W2A2.k])
        P.dma("pool", G2.t[:], w["g2"].ap(), W=[G2.k])
        if moe[l]:
            P.dma("sp", ROUT.t[:], w["router"].ap(), W=[ROUT.k])
        dve(lambda: V.tensor_scalar(OMM.t[:], VEC.t[:, 0:14], -1.0, 1.0, ALU.mult, ALU.add), [VEC.k], [OMM.k])
        dve(lambda: V.tensor_scalar(OMKA.t[:], VEC.t[:, 26:30], -1.0, 1.0, ALU.mult, ALU.add), [VEC.k], [OMKA.k])
        dve(lambda: V.memset(TAILS.t[:], 0.0), [], [TAILS.k])
        dve(lambda: V.memset(UH.t[:, :, 0:32], 0.0), [], [UH.k])
        dve(lambda: V.memset(GH.t[:, :, 0:32], 0.0), [], [GH.k])
        dve(lambda: V.memset(SRING[sci[0] % 3].t[:], 0.0), [], [SRING[sci[0] % 3].k])
        P.barrier()
        for p in range(npass):
            if "R" in phases:
                phase_R(l, p, src if "X" in phases else None)
            elif "X" in phases:
                phase_X(src, p)
            if "B" in phases:
                phase_BC(l, p)
            if "G" in phases:
                phase_GO(l, p, src)
            if "F" in phases:
                phase_F(l, p, dst, tok_dst)
    P.barrier()
    es.close()
    return nc


def make_consts():
    c = np.zeros((128, K_END), np.float32)
    c[:, K_ID:K_ID + 128] = np.eye(128, dtype=np.float32)
    c[0:64, K_I2:K_I2 + 64] = np.eye(64, dtype=np.float32)
    c[64:128, K_I2:K_I2 + 64] = np.eye(64, dtype=np.float32)
    s = np.arange(64)[:, None]
    n = np.arange(64)[None, :]
    c[0:64, K_MKA:K_MKA + 64] = (s < n)
    c[0:64, K_MKA + 64:K_MKA + 128] = (s <= n)
    c[0:64, K_ML:K_ML + 64] = (n < s)
    c[0:64, K_OB:K_OB + 64] = 1.0
    c[64:128, K_OB + 64:K_OB + 128] = 1.0
    c[:, K_OF:K_OF + 128] = 1.0
    sm = np.ones(512, np.float32)
    sm[::64] = 0.0
    c[:, K_SM:K_SM + 512] = sm[None, :]
    return c


def prep_layer(inp, l, is_moe, j):
    f = np.float32
    out = {}
    w_in = np.asarray(inp["w_in"][l], f)
    W = w_in.reshape(8, 128, 48, 128).transpose(2, 1, 0, 3)
    out["winA%d" % l] = np.ascontiguousarray(W[0:14])
    order = list(range(14, 24)) + [24 + br * 8 + i for i in range(8) for br in range(3)]
    out["winB%d" % l] = np.ascontiguousarray(W[order])
    vec = np.zeros((128, NVEC), f)
    vec[:, 0:14] = np.asarray(inp["rwkv_mu"][l], f).reshape(14, 128).T
    for c0, name in ((14, "rwkv_w0"), (18, "rwkv_a0"), (22, "rwkv_k_k"), (26, "rwkv_k_a"), (30, "rwkv_r_k")):
        vec[:, c0:c0 + 4] = np.asarray(inp[name][l], f).reshape(4, 128).T
    cdw = np.asarray(inp["conf_dw"][l], f)
    for c in range(2):
        vec[:, 34 + c * 31:34 + (c + 1) * 31] = cdw[:, c * 128:(c + 1) * 128].T
    vec[:, 96:98] = np.asarray(inp["conf_dw_b"][l], f).reshape(2, 128).T
    vec[:, 98:100] = np.asarray(inp["conf_ln_g"][l], f).reshape(2, 128).T
    vec[:, 100:102] = np.asarray(inp["conf_ln_b"][l], f).reshape(2, 128).T
    sdw = np.asarray(inp["short_dw"][l], f)
    for c in range(2):
        vec[:, 102 + c * 3:102 + (c + 1) * 3] = sdw[:, c * 128:(c + 1) * 128].T
    out["vec%d" % l] = vec
    v64 = np.zeros((64, 16), f)
    v64[:, 0:8] = np.asarray(inp["rwkv_ln_g"][l], f).reshape(8, 64).T
    v64[:, 8:16] = np.asarray(inp["rwkv_ln_b"][l], f).reshape(8, 64).T
    out["vec64_%d" % l] = v64
    out["w2a2_%d" % l] = np.ascontiguousarray(np.concatenate([np.asarray(inp["rwkv_w2"][l], f), np.asarray(inp["rwkv_a2"][l], f)], 0))
    out["g2_%d" % l] = np.ascontiguousarray(np.asarray(inp["rwkv_g2"][l], f))
    out["woa%d" % l] = np.ascontiguousarray(np.asarray(inp["rwkv_w_o"][l], f).reshape(8, 64, 1024).transpose(1, 0, 2))
    out["wob%d" % l] = np.ascontiguousarray(np.asarray(inp["conf_w_o"][l], f).reshape(2, 128, 1024).transpose(1, 0, 2))
    out["woc%d" % l] = np.ascontiguousarray(np.asarray(inp["short_w_o"][l], f).reshape(2, 128, 1024).transpose(1, 0, 2))
    out["wout%d" % l] = np.ascontiguousarray(np.asarray(inp["w_out"][l], f).reshape(8, 128, 1024).transpose(1, 0, 2))
    lnp = np.stack([np.asarray(inp[n][l], f) for n in ("ln1_g", "ln1_b", "ln2_g", "ln2_b")], 0)
    out["lnp%d" % l] = np.ascontiguousarray(np.broadcast_to(lnp[None], (128, 4, 1024)))
    if is_moe:
        wg = np.asarray(inp["moe_w_gate"][j], f)
        wu = np.asarray(inp["moe_w_up"][j], f)
        wd = np.asarray(inp["moe_w_down"][j], f)
        out["router%d" % l] = np.ascontiguousarray(np.asarray(inp["moe_router"][j], f).reshape(8, 128, 8).transpose(1, 0, 2))
    else:
        wg = np.asarray(inp["ffn_w_gate"][j], f)[None]
        wu = np.asarray(inp["ffn_w_up"][j], f)[None]
        wd = np.asarray(inp["ffn_w_down"][j], f)[None]
    E = wg.shape[0]
    out["wg%d" % l] = np.ascontiguousarray(wg.reshape(E, 8, 128, NFG, 256).transpose(0, 3, 2, 1, 4))
    out["wu%d" % l] = np.ascontiguousarray(wu.reshape(E, 8, 128, NFG, 256).transpose(0, 3, 2, 1, 4))
    out["wd%d" % l] = np.ascontiguousarray(wd.reshape(E, NFG, 2, 128, 1024).transpose(0, 1, 3, 2, 4))
    return out


_NC_CACHE = {}


def kernel(**inputs):
    x = np.asarray(inputs["x"], np.float32)
    B, S, _ = x.shape
    if S not in _NC_CACHE:
        _NC_CACHE[S] = build(S)
    nc = _NC_CACHE[S]
    shared = {"consts": make_consts()}
    for l in range(2):
        shared.update(prep_layer(inputs, l, l % 2 == 1, l // 2))
    maps = []
    for b in range(B):
        m = dict(shared)
        m["x"] = np.ascontiguousarray(x[b])
        maps.append(m)
    in_maps = [maps[c % B] for c in range(8)]
    res = run_bass_kernel_spmd(nc, in_maps, core_ids=list(range(8)))
    return np.stack([np.asarray(res.results[b]["y"], np.float32) for b in range(B)], 0)
```
